# Optimizing a Trainium2 kernel written in Bass

```python
import jax, jax.numpy as jnp
from jax import lax
import numpy as np

D_MODEL = 4096
BATCH = 4
SEQ = 2048
DEPTH = 1

CONV_DIM = 2048
CONV_WIDTH = 3
N_HEADS = 16
HEAD_DIM = 128
ATTN_DIM = N_HEADS * HEAD_DIM
MOBA_BLOCK = 256
MOBA_TOPK = 3
Q_CHUNK = 16
N_BRANCH = 2
SPLIT_SIZES = [CONV_DIM] * 3 + [ATTN_DIM] * 3 + [D_MODEL] * N_BRANCH
SPLIT_IDX = [int(i) for i in np.cumsum(SPLIT_SIZES)[:-1]]
IN_COLS = int(sum(SPLIT_SIZES))
PEER_HEADS = 8
N_KEYS = 128
N_EXPERTS = N_KEYS * N_KEYS
PEER_KEY_DIM = 256
PEER_HALF = PEER_KEY_DIM // 2
PEER_TOPK = 16
TOKEN_CHUNK = 128
EPS = 1e-6

kernel_name = "hybrid_conv_moba_peer_block"


def rms_norm(x, g):
    xf = x.astype(jnp.float32)
    y = xf * lax.rsqrt(jnp.mean(xf * xf, axis=-1, keepdims=True) + EPS)
    return (y * g.astype(jnp.float32)).astype(x.dtype)


def short_conv_mixer(b_gate, c_gate, u, conv_w, w_out):
    z = c_gate * u
    z = lax.conv_general_dilated(
        z, conv_w[:, None, :].astype(z.dtype), window_strides=(1,),
        padding=[(CONV_WIDTH - 1, 0)], dimension_numbers=("NWC", "WIO", "NWC"),
        feature_group_count=CONV_DIM)
    return (b_gate * z) @ w_out


def moba_attention(q, k, v, w_out):
    b_, s_, h_, hd = q.shape
    f32 = jnp.float32
    n_blk = -(-s_ // MOBA_BLOCK)
    sp = n_blk * MOBA_BLOCK
    pad = [(0, 0), (0, sp - s_), (0, 0), (0, 0)]
    q, k, v = [jnp.pad(t, pad).transpose(0, 2, 1, 3) for t in (q, k, v)]
    k_blk = k.reshape(b_, h_, n_blk, MOBA_BLOCK, hd)
    v_blk = v.reshape(b_, h_, n_blk, MOBA_BLOCK, hd)
    k_mean = jnp.mean(k_blk.astype(f32), axis=3)
    q_blk_id = jnp.arange(sp) // MOBA_BLOCK
    gate = jnp.einsum("bhsd,bhnd->bhsn", q.astype(f32), k_mean)
    past = jnp.arange(n_blk)[None, :] < q_blk_id[:, None]
    gate = jnp.where(past, gate, -jnp.inf)
    n_sel = min(MOBA_TOPK, n_blk)
    _, sel = lax.top_k(gate, n_sel)
    sel_valid = sel < q_blk_id[:, None]
    n_chunk = sp // Q_CHUNK
    scale = HEAD_DIM ** -0.5
    bi = jnp.arange(b_)[:, None, None, None]
    hi = jnp.arange(h_)[None, :, None, None]

    def chunk(args):
        qc, selc, validc, c = args
        start = c * Q_CHUNK
        own = start // MOBA_BLOCK
        k_own = lax.dynamic_index_in_dim(k_blk, own, axis=2, keepdims=False)
        v_own = lax.dynamic_index_in_dim(v_blk, own, axis=2, keepdims=False)
        q_pos = start + jnp.arange(Q_CHUNK)
        k_pos = own * MOBA_BLOCK + jnp.arange(MOBA_BLOCK)
        s_own = jnp.einsum("bhqd,bhkd->bhqk", qc, k_own).astype(f32) * scale
        s_own = jnp.where(k_pos[None, :] <= q_pos[:, None], s_own, -jnp.inf)
        k_sel = k_blk[bi, hi, selc]
        v_sel = v_blk[bi, hi, selc]
        s_sel = jnp.einsum("bhqd,bhqnkd->bhqnk", qc, k_sel).astype(f32) * scale
        s_sel = jnp.where(validc[..., None], s_sel, -jnp.inf)
        logits = jnp.concatenate([s_own, s_sel.reshape(b_, h_, Q_CHUNK, -1)], axis=-1)
        p = jax.nn.softmax(logits, axis=-1).astype(qc.dtype)
        p_own = p[..., :MOBA_BLOCK]
        p_sel = p[..., MOBA_BLOCK:].reshape(b_, h_, Q_CHUNK, n_sel, MOBA_BLOCK)
        return (jnp.einsum("bhqk,bhkd->bhqd", p_own, v_own)
                + jnp.einsum("bhqnk,bhqnkd->bhqd", p_sel, v_sel))

    qs = q.reshape(b_, h_, n_chunk, Q_CHUNK, hd).transpose(2, 0, 1, 3, 4)
    sels = sel.reshape(b_, h_, n_chunk, Q_CHUNK, n_sel).transpose(2, 0, 1, 3, 4)
    vals = sel_valid.reshape(b_, h_, n_chunk, Q_CHUNK, n_sel).transpose(2, 0, 1, 3, 4)
    out = lax.map(chunk, (qs, sels, vals, jnp.arange(n_chunk)))
    out = out.transpose(1, 0, 3, 2, 4).reshape(b_, sp, h_ * hd)[:, :s_]
    return out @ w_out


def peer_ffn(x, w_q, sub_keys, u_emb, v_emb):
    b_, s_, d_ = x.shape
    xt = x.reshape(-1, d_)
    t_ = xt.shape[0]
    q = (xt @ w_q).reshape(t_, PEER_HEADS, 2, PEER_HALF)
    s = jnp.einsum("thcd,hckd->thck", q, sub_keys).astype(jnp.float32)
    top_s, top_i = lax.top_k(s, PEER_TOPK)
    cand_s = top_s[:, :, 0, :, None] + top_s[:, :, 1, None, :]
    cand_i = top_i[:, :, 0, :, None] * N_KEYS + top_i[:, :, 1, None, :]
    best_s, best_j = lax.top_k(cand_s.reshape(t_, PEER_HEADS, -1), PEER_TOPK)
    expert = jnp.take_along_axis(cand_i.reshape(t_, PEER_HEADS, -1), best_j, axis=-1)
    g = jax.nn.softmax(best_s, axis=-1).astype(x.dtype)
    n_chunk = t_ // TOKEN_CHUNK

    def chunk(args):
        xc, ec, gc = args
        hid = jax.nn.gelu(jnp.einsum("td,thkd->thk", xc, u_emb[ec]), approximate=False) * gc
        return jnp.einsum("thk,thkd->td", hid, v_emb[ec])

    out = lax.map(chunk, (xt.reshape(n_chunk, TOKEN_CHUNK, d_),
                          expert.reshape(n_chunk, TOKEN_CHUNK, PEER_HEADS, PEER_TOPK),
                          g.reshape(n_chunk, TOKEN_CHUNK, PEER_HEADS, PEER_TOPK)))
    return out.reshape(b_, s_, d_)


def setup_inputs(seed: int = 0) -> dict:
    key = jax.random.key(seed)
    ks = jax.random.split(key, 16)
    nrm = jax.random.normal
    f32 = jnp.float32
    return {
        "x": nrm(ks[0], (BATCH, SEQ, D_MODEL), f32),
        "norm_mix": 1.0 + 0.02 * nrm(ks[1], (DEPTH, D_MODEL), f32),
        "w_in": nrm(ks[2], (DEPTH, D_MODEL, IN_COLS), f32) * D_MODEL ** -0.5,
        "b_gate": 0.02 * nrm(ks[3], (DEPTH, N_BRANCH * D_MODEL), f32),
        "conv_w": nrm(ks[4], (DEPTH, CONV_WIDTH, CONV_DIM), f32) * CONV_WIDTH ** -0.5,
        "w_conv_out": nrm(ks[5], (DEPTH, CONV_DIM, D_MODEL), f32) * CONV_DIM ** -0.5,
        "w_attn_out": nrm(ks[6], (DEPTH, ATTN_DIM, D_MODEL), f32) * ATTN_DIM ** -0.5,
        "w_o": nrm(ks[7], (DEPTH, D_MODEL, D_MODEL), f32) * D_MODEL ** -0.5,
        "norm_ffn": 1.0 + 0.02 * nrm(ks[8], (DEPTH, D_MODEL), f32),
        "w_peer_q": nrm(ks[9], (DEPTH, D_MODEL, PEER_HEADS * PEER_KEY_DIM), f32) * D_MODEL ** -0.5,
        "sub_keys": nrm(ks[10], (DEPTH, PEER_HEADS, 2, N_KEYS, PEER_HALF), f32) * PEER_HALF ** -0.5,
        "u_emb": nrm(ks[11], (DEPTH, N_EXPERTS, D_MODEL), f32) * D_MODEL ** -0.5,
        "v_emb": nrm(ks[12], (DEPTH, N_EXPERTS, D_MODEL), f32) * PEER_HEADS ** -0.5,
        "norm_final": 1.0 + 0.02 * nrm(ks[13], (D_MODEL,), f32),
    }


def reference(x, norm_mix, w_in, b_gate, conv_w, w_conv_out, w_attn_out, w_o,
              norm_ffn, w_peer_q, sub_keys, u_emb, v_emb, norm_final):
    h = x
    b_, s_, _ = x.shape
    for l in range(DEPTH):
        hn = rms_norm(h, norm_mix[l])
        z = hn @ w_in[l]
        bc, cc, uc, q, k, v, gc, ga = jnp.split(z, SPLIT_IDX, axis=-1)
        bias_c, bias_a = jnp.split(b_gate[l], 2)
        y_conv = short_conv_mixer(bc, cc, uc, conv_w[l], w_conv_out[l])
        y_attn = moba_attention(q.reshape(b_, s_, N_HEADS, HEAD_DIM),
                                k.reshape(b_, s_, N_HEADS, HEAD_DIM),
                                v.reshape(b_, s_, N_HEADS, HEAD_DIM), w_attn_out[l])
        merged = jax.nn.sigmoid(gc + bias_c) * y_conv + jax.nn.sigmoid(ga + bias_a) * y_attn
        h = h + merged @ w_o[l]
        h = h + peer_ffn(rms_norm(h, norm_ffn[l]), w_peer_q[l], sub_keys[l], u_emb[l], v_emb[l])
    return rms_norm(h, norm_final)
```

```python
import numpy as np
import concourse.bass as bass
import concourse.mybir as mybir
from concourse.bass_utils import run_bass_kernel_spmd

F32 = mybir.dt.float32
BF16 = mybir.dt.bfloat16
U8 = mybir.dt.uint8
AF = mybir.ActivationFunctionType
ALU = mybir.AluOpType
AX = mybir.AxisListType

SEM_LIMIT = 30000
SWDGE_MAX_DESC = 6000
D = 4096
T = 1024
NEG = -60000.0
BIG = 1.0e30
EPS = 1e-6


class SemCtr:
    def __init__(self, nc, name):
        self.nc = nc
        self.name = name
        self.n = 0
        self.sem = nc.alloc_semaphore(name=f"{name}_{self.n}")
        self.val = 0

    def bump(self, k):
        if self.val + k > SEM_LIMIT:
            self.n += 1
            self.sem = self.nc.alloc_semaphore(name=f"{self.name}_{self.n}")
            self.val = 0
        self.val += k
        return (self.sem, self.val)


class Buf:
    _id = 0

    def __init__(self, fw, name, ap):
        self.fw = fw
        self.name = name
        self.ap = ap
        self.w = []
        self.r = []
        self.dsem = None

    def __getitem__(self, idx):
        return self.ap[idx]

    def dma_sem(self):
        if self.dsem is None:
            Buf._id += 1
            self.dsem = self.fw.get_dma_sem()
        return self.dsem


class Eng:
    def __init__(self, fw, name, handle):
        self.name = name
        self.h = handle
        self.ctr = SemCtr(fw.nc, f"e_{name}")
        self.seen = {}
        self.prog = []


class FW:
    def __init__(self, nc):
        self.nc = nc
        self.engs = {
            "pe": Eng(self, "pe", nc.tensor),
            "dve": Eng(self, "dve", nc.vector),
            "act": Eng(self, "act", nc.scalar),
            "pool": Eng(self, "pool", nc.gpsimd),
            "sp": Eng(self, "sp", nc.sync),
        }
        self.dma_sems = []
        self.free_dma_sems = []
        self.n_inst = 0
        self.swq = []

    def get_dma_sem(self):
        if self.free_dma_sems:
            return self.free_dma_sems.pop()
        s = SemCtr(self.nc, f"d{len(self.dma_sems)}")
        self.dma_sems.append(s)
        return s

    def dram(self, name, shape, dtype, kind="Internal"):
        t = self.nc.dram_tensor(name, list(shape), dtype, kind=kind)
        return Buf(self, name, t.ap())

    def _deps(self, eng, reads, writes):
        deps = {}

        def add(tok):
            sem, val = tok
            k = id(sem)
            if k not in deps or deps[k][1] < val:
                deps[k] = (sem, val)

        for b in reads:
            for tok in b.w:
                add(tok)
        for b in writes:
            for tok in b.w:
                add(tok)
            for tok in b.r:
                add(tok)
        out = []
        for k, (sem, val) in deps.items():
            if eng.name == "pe" and sem is eng.ctr.sem:
                continue
            if eng.seen.get(k, 0) >= val:
                continue
            eng.seen[k] = val
            out.append((sem, val))
        return out

    def _record(self, tok, reads, writes):
        for b in writes:
            b.w = [tok]
            b.r = []
        for b in reads:
            b.r = [t for t in b.r if t[0] is not tok[0]] + [tok]

    def op(self, engname, fn, reads=(), writes=()):
        eng = self.engs[engname]
        waits = self._deps(eng, reads, writes)
        tok = eng.ctr.bump(1)
        eng.prog.append((waits, fn, tok))
        self._record(tok, reads, writes)
        self.n_inst += 1
        return tok

    def dma(self, engname, out_ap, in_ap, reads=(), writes=(), **kw):
        eng = self.engs[engname]
        waits = self._deps(eng, reads, writes)
        if engname == "pool":
            nd = kw.pop("ndesc", 4096)
            while self.swq and sum(n for _, n in self.swq) + nd > SWDGE_MAX_DESC:
                (sem, val), _ = self.swq.pop(0)
                if eng.seen.get(id(sem), 0) < val:
                    eng.seen[id(sem)] = val
                    waits.append((sem, val))
        else:
            kw.pop("ndesc", None)
        tok = writes[0].dma_sem().bump(16)
        if engname == "pool":
            self.swq.append((tok, nd))

        def fn(h, out_ap=out_ap, in_ap=in_ap, kw=kw):
            return h.dma_start(out=out_ap, in_=in_ap, **kw)

        eng.prog.append((waits, fn, ("dma", tok)))
        self._record(tok, reads, writes)
        self.n_inst += 1
        return tok

    def barrier(self, bufs=()):
        toks = []
        for e in self.engs.values():
            if e.ctr.val > 0:
                toks.append((e.ctr.sem, e.ctr.val))
        for s in self.dma_sems:
            if s.val > 0:
                toks.append((s.sem, s.val))
        for e in self.engs.values():
            waits = []
            for sem, val in toks:
                k = id(sem)
                if e.seen.get(k, 0) >= val:
                    continue
                e.seen[k] = val
                waits.append((sem, val))
            e.prog.append((waits, None, None))

    def finish(self):
        nc = self.nc
        with nc.Block() as block:
            def replay(eng):
                def body(h):
                    for waits, fn, tok in eng.prog:
                        for sem, val in waits:
                            h.wait_ge(sem, val)
                        if fn is None:
                            continue
                        inst = fn(h)
                        if tok[0] == "dma":
                            inst.then_inc(tok[1][0], 16)
                        else:
                            inst.then_inc(tok[0], 1)
                return body

            block.tensor(replay(self.engs["pe"]))
            block.vector(replay(self.engs["dve"]))
            block.scalar(replay(self.engs["act"]))
            block.gpsimd(replay(self.engs["pool"]))
            block.sync(replay(self.engs["sp"]))


class Arena:
    def __init__(self, fw, nbytes):
        self.fw = fw
        self.t = fw.nc.alloc_sbuf_tensor("arena", [128, nbytes], U8)
        self.size = nbytes
        self.off = 0

    def alloc(self, name, shape, dtype):
        esz = 2 if dtype == BF16 else 4
        n = 1
        for s in shape:
            n *= s
        nb = (n * esz + 63) // 64 * 64
        assert self.off + nb <= self.size, f"arena overflow at {name}: {self.off + nb}"
        ap = self.t[:, self.off:self.off + n * esz].bitcast(dtype)
        self.off += nb
        if len(shape) == 2:
            ap = ap.rearrange("p (a b) -> p a b", b=shape[1])
        elif len(shape) == 3:
            ap = ap.rearrange("p (a b c) -> p a b c", b=shape[1], c=shape[2])
        return Buf(self.fw, name, ap)

    def mark(self):
        return self.off

    def reset(self, mark):
        self.fw.barrier()
        self.off = mark


def build(debug=False, upto=None):
    nc = bass.Bass("TRN2", target_bir_lowering=False)
    fw = FW(nc)
    op, dma = fw.op, fw.dma
    EI = "ExternalInput"
    xe = fw.dram("xe", [2048, D], F32, EI)
    w_in = fw.dram("w_in", [D, 20480], F32, EI)
    w_co = fw.dram("w_co", [2048, D], F32, EI)
    w_ao = fw.dram("w_ao", [2048, D], F32, EI)
    w_o = fw.dram("w_o", [D, D], F32, EI)
    w_pq = fw.dram("w_pq", [D, 2048], F32, EI)
    skeys = fw.dram("skeys", [16, 128, 128], F32, EI)
    u_emb = fw.dram("u_emb", [16384, D], F32, EI)
    v_emb = fw.dram("v_emb", [16384, D], F32, EI)
    gvec = fw.dram("gvec", [3, D], F32, EI)
    cfm = fw.dram("cfm", [128, 64 + 48], F32, EI)
    cid = fw.dram("cid", [128, 128], F32, EI)
    ccausal = fw.dram("ccausal", [128, 512], F32, EI)
    csel8 = fw.dram("csel8", [8, 1024], F32, EI)
    cpast = fw.dram("cpast", [128, 64], F32, EI)
    sk = "ExternalOutput" if debug else "Internal"
    KTc = fw.dram("KTc", [16, 128, 1024], BF16, sk)
    Vc = fw.dram("Vc", [16, 8, 128, 128], BF16, sk)
    SG = fw.dram("SG", [2, D, T], F32, sk)
    ZC = fw.dram("ZC", [2048, T], BF16, sk)
    AT = fw.dram("AT", [2048, T], BF16, sk)
    H2 = fw.dram("H2", [T, D], F32, sk)
    out = fw.dram("out", [T, D], F32, "ExternalOutput")

    ar = Arena(fw, 206 * 1024)
    ps = []
    for i in range(8):
        t = nc.alloc_psum_tensor(f"ps{i}", [128, 512], F32)
        ps.append(Buf(fw, f"ps{i}", t[:]))

    def psbf(i):
        return ps[i].ap.bitcast(BF16)

    ident_f = ar.alloc("ident_f", [128], F32)
    ident = ar.alloc("ident", [128], BF16)
    ones = ar.alloc("ones", [128], BF16)
    small_mark = ar.mark()
    causal_f = ar.alloc("causal_f", [512], F32)
    causal = ar.alloc("causal", [2, 256], BF16)
    sel8_f = ar.alloc("sel8_f", [1024], F32)
    sel8 = ar.alloc("sel8", [8, 128], BF16)
    past = ar.alloc("past", [8, 8], F32)
    cf = ar.alloc("cf", [112], F32)
    kmsum = ar.alloc("kmsum", [16, 8], F32)
    halo = ar.alloc("halo", [32, 32], BF16)
    dma("sp", ident_f[:], cid[:], writes=[ident_f])
    dma("sp", causal_f[:], ccausal[:], writes=[causal_f])
    dma("sp", sel8_f[0:8], csel8[:], writes=[sel8_f])
    dma("sp", past[:].rearrange("p a b -> p (a b)"), cpast[:], writes=[past])
    dma("sp", cf[:], cfm[:], writes=[cf])
    op("dve", lambda h: h.tensor_copy(out=ident[:], in_=ident_f[:]), [ident_f], [ident])
    op("dve", lambda h: h.memset(ones[:], 1.0), [], [ones])
    op("dve", lambda h: h.tensor_copy(out=causal[:].rearrange("p a b -> p (a b)"), in_=causal_f[:]), [causal_f], [causal])
    op("dve", lambda h: h.tensor_copy(out=sel8[0:8].rearrange("p a b -> p (a b)"), in_=sel8_f[0:8]), [sel8_f], [sel8])
    base_mark = ar.mark()
    if upto == "A0":
        fw.barrier()
        fw.finish()
        return nc, fw

    def bgate(n):
        return cf[:, n:n + 1]

    def convw(k, j):
        return cf[:, 64 + k * 16 + j:64 + k * 16 + j + 1]

    def load_w(dst, src, rows, parts):
        nkc = rows // 128
        off = 0
        for (c0, wd) in parts:
            s = src[0:rows, c0:c0 + wd].rearrange("(kc p) n -> p kc n", p=128)
            dma("pool", dst[:, 0:nkc, off:off + wd], s, writes=[dst], ndesc=128 * nkc)
            off += wd

    def norm_transpose(src_fn, gidx, dstT, ntiles, tag):
        g_bc = ar.alloc(f"gbc{tag}", [D], F32)
        dma("sp", g_bc[:], gvec[gidx:gidx + 1, :].partition_broadcast(128), writes=[g_bc])
        xts = [ar.alloc(f"xt{tag}{i}", [D], F32) for i in range(2)]
        xns = [ar.alloc(f"xn{tag}{i}", [D], BF16) for i in range(2)]
        st = [ar.alloc(f"st{tag}{i}", [4], F32) for i in range(2)]
        for tt in range(ntiles):
            xt, xn, s = xts[tt % 2], xns[tt % 2], st[tt % 2]
            dma("sp", xt[:], src_fn(tt), writes=[xt])
            op("act", lambda h, xt=xt, xn=xn, s=s: h.activation(out=xn[:], in_=xt[:], func=AF.Square, accum_out=s[:, 0:1]), [xt], [xn, s])
            op("dve", lambda h, s=s: h.tensor_scalar(out=s[:, 1:2], in0=s[:, 0:1], scalar1=1.0 / D, scalar2=EPS, op0=ALU.mult, op1=ALU.add), [s], [s])
            op("act", lambda h, s=s: h.activation(out=s[:, 2:3], in_=s[:, 1:2], func=AF.Sqrt), [s], [s])
            op("dve", lambda h, s=s: h.reciprocal(out=s[:, 3:4], in_=s[:, 2:3]), [s], [s])
            op("dve", lambda h, xt=xt, xn=xn, s=s: h.scalar_tensor_tensor(out=xn[:], in0=xt[:], scalar=s[:, 3:4], in1=g_bc[:], op0=ALU.mult, op1=ALU.mult), [xt, s, g_bc], [xn])
            for b in range(4):
                pb = ps[(tt * 4 + b) % 8]
                pv = psbf((tt * 4 + b) % 8)
                for i in range(8):
                    dc = b * 8 + i
                    op("pe", lambda h, pv=pv, xn=xn, i=i, dc=dc: h.transpose(out=pv[:, i * 128:(i + 1) * 128], in_=xn[:, dc * 128:(dc + 1) * 128], identity=ident[:]), [xn, ident], [pb])
                e = "act" if b % 2 == 0 else "dve"
                src = pv.rearrange("p (a b) -> p a b", b=128)
                dst = dstT[:, b * 8:(b + 1) * 8, tt * 128:(tt + 1) * 128]
                if e == "act":
                    op("act", lambda h, src=src, dst=dst: h.activation(out=dst, in_=src, func=AF.Copy), [pb], [dstT])
                else:
                    op("dve", lambda h, src=src, dst=dst: h.tensor_copy(out=dst, in_=src), [pb], [dstT])

    def proj_fm(wb, col0, hnT, nkc, half, pbuf):
        for kc in range(nkc):
            op("pe", lambda h, kc=kc: h.matmul(pbuf[:], lhsT=wb[:, kc, col0:col0 + 128], rhs=hnT[:, kc, half * 512:(half + 1) * 512], start=(kc == 0), stop=(kc == nkc - 1)), [wb, hnT], [pbuf])

    hnT = ar.alloc("hnT", [32, T], BF16)
    m_hn = ar.mark()
    norm_transpose(lambda tt: xe[tt * 128:(tt + 1) * 128, :], 0, hnT, 8, "c")
    op("dve", lambda h: h.memset(halo[:], 0.0), [], [halo])
    op("dve", lambda h: h.tensor_copy(out=halo[:, :, 0:2], in_=hnT[:, :, T - 2:T]), [hnT], [halo])
    if upto == "A":
        fw.barrier()
        fw.finish()
        return nc, fw
    ar.reset(m_hn)

    def kv_transposes(vt, vtk, pbank):
        pv = psbf(pbank)
        for tt in range(8):
            op("pe", lambda h, tt=tt: h.transpose(out=pv[:, tt * 128:(tt + 1) * 128], in_=vt[:, tt * 128:(tt + 1) * 128], identity=ident[:]), [vt, ident], [ps[pbank]])
        op("act", lambda h: h.activation(out=vtk[:].rearrange("p a b -> p (a b)"), in_=pv, func=AF.Copy), [ps[pbank]], [vtk])

    wbs = [ar.alloc(f"wbB{i}", [32, 256], BF16) for i in range(2)]
    kts = [ar.alloc(f"ktB{i}", [T], BF16) for i in range(2)]
    vts = [ar.alloc(f"vtB{i}", [T], BF16) for i in range(2)]
    vtoks = [ar.alloc(f"vtokB{i}", [8, 128], BF16) for i in range(2)]

    def ldB(hd):
        load_w(wbs[hd % 2], w_in, D, [(4 * 2048 + hd * 128, 128), (5 * 2048 + hd * 128, 128)])

    ldB(0)
    pi = 0
    if upto == "B0":
        fw.barrier()
        fw.finish()
        return nc, fw
    for hd in range(16):
        if hd + 1 < 16:
            ldB(hd + 1)
        wb, kt, vt, vtk = wbs[hd % 2], kts[hd % 2], vts[hd % 2], vtoks[hd % 2]
        for part, dst in ((0, kt), (1, vt)):
            for half in range(2):
                pb = ps[pi % 4]
                pi += 1
                proj_fm(wb, part * 128, hnT, 32, half, pb)
                if part == 0:
                    for q_ in range(2):
                        o_ = half * 512 + q_ * 256
                        op("act", lambda h, pb=pb, dst=dst, o_=o_, q_=q_, jj=2 * half + q_, hd=hd: h.activation(out=dst[:, o_:o_ + 256], in_=pb[:, q_ * 256:(q_ + 1) * 256], func=AF.Copy, accum_out=kmsum[:, hd, jj:jj + 1]), [pb], [dst, kmsum])
                else:
                    op("act", lambda h, pb=pb, dst=dst, half=half: h.activation(out=dst[:, half * 512:(half + 1) * 512], in_=pb[:], func=AF.Copy), [pb], [dst])
        if upto == "B1" and hd == 0:
            fw.barrier()
            fw.finish()
            return nc, fw
        dma("sp", KTc[hd], kt[:], reads=[kt], writes=[KTc])
        if upto == "B2" and hd == 0:
            fw.barrier()
            fw.finish()
            return nc, fw
        kv_transposes(vt, vtk, 4 + hd % 2)
        if upto == "B3" and hd == 0:
            fw.barrier()
            fw.finish()
            return nc, fw
        dma("sp", Vc[hd].rearrange("tt p d -> p tt d"), vtk[:], reads=[vtk], writes=[Vc])

    if upto == "B":
        fw.barrier()
        fw.finish()
        return nc, fw
    ar.reset(m_hn)
    norm_transpose(lambda tt: xe[T + tt * 128:T + (tt + 1) * 128, :], 0, hnT, 8, "o")
    ar.reset(m_hn)

    wbs = [ar.alloc(f"wbG{i}", [32, 512], BF16) for i in range(2)]
    sgt = [ar.alloc(f"sgt{i}", [T], F32) for i in range(4)]

    def ldG(nb):
        load_w(wbs[nb % 2], w_in, D, [(6 * 2048 + nb * 256, 256), (6 * 2048 + 4096 + nb * 256, 256)])

    ldG(0)
    si = 0
    for nb in range(16):
        if nb + 1 < 16:
            ldG(nb + 1)
        wb = wbs[nb % 2]
        for part in range(2):
            for sc in range(2):
                n = nb * 2 + sc
                sg = sgt[si % 4]
                si += 1
                for half in range(2):
                    pb = ps[pi % 4]
                    pi += 1
                    proj_fm(wb, part * 256 + sc * 128, hnT, 32, half, pb)
                    op("act", lambda h, pb=pb, sg=sg, half=half, bn=part * 32 + n: h.activation(out=sg[:, half * 512:(half + 1) * 512], in_=pb[:], func=AF.Sigmoid, bias=bgate(bn)), [pb, cf], [sg])
                dma("sp", SG[part, n * 128:(n + 1) * 128, :], sg[:], reads=[sg], writes=[SG])

    if upto == "C2":
        fw.barrier()
        fw.finish()
        return nc, fw
    ar.reset(m_hn)
    wbs = [ar.alloc(f"wbC{i}", [32, 384], BF16) for i in range(2)]
    zb = [ar.alloc(f"z{i}", [T + 2], F32) for i in range(2)]
    zct = [ar.alloc(f"zct{i}", [T], BF16) for i in range(2)]
    usb = [ar.alloc(f"usb{i}", [512], F32) for i in range(2)]
    tmpc = [ar.alloc(f"tmpc{i}", [512], F32) for i in range(2)]
    uh = ar.alloc("uh", [2], F32)

    def ldC(j):
        load_w(wbs[j % 2], w_in, D, [(j * 128, 128), (2048 + j * 128, 128), (4096 + j * 128, 128)])

    ldC(0)
    for j in range(16):
        if j + 1 < 16:
            ldC(j + 1)
        wb, z, zc = wbs[j % 2], zb[j % 2], zct[j % 2]
        for pidx, c0 in ((0, 128), (1, 256)):
            for kc in range(32):
                op("pe", lambda h, kc=kc, c0=c0, pidx=pidx, wb=wb: h.matmul(ps[6 + pidx][:, 0:32], lhsT=wb[:, kc, c0:c0 + 128], rhs=halo[:, kc, :], start=(kc == 0), stop=(kc == 31)), [wb, halo], [ps[6 + pidx]])
        for half in range(2):
            pB, pC, pU = ps[half * 3], ps[half * 3 + 1], ps[half * 3 + 2]
            proj_fm(wb, 0, hnT, 32, half, pB)
            proj_fm(wb, 128, hnT, 32, half, pC)
            proj_fm(wb, 256, hnT, 32, half, pU)
            us, tm = usb[half], tmpc[half]
            o = half * 512
            if half == 0:
                op("act", lambda h: h.activation(out=uh[:], in_=ps[7][:, 0:2], func=AF.Copy), [ps[7], pU], [uh])
            op("act", lambda h, us=us, pU=pU: h.activation(out=us[:], in_=pU[:], func=AF.Copy), [pU], [us])
            op("dve", lambda h, z=z, o=o, pC=pC, us=us: h.tensor_tensor(out=z[:, 2 + o:2 + o + 512], in0=pC[:], in1=us[:], op=ALU.mult), [pC, us], [z])
            if half == 0:
                op("dve", lambda h, z=z: h.tensor_tensor(out=z[:, 0:2], in0=ps[6][:, 0:2], in1=uh[:], op=ALU.mult), [ps[6], uh], [z])
            op("dve", lambda h, z=z, o=o, tm=tm, j=j: h.tensor_scalar(out=tm[:], in0=z[:, 2 + o:2 + o + 512], scalar1=convw(2, j), scalar2=None, op0=ALU.mult), [z, cf], [tm])
            op("dve", lambda h, z=z, o=o, tm=tm, j=j: h.scalar_tensor_tensor(out=tm[:], in0=z[:, 1 + o:1 + o + 512], scalar=convw(1, j), in1=tm[:], op0=ALU.mult, op1=ALU.add), [z, cf, tm], [tm])
            op("dve", lambda h, z=z, o=o, tm=tm, j=j: h.scalar_tensor_tensor(out=tm[:], in0=z[:, o:o + 512], scalar=convw(0, j), in1=tm[:], op0=ALU.mult, op1=ALU.add), [z, cf, tm], [tm])
            op("dve", lambda h, zc=zc, o=o, tm=tm, pB=pB: h.tensor_tensor(out=zc[:, o:o + 512], in0=pB[:], in1=tm[:], op=ALU.mult), [pB, tm], [zc])
        dma("sp", ZC[j * 128:(j + 1) * 128, :], zc[:], reads=[zc], writes=[ZC])

    if upto == "C3":
        fw.barrier()
        fw.finish()
        return nc, fw
    ar.reset(m_hn)
    wbs = [ar.alloc(f"wbH{i}", [32, 384], BF16) for i in range(2)]
    ktcs = [ar.alloc(f"ktc{i}", [T], BF16) for i in range(2)]
    vcs = [ar.alloc(f"vc{i}", [8, 128], BF16) for i in range(2)]
    QTs = [ar.alloc(f"QT{i}", [T], BF16) for i in range(2)]
    KTs = [ar.alloc(f"KT{i}", [T], BF16) for i in range(2)]
    VTs = [ar.alloc(f"VT{i}", [T], BF16) for i in range(2)]
    vos = [ar.alloc(f"vo{i}", [8, 128], BF16) for i in range(2)]
    negTs = [ar.alloc(f"negT{i}", [T], BF16) for i in range(2)]
    kmb = ar.alloc("kmb", [8], BF16)
    gsb = ar.alloc("gsb", [8, 8], F32)
    g8 = ar.alloc("g8", [8, 8], F32)
    tg = ar.alloc("tg", [8], F32)
    selm = ar.alloc("selm", [8, 8], F32)
    negm = ar.alloc("negm", [8, 8], BF16)
    pts = [ar.alloc(f"pt{i}", [256], BF16) for i in range(3)]
    rden = ar.alloc("rden", [256], F32)
    att = [ar.alloc(f"att{i}", [T], BF16) for i in range(2)]
    scale = 128.0 ** -0.5

    def ldH(hd):
        load_w(wbs[hd % 2], w_in, D, [(3 * 2048 + hd * 128, 128), (4 * 2048 + hd * 128, 128), (5 * 2048 + hd * 128, 128)])
        dma("sp", ktcs[hd % 2][:], KTc[hd], reads=[KTc], writes=[ktcs[hd % 2]])
        dma("sp", vcs[hd % 2][:], Vc[hd].rearrange("tt p d -> p tt d"), reads=[Vc], writes=[vcs[hd % 2]])

    def stageP(hd):
        wb = wbs[hd % 2]
        QT, KT, VT, vo, negT = QTs[hd % 2], KTs[hd % 2], VTs[hd % 2], vos[hd % 2], negTs[hd % 2]
        for part, dst in ((0, QT), (1, KT), (2, VT)):
            for half in range(2):
                pb = ps[half]
                proj_fm(wb, part * 128, hnT, 32, half, pb)
                if part == 1:
                    for q_ in range(2):
                        o_ = half * 512 + q_ * 256
                        op("act", lambda h, pb=pb, dst=dst, o_=o_, q_=q_, jj=4 + 2 * half + q_, hd=hd: h.activation(out=dst[:, o_:o_ + 256], in_=pb[:, q_ * 256:(q_ + 1) * 256], func=AF.Copy, accum_out=kmsum[:, hd, jj:jj + 1]), [pb], [dst, kmsum])
                else:
                    op("act", lambda h, pb=pb, dst=dst, half=half: h.activation(out=dst[:, half * 512:(half + 1) * 512], in_=pb[:], func=AF.Copy), [pb], [dst])
        kv_transposes(VT, vo, 2)
        op("dve", lambda h, hd=hd: h.tensor_scalar(out=kmb[:], in0=kmsum[:, hd, :], scalar1=1.0 / 256.0, scalar2=None, op0=ALU.mult), [kmsum], [kmb])
        for tt in range(8):
            op("pe", lambda h, tt=tt, QT=QT: h.matmul(ps[2][:, tt * 8:(tt + 1) * 8], lhsT=QT[:, tt * 128:(tt + 1) * 128], rhs=kmb[:], start=True, stop=True), [QT, kmb], [ps[2]])
        op("dve", lambda h: h.tensor_tensor(out=gsb[:].rearrange("p a b -> p (a b)"), in0=ps[2][:, 0:64], in1=past[:].rearrange("p a b -> p (a b)"), op=ALU.add), [ps[2], past], [gsb])
        for tt in range(8):
            op("dve", lambda h, tt=tt: h.max(out=g8[:, tt, :], in_=gsb[:, tt, :]), [gsb], [g8])
        op("dve", lambda h: h.tensor_tensor(out=tg[:], in0=g8[:, :, 2], in1=g8[:, :, 3], op=ALU.add), [g8], [tg])
        op("dve", lambda h: h.tensor_scalar(out=tg[:], in0=tg[:], scalar1=0.5, scalar2=-0.9 * BIG, op0=ALU.mult, op1=ALU.max), [tg], [tg])
        op("dve", lambda h: h.tensor_tensor(out=selm[:], in0=gsb[:], in1=tg[:].unsqueeze(2).to_broadcast([128, 8, 8]), op=ALU.is_gt), [gsb, tg], [selm])
        op("dve", lambda h: h.tensor_scalar(out=negm[:], in0=selm[:], scalar1=-1.0, scalar2=-NEG, op0=ALU.add, op1=ALU.mult), [selm], [negm])

    def stageP2(hd):
        negT = negTs[hd % 2]
        pv2 = psbf(2)
        for tt in range(8):
            op("pe", lambda h, tt=tt, pv2=pv2: h.transpose(out=pv2[0:8, tt * 128:(tt + 1) * 128], in_=negm[:, tt, :], identity=ident[:]), [negm, ident], [ps[2]])
        op("act", lambda h, pv2=pv2, negT=negT: h.activation(out=negT[0:8, :], in_=pv2[0:8, :], func=AF.Copy), [ps[2]], [negT])

    sidx = [0]

    def stageA(hd):
        ktc, vc, at = ktcs[hd % 2], vcs[hd % 2], att[hd % 2]
        QT, KT, vo, negT = QTs[hd % 2], KTs[hd % 2], vos[hd % 2], negTs[hd % 2]
        for qb in range(4):
            qs = slice(qb * 256, (qb + 1) * 256)
            blocks = [(j, kc) for j in range(4 + qb + 1) for kc in range(2)]
            for bi, (j, kc) in enumerate(blocks):
                pS = ps[(3, 4, 7)[sidx[0] % 3]]
                pt = pts[sidx[0] % 3]
                sidx[0] += 1
                if j < 4:
                    kap = ktc[:, j * 256 + kc * 128:j * 256 + kc * 128 + 128]
                    vap = vc[:, 2 * j + kc, :]
                    kbuf, vbuf = ktc, vc
                else:
                    kap = KT[:, (j - 4) * 256 + kc * 128:(j - 4) * 256 + kc * 128 + 128]
                    vap = vo[:, 2 * (j - 4) + kc, :]
                    kbuf, vbuf = KT, vo
                op("pe", lambda h, pS=pS, kap=kap, qs=qs, QT=QT: h.matmul(pS[:, 0:256], lhsT=kap, rhs=QT[:, qs], start=True, stop=False), [kbuf, QT], [pS])
                if j == 4 + qb:
                    op("pe", lambda h, pS=pS, kc=kc: h.matmul(pS[:, 0:256], lhsT=ident[:], rhs=causal[:, kc, :], start=False, stop=True), [ident, causal], [pS])
                else:
                    op("pe", lambda h, pS=pS, j=j, qs=qs, negT=negT: h.matmul(pS[:, 0:256], lhsT=sel8[0:8, j, :], rhs=negT[0:8, qs], start=False, stop=True), [sel8, negT], [pS])
                op("act", lambda h, pS=pS, pt=pt: h.activation(out=pt[:], in_=pS[:, 0:256], func=AF.Exp, scale=scale), [pS], [pt])
                first, last = bi == 0, bi == len(blocks) - 1
                op("pe", lambda h, vap=vap, pt=pt, first=first, last=last: h.matmul(ps[5][:, 0:256], lhsT=vap, rhs=pt[:], start=first, stop=last), [vbuf, pt], [ps[5]])
                op("pe", lambda h, pt=pt, first=first, last=last: h.matmul(ps[6][:, 0:256], lhsT=ones[:], rhs=pt[:], start=first, stop=last), [ones, pt], [ps[6]])
            op("dve", lambda h: h.reciprocal(out=rden[:], in_=ps[6][:, 0:256]), [ps[6]], [rden])
            op("dve", lambda h, at=at, qs=qs: h.tensor_tensor(out=at[:, qs], in0=ps[5][:, 0:256], in1=rden[:], op=ALU.mult), [ps[5], rden], [at])
        dma("sp", AT[hd * 128:(hd + 1) * 128, :], at[:], reads=[at], writes=[AT])

    ldH(0)
    stageP(0)
    stageP2(0)
    for hd in range(16):
        if hd + 1 < 16:
            ldH(hd + 1)
            stageP(hd + 1)
        stageA(hd)
        if hd + 1 < 16:
            stageP2(hd + 1)

    if upto == "C4":
        fw.barrier()
        fw.finish()
        return nc, fw
    ar.reset(base_mark)
    mergedT = ar.alloc("mergedT", [32, T], BF16)
    m_mg = ar.mark()
    zcT = ar.alloc("zcT", [16, T], BF16)
    atT = ar.alloc("atT", [16, T], BF16)
    dma("sp", zcT[:], ZC[:].rearrange("(c p) t -> p c t", p=128), reads=[ZC], writes=[zcT])
    dma("sp", atT[:], AT[:].rearrange("(c p) t -> p c t", p=128), reads=[AT], writes=[atT])
    wcs = [ar.alloc(f"wco{i}", [16, 256], BF16) for i in range(2)]
    was = [ar.alloc(f"wao{i}", [16, 256], BF16) for i in range(2)]
    sgc = [ar.alloc(f"sgc{i}", [T], F32) for i in range(2)]
    sga = [ar.alloc(f"sga{i}", [T], F32) for i in range(2)]
    m1 = [ar.alloc(f"m1_{i}", [512], F32) for i in range(2)]
    m2 = [ar.alloc(f"m2_{i}", [512], F32) for i in range(2)]

    def ldD(nb):
        load_w(wcs[nb % 2], w_co, 2048, [(nb * 256, 256)])
        load_w(was[nb % 2], w_ao, 2048, [(nb * 256, 256)])

    ldD(0)
    k = 0
    for nb in range(16):
        if nb + 1 < 16:
            ldD(nb + 1)
        wc, wa = wcs[nb % 2], was[nb % 2]
        for sc in range(2):
            n = nb * 2 + sc
            gc_, ga_ = sgc[n % 2], sga[n % 2]
            dma("sp", gc_[:], SG[0, n * 128:(n + 1) * 128, :], reads=[SG], writes=[gc_])
            dma("sp", ga_[:], SG[1, n * 128:(n + 1) * 128, :], reads=[SG], writes=[ga_])
            for half in range(2):
                pc, pa = ps[(k % 2) * 2], ps[(k % 2) * 2 + 1]
                a1, a2 = m1[k % 2], m2[k % 2]
                k += 1
                hs = slice(half * 512, (half + 1) * 512)
                proj_fm(wc, sc * 128, zcT, 16, half, pc)
                proj_fm(wa, sc * 128, atT, 16, half, pa)
                op("dve", lambda h, a1=a1, pc=pc, gc_=gc_, hs=hs: h.tensor_tensor(out=a1[:], in0=pc[:], in1=gc_[:, hs], op=ALU.mult), [pc, gc_], [a1])
                op("dve", lambda h, a2=a2, pa=pa, ga_=ga_, hs=hs: h.tensor_tensor(out=a2[:], in0=pa[:], in1=ga_[:, hs], op=ALU.mult), [pa, ga_], [a2])
                op("pool", lambda h, a1=a1, a2=a2, n=n, hs=hs: h.tensor_tensor(out=mergedT[:, n, hs], in0=a1[:], in1=a2[:], op=ALU.add), [a1, a2], [mergedT])

    ar.reset(m_mg)
    wbs = [ar.alloc(f"wbO{i}", [32, 512], BF16) for i in range(2)]
    xts = [ar.alloc(f"xtE{i}", [512], F32) for i in range(3)]
    h2s = [ar.alloc(f"h2E{i}", [512], F32) for i in range(3)]

    def ldE(nb):
        load_w(wbs[nb % 2], w_o, D, [(nb * 512, 512)])

    ldE(0)
    k = 0
    for nb in range(8):
        if nb + 1 < 8:
            ldE(nb + 1)
        wb = wbs[nb % 2]
        for tt in range(8):
            pb = ps[k % 4]
            xt, h2 = xts[k % 3], h2s[k % 3]
            k += 1
            dma("sp", xt[:], xe[T + tt * 128:T + (tt + 1) * 128, nb * 512:(nb + 1) * 512], writes=[xt])
            for kc in range(32):
                op("pe", lambda h, pb=pb, kc=kc, tt=tt, wb=wb: h.matmul(pb[:], lhsT=mergedT[:, kc, tt * 128:(tt + 1) * 128], rhs=wb[:, kc, :], start=(kc == 0), stop=(kc == 31)), [mergedT, wb], [pb])
            op("dve", lambda h, pb=pb, xt=xt, h2=h2: h.tensor_tensor(out=h2[:], in0=pb[:], in1=xt[:], op=ALU.add), [pb, xt], [h2])
            dma("sp", H2[tt * 128:(tt + 1) * 128, nb * 512:(nb + 1) * 512], h2[:], reads=[h2], writes=[H2])

    if upto == "E":
        fw.barrier()
        fw.finish()
        return nc, fw
    ar.reset(small_mark)
    skT = ar.alloc("skT", [16, 128], BF16)
    base_mark = ar.mark()
    skf = ar.alloc("skf", [16, 128], F32)
    skb = ar.alloc("skb", [16, 128], BF16)
    dma("sp", skf[:], skeys[:].rearrange("c k d -> k c d"), writes=[skf])
    op("dve", lambda h: h.tensor_copy(out=skb[:], in_=skf[:]), [skf], [skb])
    for b in range(2):
        pv = psbf(b)
        for i in range(8):
            hc = b * 8 + i
            op("pe", lambda h, pv=pv, i=i, hc=hc: h.transpose(out=pv[:, i * 128:(i + 1) * 128], in_=skb[:, hc, :], identity=ident[:]), [skb, ident], [ps[b]])
        op("act", lambda h, pv=pv, b=b: h.activation(out=skT[:, b * 8:(b + 1) * 8, :].rearrange("p a b -> p (a b)"), in_=pv, func=AF.Copy), [ps[b]], [skT])

    GS = 8
    NG = 128 // GS
    for th in range(2):
        ar.reset(base_mark)
        t0 = th * 512
        hn2T = ar.alloc("hn2T", [32, 512], BF16)
        outacc = ar.alloc("outacc", [4, D], F32)
        A1 = ar.alloc("A1", [4, 8, 128], BF16)
        A2 = ar.alloc("A2", [4, 8, 128], BF16)
        rho = ar.alloc("rho", [4, 8], F32)
        m_pe = ar.mark()
        norm_transpose(lambda tt: H2[t0 + tt * 128:t0 + (tt + 1) * 128, :], 1, hn2T, 4, f"p{th}")
        ar.reset(m_pe)
        qT = ar.alloc("qT", [16, 512], BF16)
        m_q = ar.mark()
        wbs = [ar.alloc(f"wbQ{i}", [32, 256], BF16) for i in range(2)]

        def ldQ(cb):
            load_w(wbs[cb % 2], w_pq, D, [(cb * 256, 256)])

        ldQ(0)
        k = 0
        for cb in range(8):
            if cb + 1 < 8:
                ldQ(cb + 1)
            for sc in range(2):
                pb = ps[k % 4]
                k += 1
                proj_fm(wbs[cb % 2], sc * 128, hn2T, 32, 0, pb)
                op("act", lambda h, pb=pb, hc=cb * 2 + sc: h.activation(out=qT[:, hc, :], in_=pb[:], func=AF.Copy), [pb], [qT])
        ar.reset(m_q)
        S = ar.alloc("S", [16, 128], F32)
        t16 = ar.alloc("t16", [16, 16], F32)
        t16b = [Buf(fw, f"t16_{i}", t16.ap[:, i, :]) for i in range(16)]
        wkb = [ar.alloc(f"wk{i}", [128], F32) for i in range(16)]
        cand = ar.alloc("cand", [8, 256], F32)
        c24 = ar.alloc("c24", [8, 24], F32)
        c24b = [Buf(fw, f"c24_{i}", c24.ap[:, i, :]) for i in range(8)]
        wka = [ar.alloc(f"wka{i}", [256], F32) for i in range(8)]
        wkc = [ar.alloc(f"wkc{i}", [256], F32) for i in range(8)]
        sm = ar.alloc("sm", [8, 8], F32)
        ex = ar.alloc("ex", [8, 16], F32)
        for tt in range(4):
            for hc in range(16):
                pb = ps[4 + hc // 4]
                op("pe", lambda h, pb=pb, hc=hc, tt=tt: h.matmul(pb[:, (hc % 4) * 128:(hc % 4 + 1) * 128], lhsT=qT[:, hc, tt * 128:(tt + 1) * 128], rhs=skT[:, hc, :], start=True, stop=True), [qT, skT], [pb])
            for q4 in range(4):
                e = "act" if q4 % 2 == 0 else "dve"
                if e == "act":
                    op("act", lambda h, q4=q4: h.activation(out=S[:, q4 * 4:(q4 + 1) * 4, :].rearrange("p a b -> p (a b)"), in_=ps[4 + q4][:], func=AF.Copy), [ps[4 + q4]], [S])
                else:
                    op("dve", lambda h, q4=q4: h.tensor_copy(out=S[:, q4 * 4:(q4 + 1) * 4, :].rearrange("p a b -> p (a b)"), in_=ps[4 + q4][:]), [ps[4 + q4]], [S])
            for hc in range(16):
                op("dve", lambda h, hc=hc: h.max(out=t16[:, hc, 0:8], in_=S[:, hc, :]), [S], [t16b[hc]])
            for hc in range(16):
                op("dve", lambda h, hc=hc, w=wkb[hc]: h.match_replace(out=w[:], in_to_replace=t16[:, hc, 0:8], in_values=S[:, hc, :], imm_value=-BIG), [S, t16b[hc]], [wkb[hc]])
            for hc in range(16):
                op("dve", lambda h, hc=hc, w=wkb[hc]: h.max(out=t16[:, hc, 8:16], in_=w[:]), [wkb[hc]], [t16b[hc]])
            t16v = t16[:].rearrange("p (h c) k -> p h c k", c=2)
            op("dve", lambda h, t16v=t16v: h.tensor_tensor(out=cand[:].rearrange("p h (i j) -> p h i j", j=16), in0=t16v[:, :, 0, :].unsqueeze(3).to_broadcast([128, 8, 16, 16]), in1=t16v[:, :, 1, :].unsqueeze(2).to_broadcast([128, 8, 16, 16]), op=ALU.add), t16b, [cand])
            for hh in range(8):
                op("dve", lambda h, hh=hh: h.max(out=c24[:, hh, 0:8], in_=cand[:, hh, :]), [cand], [c24b[hh]])
            for hh in range(8):
                op("dve", lambda h, hh=hh, w=wka[hh]: h.match_replace(out=w[:], in_to_replace=c24[:, hh, 0:8], in_values=cand[:, hh, :], imm_value=-BIG), [cand, c24b[hh]], [wka[hh]])
            for hh in range(8):
                op("dve", lambda h, hh=hh, w=wka[hh]: h.max(out=c24[:, hh, 8:16], in_=w[:]), [wka[hh]], [c24b[hh]])
            for hh in range(8):
                op("dve", lambda h, hh=hh, w=wka[hh], w2=wkc[hh]: h.match_replace(out=w2[:], in_to_replace=c24[:, hh, 8:16], in_values=w[:], imm_value=-BIG), [wka[hh], c24b[hh]], [wkc[hh]])
            for hh in range(8):
                op("dve", lambda h, hh=hh, w2=wkc[hh]: h.max(out=c24[:, hh, 16:24], in_=w2[:]), [wkc[hh]], [c24b[hh]])
            op("dve", lambda h: h.tensor_tensor(out=sm[:, :, 0], in0=c24[:, :, 15], in1=c24[:, :, 16], op=ALU.add), c24b, [sm])
            op("dve", lambda h: h.tensor_scalar(out=sm[:, :, 0], in0=sm[:, :, 0], scalar1=0.5, scalar2=None, op0=ALU.mult), [sm], [sm])
            op("dve", lambda h: h.tensor_tensor(out=ex[:], in0=c24[:, :, 0:16], in1=c24[:, :, 0:1].to_broadcast([128, 8, 16]), op=ALU.subtract), c24b, [ex])
            for hh in range(8):
                op("act", lambda h, hh=hh: h.activation(out=ex[:, hh, :], in_=ex[:, hh, :], func=AF.Exp, accum_out=sm[:, hh, 2:3]), [ex], [ex, sm])
            op("act", lambda h: h.activation(out=sm[:, :, 3], in_=sm[:, :, 2], func=AF.Ln), [sm], [sm])
            op("dve", lambda h: h.tensor_tensor(out=sm[:, :, 4], in0=c24[:, :, 0], in1=sm[:, :, 3], op=ALU.add), c24b + [sm], [sm])
            op("dve", lambda h, t16v=t16v: h.tensor_scalar(out=sm[:, :, 5], in0=t16v[:, :, 0, 0], scalar1=-1.0, scalar2=None, op0=ALU.mult), t16b, [sm])
            op("dve", lambda h, t16v=t16v: h.tensor_tensor(out=sm[:, :, 6], in0=t16v[:, :, 0, 0], in1=sm[:, :, 4], op=ALU.subtract), t16b + [sm], [sm])
            op("dve", lambda h: h.tensor_tensor(out=sm[:, :, 7], in0=sm[:, :, 0], in1=sm[:, :, 4], op=ALU.subtract), [sm], [sm])
            op("act", lambda h, tt=tt: h.activation(out=rho[:, tt, :], in_=sm[:, :, 7], func=AF.Exp), [sm], [rho])
            for hh in range(8):
                op("act", lambda h, hh=hh, tt=tt: h.activation(out=A1[:, tt, hh, :], in_=S[:, 2 * hh, :], func=AF.Exp, bias=sm[:, hh, 5:6]), [S, sm], [A1])
                op("act", lambda h, hh=hh, tt=tt: h.activation(out=A2[:, tt, hh, :], in_=S[:, 2 * hh + 1, :], func=AF.Exp, bias=sm[:, hh, 6:7]), [S, sm], [A2])
        ar.reset(m_pe)
        Gaccs = [[ar.alloc(f"Gacc{i}_{tt}", [GS * 128], BF16) for tt in range(4)] for i in range(2)]
        Ebs = [ar.alloc(f"Eb{i}", [GS * 128], F32) for i in range(3)]
        ubs = [ar.alloc(f"ub{i}", [D], BF16) for i in range(2)]
        uTs = [ar.alloc(f"uT{i}", [32, 128], BF16) for i in range(2)]
        WT = ar.alloc("WT", [GS, 512], BF16)
        vbs = [ar.alloc(f"vb{i}", [GS, 512], BF16) for i in range(2)]
        Hg = [ar.alloc(f"Hg{i}", [512], F32) for i in range(2)]

        def ldU(ci):
            dma("pool", ubs[ci % 2][:], u_emb[ci * 128:(ci + 1) * 128, :], writes=[ubs[ci % 2]], max_dma_last_dim=8192, ndesc=256)

        def ldV(idx):
            g_, db = idx // 8, idx % 8
            vb = vbs[idx % 2]
            dma("pool", vb[:], v_emb[g_ * GS * 128:(g_ + 1) * GS * 128, db * 512:(db + 1) * 512].rearrange("(c p) n -> p c n", p=128), writes=[vb], ndesc=128 * GS)

        ldU(0)
        ldV(0)
        vidx = 0
        gcount = [0]
        pend = []

        def g_mask(item):
            g_, tt, hh, E = item
            Gb = Gaccs[g_ % 2][tt]
            rh = rho[:, tt, hh:hh + 1]
            if hh == 0:
                op("dve", lambda h, E=E, Gb=Gb, rh=rh: h.scalar_tensor_tensor(out=Gb[:], in0=E[:], scalar=rh, in1=E[:], op0=ALU.is_ge, op1=ALU.mult), [E, rho], [Gb])
            else:
                op("dve", lambda h, E=E, rh=rh: h.scalar_tensor_tensor(out=E[:], in0=E[:], scalar=rh, in1=E[:], op0=ALU.is_ge, op1=ALU.mult), [E, rho], [E])
                op("pool", lambda h, E=E, Gb=Gb: h.tensor_tensor(out=Gb[:], in0=Gb[:], in1=E[:], op=ALU.add), [Gb, E], [Gb])

        def gbuild(g_, tt, hh):
            k_ = gcount[0]
            gcount[0] += 1
            E = Ebs[k_ % 3]
            a1 = A1[:, tt, hh, g_ * GS:(g_ + 1) * GS].unsqueeze(2).to_broadcast([128, GS, 128])
            a2 = A2[:, tt, hh, :].unsqueeze(1).to_broadcast([128, GS, 128])
            Ev = E[:].rearrange("p (a b) -> p a b", b=128)
            if k_ % 2 == 0:
                op("dve", lambda h, Ev=Ev, a1=a1, a2=a2: h.scalar_tensor_tensor(out=Ev, in0=a2, scalar=1.0, in1=a1, op0=ALU.mult, op1=ALU.mult), [A1, A2], [E])
            else:
                op("pool", lambda h, Ev=Ev, a1=a1, a2=a2: h.tensor_tensor(out=Ev, in0=a1, in1=a2, op=ALU.mult), [A1, A2], [E])
            if pend:
                g_mask(pend.pop())
            pend.append((g_, tt, hh, E))

        def gflush():
            while pend:
                g_mask(pend.pop())

        gitems = [(tt, hh) for tt in range(4) for hh in range(8)]
        for (tt, hh) in gitems:
            gbuild(0, tt, hh)
        gflush()

        def stageT(ci):
            ub, uT = ubs[ci % 2], uTs[ci % 2]
            for b in range(4):
                pbk = (0, 1)[b % 2]
                pv = psbf(pbk)
                for i in range(8):
                    dc = b * 8 + i
                    op("pe", lambda h, pv=pv, i=i, dc=dc, ub=ub: h.transpose(out=pv[:, i * 128:(i + 1) * 128], in_=ub[:, dc * 128:(dc + 1) * 128], identity=ident[:]), [ub, ident], [ps[pbk]])
                op("act", lambda h, pv=pv, b=b, uT=uT: h.activation(out=uT[:, b * 8:(b + 1) * 8, :].rearrange("p a b -> p (a b)"), in_=pv, func=AF.Copy), [ps[pbk]], [uT])

        def stageH(g, ii):
            ci = g * GS + ii
            uT = uTs[ci % 2]
            pg = psbf(2)
            for tt in range(4):
                Gb = Gaccs[g % 2][tt]
                op("pe", lambda h, tt=tt, ii=ii, Gb=Gb, pg=pg: h.transpose(out=pg[:, tt * 128:(tt + 1) * 128], in_=Gb[:, ii * 128:(ii + 1) * 128], identity=ident[:]), [Gb, ident], [ps[2]])
            ph = ps[3 + ci % 2]
            for kc in range(32):
                op("pe", lambda h, ph=ph, kc=kc, uT=uT: h.matmul(ph[:], lhsT=uT[:, kc, :], rhs=hn2T[:, kc, :], start=(kc == 0), stop=(kc == 31)), [uT, hn2T], [ph])
            hg = Hg[ci % 2]
            op("act", lambda h, ph=ph, hg=hg: h.activation(out=hg[:], in_=ph[:], func=AF.Gelu), [ph], [hg])
            op("dve", lambda h, hg=hg, ii=ii, pg=pg: h.tensor_tensor(out=WT[:, ii, :], in0=pg[:, 0:512], in1=hg[:], op=ALU.mult), [ps[2], hg], [WT])

        ldU(1)
        stageT(0)
        for g in range(NG):
            for ii in range(GS):
                ci = g * GS + ii
                if ci + 2 < 128:
                    ldU(ci + 2)
                if g + 1 < NG:
                    for (tt, hh) in gitems[ii * 4:(ii + 1) * 4]:
                        gbuild(g + 1, tt, hh)
                if ci + 1 < 128:
                    stageT(ci + 1)
                stageH(g, ii)
            gflush()
            for db in range(8):
                if vidx + 1 < NG * 8:
                    ldV(vidx + 1)
                vb = vbs[vidx % 2]
                vidx += 1
                for tt in range(4):
                    po = ps[5 + (db * 4 + tt) % 3]
                    for ii in range(GS):
                        op("pe", lambda h, po=po, ii=ii, tt=tt, vb=vb: h.matmul(po[:], lhsT=WT[:, ii, tt * 128:(tt + 1) * 128], rhs=vb[:, ii, :], start=(ii == 0), stop=(ii == GS - 1)), [WT, vb], [po])
                    dst = outacc[:, tt, db * 512:(db + 1) * 512]
                    if g == 0:
                        op("dve", lambda h, po=po, dst=dst: h.tensor_copy(out=dst, in_=po[:]), [po], [outacc])
                    else:
                        op("dve", lambda h, po=po, dst=dst: h.tensor_tensor(out=dst, in0=po[:], in1=dst, op=ALU.add), [po, outacc], [outacc])
        ar.reset(m_pe)
        gfin = ar.alloc("gfin", [D], F32)
        dma("sp", gfin[:], gvec[2:3, :].partition_broadcast(128), writes=[gfin])
        h2t = [ar.alloc(f"h2t{i}", [D], F32) for i in range(2)]
        jk = ar.alloc("jk", [D], BF16)
        stf = [ar.alloc(f"stf{i}", [4], F32) for i in range(2)]
        for tt in range(4):
            hx, s = h2t[tt % 2], stf[tt % 2]
            dma("sp", hx[:], H2[t0 + tt * 128:t0 + (tt + 1) * 128, :], reads=[H2], writes=[hx])
            op("dve", lambda h, hx=hx, tt=tt: h.tensor_tensor(out=hx[:], in0=hx[:], in1=outacc[:, tt, :], op=ALU.add), [hx, outacc], [hx])
            op("act", lambda h, hx=hx, s=s: h.activation(out=jk[:], in_=hx[:], func=AF.Square, accum_out=s[:, 0:1]), [hx], [jk, s])
            op("dve", lambda h, s=s: h.tensor_scalar(out=s[:, 1:2], in0=s[:, 0:1], scalar1=1.0 / D, scalar2=EPS, op0=ALU.mult, op1=ALU.add), [s], [s])
            op("act", lambda h, s=s: h.activation(out=s[:, 2:3], in_=s[:, 1:2], func=AF.Sqrt), [s], [s])
            op("dve", lambda h, s=s: h.reciprocal(out=s[:, 3:4], in_=s[:, 2:3]), [s], [s])
            op("dve", lambda h, hx=hx, s=s: h.scalar_tensor_tensor(out=hx[:], in0=hx[:], scalar=s[:, 3:4], in1=gfin[:], op0=ALU.mult, op1=ALU.mult), [hx, s, gfin], [hx])
            dma("sp", out[t0 + tt * 128:t0 + (tt + 1) * 128, :], hx[:], reads=[hx], writes=[out])

    fw.barrier()
    fw.finish()
    return nc, fw


_CACHE = {}


def _consts():
    ident = np.eye(128, dtype=np.float32)
    causal = np.zeros((128, 2, 256), np.float32)
    kk = np.arange(128)[:, None]
    qq = np.arange(256)[None, :]
    for kc in range(2):
        causal[:, kc, :] = np.where(kc * 128 + kk <= qq, 0.0, NEG)
    sel8 = np.zeros((8, 8, 128), np.float32)
    for j in range(8):
        sel8[j, j, :] = 1.0
    return ident, causal.reshape(128, 512), sel8.reshape(8, 1024)


def _past(second_half):
    p = np.full((8, 8), -BIG, np.float32)
    for tt in range(8):
        qb = 4 + tt // 2
        for j in range(8):
            if j < qb and (j >= 4 or second_half):
                p[tt, j] = 0.0
    return np.ascontiguousarray(np.broadcast_to(p.reshape(1, 64), (128, 64)))


def make_in_maps(x, norm_mix, w_in, b_gate, conv_w, w_conv_out, w_attn_out, w_o,
                 norm_ffn, w_peer_q, sub_keys, u_emb, v_emb, norm_final, cores=range(8)):
    f = lambda a: np.ascontiguousarray(np.asarray(a, dtype=np.float32))
    x = f(x)
    ident, causal, sel8 = _consts()
    gvec = f(np.stack([np.asarray(norm_mix)[0], np.asarray(norm_ffn)[0], np.asarray(norm_final)]))
    bg = np.asarray(b_gate, np.float32)[0].reshape(64, 128).T
    cw = np.asarray(conv_w, np.float32)[0].reshape(3, 16, 128).transpose(2, 0, 1).reshape(128, 48)
    cfm = f(np.concatenate([bg, cw], axis=1))
    shared = {
        "w_in": f(w_in[0]), "w_co": f(w_conv_out[0]), "w_ao": f(w_attn_out[0]), "w_o": f(w_o[0]),
        "w_pq": f(w_peer_q[0]), "skeys": f(np.asarray(sub_keys)[0].reshape(16, 128, 128)),
        "u_emb": f(u_emb[0]), "v_emb": f(v_emb[0]), "gvec": gvec, "cfm": cfm,
        "cid": ident, "ccausal": causal, "csel8": sel8,
    }
    maps = []
    for c in cores:
        b, hf = c // 2, c % 2
        xe = np.zeros((2048, D), np.float32)
        if hf == 1:
            xe[:] = x[b]
        else:
            xe[T:] = x[b, :T]
        m = dict(shared)
        m["xe"] = xe
        m["cpast"] = _past(hf == 1)
        maps.append(m)
    return maps


def kernel(**inputs):
    if "nc" not in _CACHE:
        _CACHE["nc"] = build()[0]
    nc = _CACHE["nc"]
    in_maps = make_in_maps(**inputs)
    res = run_bass_kernel_spmd(nc, in_maps, core_ids=list(range(8)))
    outp = np.empty((4, 2048, D), np.float32)
    for c in range(8):
        b, hf = c // 2, c % 2
        outp[b, hf * T:(hf + 1) * T] = res.results[c]["out"]
    return outp
```

```python
import numpy as np
import concourse.bass as bass
import concourse.mybir as mybir
from concourse.bass_utils import run_bass_kernel_spmd

F32 = mybir.dt.float32
BF16 = mybir.dt.bfloat16
U8 = mybir.dt.uint8
AF = mybir.ActivationFunctionType
ALU = mybir.AluOpType
AX = mybir.AxisListType

SEM_LIMIT = 30000
SWDGE_MAX_DESC = 6000
D = 4096
T = 1024
NEG = -60000.0
BIG = 1.0e30
EPS = 1e-6


class SemCtr:
    def __init__(self, nc, name):
        self.nc = nc
        self.name = name
        self.n = 0
        self.sem = nc.alloc_semaphore(name=f"{name}_{self.n}")
        self.val = 0

    def bump(self, k):
        if self.val + k > SEM_LIMIT:
            self.n += 1
            self.sem = self.nc.alloc_semaphore(name=f"{self.name}_{self.n}")
            self.val = 0
        self.val += k
        return (self.sem, self.val)


class Buf:
    _id = 0

    def __init__(self, fw, name, ap):
        self.fw = fw
        self.name = name
        self.ap = ap
        self.w = []
        self.r = []
        self.dsem = None

    def __getitem__(self, idx):
        return self.ap[idx]

    def dma_sem(self):
        if self.dsem is None:
            Buf._id += 1
            self.dsem = self.fw.get_dma_sem()
        return self.dsem


class Eng:
    def __init__(self, fw, name, handle):
        self.name = name
        self.h = handle
        self.ctr = SemCtr(fw.nc, f"e_{name}")
        self.seen = {}
        self.prog = []


class FW:
    def __init__(self, nc):
        self.nc = nc
        self.engs = {
            "pe": Eng(self, "pe", nc.tensor),
            "dve": Eng(self, "dve", nc.vector),
            "act": Eng(self, "act", nc.scalar),
            "pool": Eng(self, "pool", nc.gpsimd),
            "sp": Eng(self, "sp", nc.sync),
        }
        self.dma_sems = []
        self.free_dma_sems = []
        self.n_inst = 0
        self.swq = []

    def get_dma_sem(self):
        if self.free_dma_sems:
            return self.free_dma_sems.pop()
        s = SemCtr(self.nc, f"d{len(self.dma_sems)}")
        self.dma_sems.append(s)
        return s

    def dram(self, name, shape, dtype, kind="Internal"):
        t = self.nc.dram_tensor(name, list(shape), dtype, kind=kind)
        return Buf(self, name, t.ap())

    def _deps(self, eng, reads, writes):
        deps = {}

        def add(tok):
            sem, val = tok
            k = id(sem)
            if k not in deps or deps[k][1] < val:
                deps[k] = (sem, val)

        for b in reads:
            for tok in b.w:
                add(tok)
        for b in writes:
            for tok in b.w:
                add(tok)
            for tok in b.r:
                add(tok)
        out = []
        for k, (sem, val) in deps.items():
            if eng.name == "pe" and sem is eng.ctr.sem:
                continue
            if eng.seen.get(k, 0) >= val:
                continue
            eng.seen[k] = val
            out.append((sem, val))
        return out

    def _record(self, tok, reads, writes):
        for b in writes:
            b.w = [tok]
            b.r = []
        for b in reads:
            b.r = [t for t in b.r if t[0] is not tok[0]] + [tok]

    def op(self, engname, fn, reads=(), writes=()):
        eng = self.engs[engname]
        waits = self._deps(eng, reads, writes)
        tok = eng.ctr.bump(1)
        eng.prog.append((waits, fn, tok))
        self._record(tok, reads, writes)
        self.n_inst += 1
        return tok

    def dma(self, engname, out_ap, in_ap, reads=(), writes=(), **kw):
        eng = self.engs[engname]
        waits = self._deps(eng, reads, writes)
        if engname == "pool":
            nd = kw.pop("ndesc", 4096)
            while self.swq and sum(n for _, n in self.swq) + nd > SWDGE_MAX_DESC:
                (sem, val), _ = self.swq.pop(0)
                if eng.seen.get(id(sem), 0) < val:
                    eng.seen[id(sem)] = val
                    waits.append((sem, val))
        else:
            kw.pop("ndesc", None)
        tok = writes[0].dma_sem().bump(16)
        if engname == "pool":
            self.swq.append((tok, nd))

        def fn(h, out_ap=out_ap, in_ap=in_ap, kw=kw):
            return h.dma_start(out=out_ap, in_=in_ap, **kw)

        eng.prog.append((waits, fn, ("dma", tok)))
        self._record(tok, reads, writes)
        self.n_inst += 1
        return tok

    def barrier(self, bufs=()):
        toks = []
        for e in self.engs.values():
            if e.ctr.val > 0:
                toks.append((e.ctr.sem, e.ctr.val))
        for s in self.dma_sems:
            if s.val > 0:
                toks.append((s.sem, s.val))
        for e in self.engs.values():
            waits = []
            for sem, val in toks:
                k = id(sem)
                if e.seen.get(k, 0) >= val:
                    continue
                e.seen[k] = val
                waits.append((sem, val))
            e.prog.append((waits, None, None))

    def finish(self):
        nc = self.nc
        with nc.Block() as block:
            def replay(eng):
                def body(h):
                    for waits, fn, tok in eng.prog:
                        for sem, val in waits:
                            h.wait_ge(sem, val)
                        if fn is None:
                            continue
                        inst = fn(h)
                        if tok[0] == "dma":
                            inst.then_inc(tok[1][0], 16)
                        else:
                            inst.then_inc(tok[0], 1)
                return body

            block.tensor(replay(self.engs["pe"]))
            block.vector(replay(self.engs["dve"]))
            block.scalar(replay(self.engs["act"]))
            block.gpsimd(replay(self.engs["pool"]))
            block.sync(replay(self.engs["sp"]))


class Arena:
    def __init__(self, fw, nbytes):
        self.fw = fw
        self.t = fw.nc.alloc_sbuf_tensor("arena", [128, nbytes], U8)
        self.size = nbytes
        self.off = 0

    def alloc(self, name, shape, dtype):
        esz = 2 if dtype == BF16 else 4
        n = 1
        for s in shape:
            n *= s
        nb = (n * esz + 63) // 64 * 64
        assert self.off + nb <= self.size, f"arena overflow at {name}: {self.off + nb}"
        ap = self.t[:, self.off:self.off + n * esz].bitcast(dtype)
        self.off += nb
        if len(shape) == 2:
            ap = ap.rearrange("p (a b) -> p a b", b=shape[1])
        elif len(shape) == 3:
            ap = ap.rearrange("p (a b c) -> p a b c", b=shape[1], c=shape[2])
        return Buf(self.fw, name, ap)

    def mark(self):
        return self.off

    def reset(self, mark):
        self.fw.barrier()
        self.off = mark


def build(debug=False, upto=None):
    nc = bass.Bass("TRN2", target_bir_lowering=False)
    fw = FW(nc)
    op, dma = fw.op, fw.dma
    EI = "ExternalInput"
    xe = fw.dram("xe", [2048, D], F32, EI)
    w_in = fw.dram("w_in", [D, 20480], F32, EI)
    w_co = fw.dram("w_co", [2048, D], F32, EI)
    w_ao = fw.dram("w_ao", [2048, D], F32, EI)
    w_o = fw.dram("w_o", [D, D], F32, EI)
    w_pq = fw.dram("w_pq", [D, 2048], F32, EI)
    skeys = fw.dram("skeys", [16, 128, 128], F32, EI)
    u_emb = fw.dram("u_emb", [16384, D], F32, EI)
    v_emb = fw.dram("v_emb", [16384, D], F32, EI)
    gvec = fw.dram("gvec", [3, D], F32, EI)
    cfm = fw.dram("cfm", [128, 64 + 48], F32, EI)
    cid = fw.dram("cid", [128, 128], F32, EI)
    ccausal = fw.dram("ccausal", [128, 512], F32, EI)
    csel8 = fw.dram("csel8", [8, 1024], F32, EI)
    cpast = fw.dram("cpast", [128, 64], F32, EI)
    sk = "ExternalOutput" if debug else "Internal"
    KTc = fw.dram("KTc", [16, 128, 1024], BF16, sk)
    Vc = fw.dram("Vc", [16, 8, 128, 128], BF16, sk)
    SG = fw.dram("SG", [2, D, T], F32, sk)
    ZC = fw.dram("ZC", [2048, T], BF16, sk)
    AT = fw.dram("AT", [2048, T], BF16, sk)
    H2 = fw.dram("H2", [T, D], F32, sk)
    UT = fw.dram("UT", [128, 128, D], BF16, "Internal")
    out = fw.dram("out", [T, D], F32, "ExternalOutput")

    ar = Arena(fw, 206 * 1024)
    ps = []
    for i in range(8):
        t = nc.alloc_psum_tensor(f"ps{i}", [128, 512], F32)
        ps.append(Buf(fw, f"ps{i}", t[:]))

    def psbf(i):
        return ps[i].ap.bitcast(BF16)

    ident_f = ar.alloc("ident_f", [128], F32)
    ident = ar.alloc("ident", [128], BF16)
    ones = ar.alloc("ones", [128], BF16)
    small_mark = ar.mark()
    causal_f = ar.alloc("causal_f", [512], F32)
    causal = ar.alloc("causal", [2, 256], BF16)
    sel8_f = ar.alloc("sel8_f", [1024], F32)
    sel8 = ar.alloc("sel8", [8, 128], BF16)
    past = ar.alloc("past", [8, 8], F32)
    cf = ar.alloc("cf", [112], F32)
    kmsum = ar.alloc("kmsum", [16, 8], F32)
    halo = ar.alloc("halo", [32, 32], BF16)
    dma("sp", ident_f[:], cid[:], writes=[ident_f])
    dma("sp", causal_f[:], ccausal[:], writes=[causal_f])
    dma("sp", sel8_f[0:8], csel8[:], writes=[sel8_f])
    dma("sp", past[:].rearrange("p a b -> p (a b)"), cpast[:], writes=[past])
    dma("sp", cf[:], cfm[:], writes=[cf])
    op("dve", lambda h: h.tensor_copy(out=ident[:], in_=ident_f[:]), [ident_f], [ident])
    op("dve", lambda h: h.memset(ones[:], 1.0), [], [ones])
    op("dve", lambda h: h.tensor_copy(out=causal[:].rearrange("p a b -> p (a b)"), in_=causal_f[:]), [causal_f], [causal])
    op("dve", lambda h: h.tensor_copy(out=sel8[0:8].rearrange("p a b -> p (a b)"), in_=sel8_f[0:8]), [sel8_f], [sel8])
    base_mark = ar.mark()
    if upto == "A0":
        fw.barrier()
        fw.finish()
        return nc, fw

    def bgate(n):
        return cf[:, n:n + 1]

    def convw(k, j):
        return cf[:, 64 + k * 16 + j:64 + k * 16 + j + 1]

    def load_w(dst, src, rows, parts):
        nkc = rows // 128
        off = 0
        for (c0, wd) in parts:
            s = src[0:rows, c0:c0 + wd].rearrange("(kc p) n -> p kc n", p=128)
            dma("pool", dst[:, 0:nkc, off:off + wd], s, writes=[dst], ndesc=128 * nkc)
            off += wd

    def norm_transpose(src_fn, gidx, dstT, ntiles, tag):
        g_bc = ar.alloc(f"gbc{tag}", [D], F32)
        dma("sp", g_bc[:], gvec[gidx:gidx + 1, :].partition_broadcast(128), writes=[g_bc])
        xts = [ar.alloc(f"xt{tag}{i}", [D], F32) for i in range(2)]
        xns = [ar.alloc(f"xn{tag}{i}", [D], BF16) for i in range(2)]
        st = [ar.alloc(f"st{tag}{i}", [4], F32) for i in range(2)]
        for tt in range(ntiles):
            xt, xn, s = xts[tt % 2], xns[tt % 2], st[tt % 2]
            dma("sp", xt[:], src_fn(tt), writes=[xt])
            op("act", lambda h, xt=xt, xn=xn, s=s: h.activation(out=xn[:], in_=xt[:], func=AF.Square, accum_out=s[:, 0:1]), [xt], [xn, s])
            op("dve", lambda h, s=s: h.tensor_scalar(out=s[:, 1:2], in0=s[:, 0:1], scalar1=1.0 / D, scalar2=EPS, op0=ALU.mult, op1=ALU.add), [s], [s])
            op("act", lambda h, s=s: h.activation(out=s[:, 2:3], in_=s[:, 1:2], func=AF.Sqrt), [s], [s])
            op("dve", lambda h, s=s: h.reciprocal(out=s[:, 3:4], in_=s[:, 2:3]), [s], [s])
            op("dve", lambda h, xt=xt, xn=xn, s=s: h.scalar_tensor_tensor(out=xn[:], in0=xt[:], scalar=s[:, 3:4], in1=g_bc[:], op0=ALU.mult, op1=ALU.mult), [xt, s, g_bc], [xn])
            for b in range(4):
                pb = ps[(tt * 4 + b) % 8]
                pv = psbf((tt * 4 + b) % 8)
                for i in range(8):
                    dc = b * 8 + i
                    op("pe", lambda h, pv=pv, xn=xn, i=i, dc=dc: h.transpose(out=pv[:, i * 128:(i + 1) * 128], in_=xn[:, dc * 128:(dc + 1) * 128], identity=ident[:]), [xn, ident], [pb])
                e = "act" if b % 2 == 0 else "dve"
                src = pv.rearrange("p (a b) -> p a b", b=128)
                dst = dstT[:, b * 8:(b + 1) * 8, tt * 128:(tt + 1) * 128]
                if e == "act":
                    op("act", lambda h, src=src, dst=dst: h.activation(out=dst, in_=src, func=AF.Copy), [pb], [dstT])
                else:
                    op("dve", lambda h, src=src, dst=dst: h.tensor_copy(out=dst, in_=src), [pb], [dstT])

    def proj_fm(wb, col0, hnT, nkc, half, pbuf):
        for kc in range(nkc):
            op("pe", lambda h, kc=kc: h.matmul(pbuf[:], lhsT=wb[:, kc, col0:col0 + 128], rhs=hnT[:, kc, half * 512:(half + 1) * 512], start=(kc == 0), stop=(kc == nkc - 1)), [wb, hnT], [pbuf])

    hnT = ar.alloc("hnT", [32, T], BF16)
    m_hn = ar.mark()
    norm_transpose(lambda tt: xe[tt * 128:(tt + 1) * 128, :], 0, hnT, 8, "c")
    op("dve", lambda h: h.memset(halo[:], 0.0), [], [halo])
    op("dve", lambda h: h.tensor_copy(out=halo[:, :, 0:2], in_=hnT[:, :, T - 2:T]), [hnT], [halo])
    if upto == "A":
        fw.barrier()
        fw.finish()
        return nc, fw
    ar.reset(m_hn)

    def kv_transposes(vt, vtk, pbank):
        pv = psbf(pbank)
        for tt in range(8):
            op("pe", lambda h, tt=tt: h.transpose(out=pv[:, tt * 128:(tt + 1) * 128], in_=vt[:, tt * 128:(tt + 1) * 128], identity=ident[:]), [vt, ident], [ps[pbank]])
        op("act", lambda h: h.activation(out=vtk[:].rearrange("p a b -> p (a b)"), in_=pv, func=AF.Copy), [ps[pbank]], [vtk])

    wbs = [ar.alloc(f"wbB{i}", [32, 256], BF16) for i in range(2)]
    kts = [ar.alloc(f"ktB{i}", [T], BF16) for i in range(2)]
    vts = [ar.alloc(f"vtB{i}", [T], BF16) for i in range(2)]
    vtoks = [ar.alloc(f"vtokB{i}", [8, 128], BF16) for i in range(2)]

    def ldB(hd):
        load_w(wbs[hd % 2], w_in, D, [(4 * 2048 + hd * 128, 128), (5 * 2048 + hd * 128, 128)])

    ldB(0)
    pi = 0
    if upto == "B0":
        fw.barrier()
        fw.finish()
        return nc, fw
    for hd in range(16):
        if hd + 1 < 16:
            ldB(hd + 1)
        wb, kt, vt, vtk = wbs[hd % 2], kts[hd % 2], vts[hd % 2], vtoks[hd % 2]
        for part, dst in ((0, kt), (1, vt)):
            for half in range(2):
                pb = ps[pi % 4]
                pi += 1
                proj_fm(wb, part * 128, hnT, 32, half, pb)
                if part == 0:
                    for q_ in range(2):
                        o_ = half * 512 + q_ * 256
                        op("act", lambda h, pb=pb, dst=dst, o_=o_, q_=q_, jj=2 * half + q_, hd=hd: h.activation(out=dst[:, o_:o_ + 256], in_=pb[:, q_ * 256:(q_ + 1) * 256], func=AF.Copy, accum_out=kmsum[:, hd, jj:jj + 1]), [pb], [dst, kmsum])
                else:
                    op("act", lambda h, pb=pb, dst=dst, half=half: h.activation(out=dst[:, half * 512:(half + 1) * 512], in_=pb[:], func=AF.Copy), [pb], [dst])
        if upto == "B1" and hd == 0:
            fw.barrier()
            fw.finish()
            return nc, fw
        dma("sp", KTc[hd], kt[:], reads=[kt], writes=[KTc])
        if upto == "B2" and hd == 0:
            fw.barrier()
            fw.finish()
            return nc, fw
        kv_transposes(vt, vtk, 4 + hd % 2)
        if upto == "B3" and hd == 0:
            fw.barrier()
            fw.finish()
            return nc, fw
        dma("sp", Vc[hd].rearrange("tt p d -> p tt d"), vtk[:], reads=[vtk], writes=[Vc])

    if upto == "B":
        fw.barrier()
        fw.finish()
        return nc, fw
    ar.reset(m_hn)
    norm_transpose(lambda tt: xe[T + tt * 128:T + (tt + 1) * 128, :], 0, hnT, 8, "o")
    ar.reset(m_hn)

    wbs = [ar.alloc(f"wbG{i}", [32, 512], BF16) for i in range(2)]
    sgt = [ar.alloc(f"sgt{i}", [T], F32) for i in range(4)]

    def ldG(nb):
        load_w(wbs[nb % 2], w_in, D, [(6 * 2048 + nb * 256, 256), (6 * 2048 + 4096 + nb * 256, 256)])

    ldG(0)
    si = 0
    for nb in range(16):
        if nb + 1 < 16:
            ldG(nb + 1)
        wb = wbs[nb % 2]
        for part in range(2):
            for sc in range(2):
                n = nb * 2 + sc
                sg = sgt[si % 4]
                si += 1
                for half in range(2):
                    pb = ps[pi % 4]
                    pi += 1
                    proj_fm(wb, part * 256 + sc * 128, hnT, 32, half, pb)
                    op("act", lambda h, pb=pb, sg=sg, half=half, bn=part * 32 + n: h.activation(out=sg[:, half * 512:(half + 1) * 512], in_=pb[:], func=AF.Sigmoid, bias=bgate(bn)), [pb, cf], [sg])
                dma("sp", SG[part, n * 128:(n + 1) * 128, :], sg[:], reads=[sg], writes=[SG])

    if upto == "C2":
        fw.barrier()
        fw.finish()
        return nc, fw
    ar.reset(m_hn)
    wbs = [ar.alloc(f"wbC{i}", [32, 384], BF16) for i in range(2)]
    zb = [ar.alloc(f"z{i}", [T + 2], F32) for i in range(2)]
    zct = [ar.alloc(f"zct{i}", [T], BF16) for i in range(2)]
    usb = [ar.alloc(f"usb{i}", [512], F32) for i in range(2)]
    tmpc = [ar.alloc(f"tmpc{i}", [512], F32) for i in range(2)]
    uh = ar.alloc("uh", [2], F32)

    def ldC(j):
        load_w(wbs[j % 2], w_in, D, [(j * 128, 128), (2048 + j * 128, 128), (4096 + j * 128, 128)])

    ldC(0)
    for j in range(16):
        if j + 1 < 16:
            ldC(j + 1)
        wb, z, zc = wbs[j % 2], zb[j % 2], zct[j % 2]
        for pidx, c0 in ((0, 128), (1, 256)):
            for kc in range(32):
                op("pe", lambda h, kc=kc, c0=c0, pidx=pidx, wb=wb: h.matmul(ps[6 + pidx][:, 0:32], lhsT=wb[:, kc, c0:c0 + 128], rhs=halo[:, kc, :], start=(kc == 0), stop=(kc == 31)), [wb, halo], [ps[6 + pidx]])
        for half in range(2):
            pB, pC, pU = ps[half * 3], ps[half * 3 + 1], ps[half * 3 + 2]
            proj_fm(wb, 0, hnT, 32, half, pB)
            proj_fm(wb, 128, hnT, 32, half, pC)
            proj_fm(wb, 256, hnT, 32, half, pU)
            us, tm = usb[half], tmpc[half]
            o = half * 512
            if half == 0:
                op("act", lambda h: h.activation(out=uh[:], in_=ps[7][:, 0:2], func=AF.Copy), [ps[7], pU], [uh])
            op("act", lambda h, us=us, pU=pU: h.activation(out=us[:], in_=pU[:], func=AF.Copy), [pU], [us])
            op("dve", lambda h, z=z, o=o, pC=pC, us=us: h.tensor_tensor(out=z[:, 2 + o:2 + o + 512], in0=pC[:], in1=us[:], op=ALU.mult), [pC, us], [z])
            if half == 0:
                op("dve", lambda h, z=z: h.tensor_tensor(out=z[:, 0:2], in0=ps[6][:, 0:2], in1=uh[:], op=ALU.mult), [ps[6], uh], [z])
            op("dve", lambda h, z=z, o=o, tm=tm, j=j: h.tensor_scalar(out=tm[:], in0=z[:, 2 + o:2 + o + 512], scalar1=convw(2, j), scalar2=None, op0=ALU.mult), [z, cf], [tm])
            op("dve", lambda h, z=z, o=o, tm=tm, j=j: h.scalar_tensor_tensor(out=tm[:], in0=z[:, 1 + o:1 + o + 512], scalar=convw(1, j), in1=tm[:], op0=ALU.mult, op1=ALU.add), [z, cf, tm], [tm])
            op("dve", lambda h, z=z, o=o, tm=tm, j=j: h.scalar_tensor_tensor(out=tm[:], in0=z[:, o:o + 512], scalar=convw(0, j), in1=tm[:], op0=ALU.mult, op1=ALU.add), [z, cf, tm], [tm])
            op("dve", lambda h, zc=zc, o=o, tm=tm, pB=pB: h.tensor_tensor(out=zc[:, o:o + 512], in0=pB[:], in1=tm[:], op=ALU.mult), [pB, tm], [zc])
        dma("sp", ZC[j * 128:(j + 1) * 128, :], zc[:], reads=[zc], writes=[ZC])

    if upto == "C3":
        fw.barrier()
        fw.finish()
        return nc, fw
    ar.reset(m_hn)
    wbs = [ar.alloc(f"wbH{i}", [32, 384], BF16) for i in range(2)]
    ktcs = [ar.alloc(f"ktc{i}", [T], BF16) for i in range(2)]
    vcs = [ar.alloc(f"vc{i}", [8, 128], BF16) for i in range(2)]
    QTs = [ar.alloc(f"QT{i}", [T], BF16) for i in range(2)]
    KTs = [ar.alloc(f"KT{i}", [T], BF16) for i in range(2)]
    VTs = [ar.alloc(f"VT{i}", [T], BF16) for i in range(2)]
    vos = [ar.alloc(f"vo{i}", [8, 128], BF16) for i in range(2)]
    negTs = [ar.alloc(f"negT{i}", [T], BF16) for i in range(2)]
    kmb = ar.alloc("kmb", [8], BF16)
    gsb = ar.alloc("gsb", [8, 8], F32)
    g8 = ar.alloc("g8", [8, 8], F32)
    tg = ar.alloc("tg", [8], F32)
    selm = ar.alloc("selm", [8, 8], F32)
    negm = ar.alloc("negm", [8, 8], BF16)
    pts = [ar.alloc(f"pt{i}", [256], BF16) for i in range(3)]
    rden = ar.alloc("rden", [256], F32)
    att = [ar.alloc(f"att{i}", [T], BF16) for i in range(2)]
    scale = 128.0 ** -0.5

    def ldH(hd):
        load_w(wbs[hd % 2], w_in, D, [(3 * 2048 + hd * 128, 128), (4 * 2048 + hd * 128, 128), (5 * 2048 + hd * 128, 128)])
        dma("sp", ktcs[hd % 2][:], KTc[hd], reads=[KTc], writes=[ktcs[hd % 2]])
        dma("sp", vcs[hd % 2][:], Vc[hd].rearrange("tt p d -> p tt d"), reads=[Vc], writes=[vcs[hd % 2]])

    def stageP(hd):
        wb = wbs[hd % 2]
        QT, KT, VT, vo, negT = QTs[hd % 2], KTs[hd % 2], VTs[hd % 2], vos[hd % 2], negTs[hd % 2]
        for part, dst in ((0, QT), (1, KT), (2, VT)):
            for half in range(2):
                pb = ps[half]
                proj_fm(wb, part * 128, hnT, 32, half, pb)
                if part == 1:
                    for q_ in range(2):
                        o_ = half * 512 + q_ * 256
                        op("act", lambda h, pb=pb, dst=dst, o_=o_, q_=q_, jj=4 + 2 * half + q_, hd=hd: h.activation(out=dst[:, o_:o_ + 256], in_=pb[:, q_ * 256:(q_ + 1) * 256], func=AF.Copy, accum_out=kmsum[:, hd, jj:jj + 1]), [pb], [dst, kmsum])
                else:
                    op("act", lambda h, pb=pb, dst=dst, half=half: h.activation(out=dst[:, half * 512:(half + 1) * 512], in_=pb[:], func=AF.Copy), [pb], [dst])
        kv_transposes(VT, vo, 2)
        op("dve", lambda h, hd=hd: h.tensor_scalar(out=kmb[:], in0=kmsum[:, hd, :], scalar1=1.0 / 256.0, scalar2=None, op0=ALU.mult), [kmsum], [kmb])
        for tt in range(8):
            op("pe", lambda h, tt=tt, QT=QT: h.matmul(ps[2][:, tt * 8:(tt + 1) * 8], lhsT=QT[:, tt * 128:(tt + 1) * 128], rhs=kmb[:], start=True, stop=True), [QT, kmb], [ps[2]])
        op("dve", lambda h: h.tensor_tensor(out=gsb[:].rearrange("p a b -> p (a b)"), in0=ps[2][:, 0:64], in1=past[:].rearrange("p a b -> p (a b)"), op=ALU.add), [ps[2], past], [gsb])
        for tt in range(8):
            op("dve", lambda h, tt=tt: h.max(out=g8[:, tt, :], in_=gsb[:, tt, :]), [gsb], [g8])
        op("dve", lambda h: h.tensor_tensor(out=tg[:], in0=g8[:, :, 2], in1=g8[:, :, 3], op=ALU.add), [g8], [tg])
        op("dve", lambda h: h.tensor_scalar(out=tg[:], in0=tg[:], scalar1=0.5, scalar2=-0.9 * BIG, op0=ALU.mult, op1=ALU.max), [tg], [tg])
        op("dve", lambda h: h.tensor_tensor(out=selm[:], in0=gsb[:], in1=tg[:].unsqueeze(2).to_broadcast([128, 8, 8]), op=ALU.is_gt), [gsb, tg], [selm])
        op("dve", lambda h: h.tensor_scalar(out=negm[:], in0=selm[:], scalar1=-1.0, scalar2=-NEG, op0=ALU.add, op1=ALU.mult), [selm], [negm])

    def stageP2(hd):
        negT = negTs[hd % 2]
        pv2 = psbf(2)
        for tt in range(8):
            op("pe", lambda h, tt=tt, pv2=pv2: h.transpose(out=pv2[0:8, tt * 128:(tt + 1) * 128], in_=negm[:, tt, :], identity=ident[:]), [negm, ident], [ps[2]])
        op("act", lambda h, pv2=pv2, negT=negT: h.activation(out=negT[0:8, :], in_=pv2[0:8, :], func=AF.Copy), [ps[2]], [negT])

    sidx = [0]

    def stageA(hd):
        ktc, vc, at = ktcs[hd % 2], vcs[hd % 2], att[hd % 2]
        QT, KT, vo, negT = QTs[hd % 2], KTs[hd % 2], vos[hd % 2], negTs[hd % 2]
        for qb in range(4):
            qs = slice(qb * 256, (qb + 1) * 256)
            blocks = [(j, kc) for j in range(4 + qb + 1) for kc in range(2)]
            for bi, (j, kc) in enumerate(blocks):
                pS = ps[(3, 4, 7)[sidx[0] % 3]]
                pt = pts[sidx[0] % 3]
                sidx[0] += 1
                if j < 4:
                    kap = ktc[:, j * 256 + kc * 128:j * 256 + kc * 128 + 128]
                    vap = vc[:, 2 * j + kc, :]
                    kbuf, vbuf = ktc, vc
                else:
                    kap = KT[:, (j - 4) * 256 + kc * 128:(j - 4) * 256 + kc * 128 + 128]
                    vap = vo[:, 2 * (j - 4) + kc, :]
                    kbuf, vbuf = KT, vo
                op("pe", lambda h, pS=pS, kap=kap, qs=qs, QT=QT: h.matmul(pS[:, 0:256], lhsT=kap, rhs=QT[:, qs], start=True, stop=False), [kbuf, QT], [pS])
                if j == 4 + qb:
                    op("pe", lambda h, pS=pS, kc=kc: h.matmul(pS[:, 0:256], lhsT=ident[:], rhs=causal[:, kc, :], start=False, stop=True), [ident, causal], [pS])
                else:
                    op("pe", lambda h, pS=pS, j=j, qs=qs, negT=negT: h.matmul(pS[:, 0:256], lhsT=sel8[0:8, j, :], rhs=negT[0:8, qs], start=False, stop=True), [sel8, negT], [pS])
                op("act", lambda h, pS=pS, pt=pt: h.activation(out=pt[:], in_=pS[:, 0:256], func=AF.Exp, scale=scale), [pS], [pt])
                first, last = bi == 0, bi == len(blocks) - 1
                op("pe", lambda h, vap=vap, pt=pt, first=first, last=last: h.matmul(ps[5][:, 0:256], lhsT=vap, rhs=pt[:], start=first, stop=last), [vbuf, pt], [ps[5]])
                op("pe", lambda h, pt=pt, first=first, last=last: h.matmul(ps[6][:, 0:256], lhsT=ones[:], rhs=pt[:], start=first, stop=last), [ones, pt], [ps[6]])
            op("dve", lambda h: h.reciprocal(out=rden[:], in_=ps[6][:, 0:256]), [ps[6]], [rden])
            op("dve", lambda h, at=at, qs=qs: h.tensor_tensor(out=at[:, qs], in0=ps[5][:, 0:256], in1=rden[:], op=ALU.mult), [ps[5], rden], [at])
        dma("sp", AT[hd * 128:(hd + 1) * 128, :], at[:], reads=[at], writes=[AT])

    ldH(0)
    stageP(0)
    stageP2(0)
    for hd in range(16):
        if hd + 1 < 16:
            ldH(hd + 1)
            stageP(hd + 1)
        stageA(hd)
        if hd + 1 < 16:
            stageP2(hd + 1)

    if upto == "C4":
        fw.barrier()
        fw.finish()
        return nc, fw
    ar.reset(base_mark)
    mergedT = ar.alloc("mergedT", [32, T], BF16)
    m_mg = ar.mark()
    zcT = ar.alloc("zcT", [16, T], BF16)
    atT = ar.alloc("atT", [16, T], BF16)
    dma("sp", zcT[:], ZC[:].rearrange("(c p) t -> p c t", p=128), reads=[ZC], writes=[zcT])
    dma("sp", atT[:], AT[:].rearrange("(c p) t -> p c t", p=128), reads=[AT], writes=[atT])
    wcs = [ar.alloc(f"wco{i}", [16, 256], BF16) for i in range(2)]
    was = [ar.alloc(f"wao{i}", [16, 256], BF16) for i in range(2)]
    sgc = [ar.alloc(f"sgc{i}", [T], F32) for i in range(2)]
    sga = [ar.alloc(f"sga{i}", [T], F32) for i in range(2)]
    m1 = [ar.alloc(f"m1_{i}", [512], F32) for i in range(2)]
    m2 = [ar.alloc(f"m2_{i}", [512], F32) for i in range(2)]

    def ldD(nb):
        load_w(wcs[nb % 2], w_co, 2048, [(nb * 256, 256)])
        load_w(was[nb % 2], w_ao, 2048, [(nb * 256, 256)])

    ldD(0)
    k = 0
    for nb in range(16):
        if nb + 1 < 16:
            ldD(nb + 1)
        wc, wa = wcs[nb % 2], was[nb % 2]
        for sc in range(2):
            n = nb * 2 + sc
            gc_, ga_ = sgc[n % 2], sga[n % 2]
            dma("sp", gc_[:], SG[0, n * 128:(n + 1) * 128, :], reads=[SG], writes=[gc_])
            dma("sp", ga_[:], SG[1, n * 128:(n + 1) * 128, :], reads=[SG], writes=[ga_])
            for half in range(2):
                pc, pa = ps[(k % 2) * 2], ps[(k % 2) * 2 + 1]
                a1, a2 = m1[k % 2], m2[k % 2]
                k += 1
                hs = slice(half * 512, (half + 1) * 512)
                proj_fm(wc, sc * 128, zcT, 16, half, pc)
                proj_fm(wa, sc * 128, atT, 16, half, pa)
                op("dve", lambda h, a1=a1, pc=pc, gc_=gc_, hs=hs: h.tensor_tensor(out=a1[:], in0=pc[:], in1=gc_[:, hs], op=ALU.mult), [pc, gc_], [a1])
                op("dve", lambda h, a2=a2, pa=pa, ga_=ga_, hs=hs: h.tensor_tensor(out=a2[:], in0=pa[:], in1=ga_[:, hs], op=ALU.mult), [pa, ga_], [a2])
                op("pool", lambda h, a1=a1, a2=a2, n=n, hs=hs: h.tensor_tensor(out=mergedT[:, n, hs], in0=a1[:], in1=a2[:], op=ALU.add), [a1, a2], [mergedT])

    ar.reset(m_mg)
    wbs = [ar.alloc(f"wbO{i}", [32, 512], BF16) for i in range(2)]
    xts = [ar.alloc(f"xtE{i}", [512], F32) for i in range(3)]
    h2s = [ar.alloc(f"h2E{i}", [512], F32) for i in range(3)]

    def ldE(nb):
        load_w(wbs[nb % 2], w_o, D, [(nb * 512, 512)])

    ldE(0)
    k = 0
    for nb in range(8):
        if nb + 1 < 8:
            ldE(nb + 1)
        wb = wbs[nb % 2]
        for tt in range(8):
            pb = ps[k % 4]
            xt, h2 = xts[k % 3], h2s[k % 3]
            k += 1
            dma("sp", xt[:], xe[T + tt * 128:T + (tt + 1) * 128, nb * 512:(nb + 1) * 512], writes=[xt])
            for kc in range(32):
                op("pe", lambda h, pb=pb, kc=kc, tt=tt, wb=wb: h.matmul(pb[:], lhsT=mergedT[:, kc, tt * 128:(tt + 1) * 128], rhs=wb[:, kc, :], start=(kc == 0), stop=(kc == 31)), [mergedT, wb], [pb])
            op("dve", lambda h, pb=pb, xt=xt, h2=h2: h.tensor_tensor(out=h2[:], in0=pb[:], in1=xt[:], op=ALU.add), [pb, xt], [h2])
            dma("sp", H2[tt * 128:(tt + 1) * 128, nb * 512:(nb + 1) * 512], h2[:], reads=[h2], writes=[H2])

    if upto == "E":
        fw.barrier()
        fw.finish()
        return nc, fw
    ar.reset(small_mark)
    skT = ar.alloc("skT", [16, 128], BF16)
    base_mark = ar.mark()
    skf = ar.alloc("skf", [16, 128], F32)
    skb = ar.alloc("skb", [16, 128], BF16)
    dma("sp", skf[:], skeys[:].rearrange("c k d -> k c d"), writes=[skf])
    op("dve", lambda h: h.tensor_copy(out=skb[:], in_=skf[:]), [skf], [skb])
    for b in range(2):
        pv = psbf(b)
        for i in range(8):
            hc = b * 8 + i
            op("pe", lambda h, pv=pv, i=i, hc=hc: h.transpose(out=pv[:, i * 128:(i + 1) * 128], in_=skb[:, hc, :], identity=ident[:]), [skb, ident], [ps[b]])
        op("act", lambda h, pv=pv, b=b: h.activation(out=skT[:, b * 8:(b + 1) * 8, :].rearrange("p a b -> p (a b)"), in_=pv, func=AF.Copy), [ps[b]], [skT])

    GS = 8
    NG = 128 // GS
    for th in range(2):
        ar.reset(base_mark)
        t0 = th * 512
        hn2T = ar.alloc("hn2T", [32, 512], BF16)
        outacc = ar.alloc("outacc", [4, D], F32)
        A1 = ar.alloc("A1", [4, 8, 128], BF16)
        A2 = ar.alloc("A2", [4, 8, 128], BF16)
        rho = ar.alloc("rho", [4, 8], F32)
        m_pe = ar.mark()
        norm_transpose(lambda tt: H2[t0 + tt * 128:t0 + (tt + 1) * 128, :], 1, hn2T, 4, f"p{th}")
        ar.reset(m_pe)
        qT = ar.alloc("qT", [16, 512], BF16)
        m_q = ar.mark()
        wbs = [ar.alloc(f"wbQ{i}", [32, 256], BF16) for i in range(2)]

        def ldQ(cb):
            load_w(wbs[cb % 2], w_pq, D, [(cb * 256, 256)])

        ldQ(0)
        k = 0
        for cb in range(8):
            if cb + 1 < 8:
                ldQ(cb + 1)
            for sc in range(2):
                pb = ps[k % 4]
                k += 1
                proj_fm(wbs[cb % 2], sc * 128, hn2T, 32, 0, pb)
                op("act", lambda h, pb=pb, hc=cb * 2 + sc: h.activation(out=qT[:, hc, :], in_=pb[:], func=AF.Copy), [pb], [qT])
        ar.reset(m_q)
        S = ar.alloc("S", [16, 128], F32)
        t16 = ar.alloc("t16", [16, 16], F32)
        t16b = [Buf(fw, f"t16_{i}", t16.ap[:, i, :]) for i in range(16)]
        wkb = [ar.alloc(f"wk{i}", [128], F32) for i in range(16)]
        cand = ar.alloc("cand", [8, 256], F32)
        c24 = ar.alloc("c24", [8, 24], F32)
        c24b = [Buf(fw, f"c24_{i}", c24.ap[:, i, :]) for i in range(8)]
        wka = [ar.alloc(f"wka{i}", [256], F32) for i in range(8)]
        wkc = [ar.alloc(f"wkc{i}", [256], F32) for i in range(8)]
        sm = ar.alloc("sm", [8, 8], F32)
        ex = ar.alloc("ex", [8, 16], F32)
        for tt in range(4):
            for hc in range(16):
                pb = ps[4 + hc // 4]
                op("pe", lambda h, pb=pb, hc=hc, tt=tt: h.matmul(pb[:, (hc % 4) * 128:(hc % 4 + 1) * 128], lhsT=qT[:, hc, tt * 128:(tt + 1) * 128], rhs=skT[:, hc, :], start=True, stop=True), [qT, skT], [pb])
            for q4 in range(4):
                e = "act" if q4 % 2 == 0 else "dve"
                if e == "act":
                    op("act", lambda h, q4=q4: h.activation(out=S[:, q4 * 4:(q4 + 1) * 4, :].rearrange("p a b -> p (a b)"), in_=ps[4 + q4][:], func=AF.Copy), [ps[4 + q4]], [S])
                else:
                    op("dve", lambda h, q4=q4: h.tensor_copy(out=S[:, q4 * 4:(q4 + 1) * 4, :].rearrange("p a b -> p (a b)"), in_=ps[4 + q4][:]), [ps[4 + q4]], [S])
            for hc in range(16):
                op("dve", lambda h, hc=hc: h.max(out=t16[:, hc, 0:8], in_=S[:, hc, :]), [S], [t16b[hc]])
            for hc in range(16):
                op("dve", lambda h, hc=hc, w=wkb[hc]: h.match_replace(out=w[:], in_to_replace=t16[:, hc, 0:8], in_values=S[:, hc, :], imm_value=-BIG), [S, t16b[hc]], [wkb[hc]])
            for hc in range(16):
                op("dve", lambda h, hc=hc, w=wkb[hc]: h.max(out=t16[:, hc, 8:16], in_=w[:]), [wkb[hc]], [t16b[hc]])
            t16v = t16[:].rearrange("p (h c) k -> p h c k", c=2)
            op("dve", lambda h, t16v=t16v: h.tensor_tensor(out=cand[:].rearrange("p h (i j) -> p h i j", j=16), in0=t16v[:, :, 0, :].unsqueeze(3).to_broadcast([128, 8, 16, 16]), in1=t16v[:, :, 1, :].unsqueeze(2).to_broadcast([128, 8, 16, 16]), op=ALU.add), t16b, [cand])
            for hh in range(8):
                op("dve", lambda h, hh=hh: h.max(out=c24[:, hh, 0:8], in_=cand[:, hh, :]), [cand], [c24b[hh]])
            for hh in range(8):
                op("dve", lambda h, hh=hh, w=wka[hh]: h.match_replace(out=w[:], in_to_replace=c24[:, hh, 0:8], in_values=cand[:, hh, :], imm_value=-BIG), [cand, c24b[hh]], [wka[hh]])
            for hh in range(8):
                op("dve", lambda h, hh=hh, w=wka[hh]: h.max(out=c24[:, hh, 8:16], in_=w[:]), [wka[hh]], [c24b[hh]])
            for hh in range(8):
                op("dve", lambda h, hh=hh, w=wka[hh], w2=wkc[hh]: h.match_replace(out=w2[:], in_to_replace=c24[:, hh, 8:16], in_values=w[:], imm_value=-BIG), [wka[hh], c24b[hh]], [wkc[hh]])
            for hh in range(8):
                op("dve", lambda h, hh=hh, w2=wkc[hh]: h.max(out=c24[:, hh, 16:24], in_=w2[:]), [wkc[hh]], [c24b[hh]])
            op("dve", lambda h: h.tensor_tensor(out=sm[:, :, 0], in0=c24[:, :, 15], in1=c24[:, :, 16], op=ALU.add), c24b, [sm])
            op("dve", lambda h: h.tensor_scalar(out=sm[:, :, 0], in0=sm[:, :, 0], scalar1=0.5, scalar2=None, op0=ALU.mult), [sm], [sm])
            op("dve", lambda h: h.tensor_tensor(out=ex[:], in0=c24[:, :, 0:16], in1=c24[:, :, 0:1].to_broadcast([128, 8, 16]), op=ALU.subtract), c24b, [ex])
            for hh in range(8):
                op("act", lambda h, hh=hh: h.activation(out=ex[:, hh, :], in_=ex[:, hh, :], func=AF.Exp, accum_out=sm[:, hh, 2:3]), [ex], [ex, sm])
            op("act", lambda h: h.activation(out=sm[:, :, 3], in_=sm[:, :, 2], func=AF.Ln), [sm], [sm])
            op("dve", lambda h: h.tensor_tensor(out=sm[:, :, 4], in0=c24[:, :, 0], in1=sm[:, :, 3], op=ALU.add), c24b + [sm], [sm])
            op("dve", lambda h, t16v=t16v: h.tensor_scalar(out=sm[:, :, 5], in0=t16v[:, :, 0, 0], scalar1=-1.0, scalar2=None, op0=ALU.mult), t16b, [sm])
            op("dve", lambda h, t16v=t16v: h.tensor_tensor(out=sm[:, :, 6], in0=t16v[:, :, 0, 0], in1=sm[:, :, 4], op=ALU.subtract), t16b + [sm], [sm])
            op("dve", lambda h: h.tensor_tensor(out=sm[:, :, 7], in0=sm[:, :, 0], in1=sm[:, :, 4], op=ALU.subtract), [sm], [sm])
            op("act", lambda h, tt=tt: h.activation(out=rho[:, tt, :], in_=sm[:, :, 7], func=AF.Exp), [sm], [rho])
            for hh in range(8):
                op("act", lambda h, hh=hh, tt=tt: h.activation(out=A1[:, tt, hh, :], in_=S[:, 2 * hh, :], func=AF.Exp, bias=sm[:, hh, 5:6]), [S, sm], [A1])
                op("act", lambda h, hh=hh, tt=tt: h.activation(out=A2[:, tt, hh, :], in_=S[:, 2 * hh + 1, :], func=AF.Exp, bias=sm[:, hh, 6:7]), [S, sm], [A2])
        ar.reset(m_pe)
        Gaccs = [[ar.alloc(f"Gacc{i}_{tt}", [GS * 128], BF16) for tt in range(4)] for i in range(2)]
        Ebs = [ar.alloc(f"Eb{i}", [GS * 128], F32) for i in range(3)]
        ubs = [ar.alloc(f"ub{i}", [D], BF16) for i in range(2)]
        uTs = [ar.alloc(f"uT{i}", [32, 128], BF16) for i in range(2)]
        WT = ar.alloc("WT", [GS, 512], BF16)
        vbs = [ar.alloc(f"vb{i}", [GS, 512], BF16) for i in range(2)]
        Hg = [ar.alloc(f"Hg{i}", [512], F32) for i in range(2)]

        def ldU(ci):
            if th == 1:
                return
            dma("pool", ubs[ci % 2][:], u_emb[ci * 128:(ci + 1) * 128, :], writes=[ubs[ci % 2]], max_dma_last_dim=8192, ndesc=256)

        def ldV(idx):
            g_, db = idx // 8, idx % 8
            vb = vbs[idx % 2]
            dma("pool", vb[:], v_emb[g_ * GS * 128:(g_ + 1) * GS * 128, db * 512:(db + 1) * 512].rearrange("(c p) n -> p c n", p=128), writes=[vb], ndesc=128 * GS)

        ldU(0)
        ldV(0)
        vidx = 0
        gcount = [0]
        pend = []

        def g_mask(item):
            g_, tt, hh, E = item
            Gb = Gaccs[g_ % 2][tt]
            rh = rho[:, tt, hh:hh + 1]
            if hh == 0:
                op("dve", lambda h, E=E, Gb=Gb, rh=rh: h.scalar_tensor_tensor(out=Gb[:], in0=E[:], scalar=rh, in1=E[:], op0=ALU.is_ge, op1=ALU.mult), [E, rho], [Gb])
            else:
                op("dve", lambda h, E=E, rh=rh: h.scalar_tensor_tensor(out=E[:], in0=E[:], scalar=rh, in1=E[:], op0=ALU.is_ge, op1=ALU.mult), [E, rho], [E])
                op("pool", lambda h, E=E, Gb=Gb: h.tensor_tensor(out=Gb[:], in0=Gb[:], in1=E[:], op=ALU.add), [Gb, E], [Gb])

        def gbuild(g_, tt, hh):
            k_ = gcount[0]
            gcount[0] += 1
            E = Ebs[k_ % 3]
            a1 = A1[:, tt, hh, g_ * GS:(g_ + 1) * GS].unsqueeze(2).to_broadcast([128, GS, 128])
            a2 = A2[:, tt, hh, :].unsqueeze(1).to_broadcast([128, GS, 128])
            Ev = E[:].rearrange("p (a b) -> p a b", b=128)
            if k_ % 2 == 0:
                op("dve", lambda h, Ev=Ev, a1=a1, a2=a2: h.scalar_tensor_tensor(out=Ev, in0=a2, scalar=1.0, in1=a1, op0=ALU.mult, op1=ALU.mult), [A1, A2], [E])
            else:
                op("pool", lambda h, Ev=Ev, a1=a1, a2=a2: h.tensor_tensor(out=Ev, in0=a1, in1=a2, op=ALU.mult), [A1, A2], [E])
            if pend:
                g_mask(pend.pop())
            pend.append((g_, tt, hh, E))

        def gflush():
            while pend:
                g_mask(pend.pop())

        gitems = [(tt, hh) for tt in range(4) for hh in range(8)]
        for (tt, hh) in gitems:
            gbuild(0, tt, hh)
        gflush()

        def stageT(ci):
            ub, uT = ubs[ci % 2], uTs[ci % 2]
            if th == 1:
                dma("sp", uT[:].rearrange("p a b -> p (a b)"), UT[ci], reads=[UT], writes=[uT])
                return
            for b in range(4):
                pbk = (0, 1)[b % 2]
                pv = psbf(pbk)
                for i in range(8):
                    dc = b * 8 + i
                    op("pe", lambda h, pv=pv, i=i, dc=dc, ub=ub: h.transpose(out=pv[:, i * 128:(i + 1) * 128], in_=ub[:, dc * 128:(dc + 1) * 128], identity=ident[:]), [ub, ident], [ps[pbk]])
                op("act", lambda h, pv=pv, b=b, uT=uT: h.activation(out=uT[:, b * 8:(b + 1) * 8, :].rearrange("p a b -> p (a b)"), in_=pv, func=AF.Copy), [ps[pbk]], [uT])
            dma("sp", UT[ci], uT[:].rearrange("p a b -> p (a b)"), reads=[uT], writes=[UT])

        def stageH(g, ii):
            ci = g * GS + ii
            uT = uTs[ci % 2]
            pg = psbf(2)
            for tt in range(4):
                Gb = Gaccs[g % 2][tt]
                op("pe", lambda h, tt=tt, ii=ii, Gb=Gb, pg=pg: h.transpose(out=pg[:, tt * 128:(tt + 1) * 128], in_=Gb[:, ii * 128:(ii + 1) * 128], identity=ident[:]), [Gb, ident], [ps[2]])
            ph = ps[3 + ci % 2]
            for kc in range(32):
                op("pe", lambda h, ph=ph, kc=kc, uT=uT: h.matmul(ph[:], lhsT=uT[:, kc, :], rhs=hn2T[:, kc, :], start=(kc == 0), stop=(kc == 31)), [uT, hn2T], [ph])
            hg = Hg[ci % 2]
            op("act", lambda h, ph=ph, hg=hg: h.activation(out=hg[:], in_=ph[:], func=AF.Gelu), [ph], [hg])
            op("dve", lambda h, hg=hg, ii=ii, pg=pg: h.tensor_tensor(out=WT[:, ii, :], in0=pg[:, 0:512], in1=hg[:], op=ALU.mult), [ps[2], hg], [WT])

        ldU(1)
        stageT(0)
        for g in range(NG):
            for ii in range(GS):
                ci = g * GS + ii
                if ci + 2 < 128:
                    ldU(ci + 2)
                if g + 1 < NG:
                    for (tt, hh) in gitems[ii * 4:(ii + 1) * 4]:
                        gbuild(g + 1, tt, hh)
                if ci + 1 < 128:
                    stageT(ci + 1)
                stageH(g, ii)
            gflush()
            for db in range(8):
                if vidx + 1 < NG * 8:
                    ldV(vidx + 1)
                vb = vbs[vidx % 2]
                vidx += 1
                for tt in range(4):
                    po = ps[5 + (db * 4 + tt) % 3]
                    for ii in range(GS):
                        op("pe", lambda h, po=po, ii=ii, tt=tt, vb=vb: h.matmul(po[:], lhsT=WT[:, ii, tt * 128:(tt + 1) * 128], rhs=vb[:, ii, :], start=(ii == 0), stop=(ii == GS - 1)), [WT, vb], [po])
                    dst = outacc[:, tt, db * 512:(db + 1) * 512]
                    if g == 0:
                        op("dve", lambda h, po=po, dst=dst: h.tensor_copy(out=dst, in_=po[:]), [po], [outacc])
                    else:
                        op("dve", lambda h, po=po, dst=dst: h.tensor_tensor(out=dst, in0=po[:], in1=dst, op=ALU.add), [po, outacc], [outacc])
        ar.reset(m_pe)
        gfin = ar.alloc("gfin", [D], F32)
        dma("sp", gfin[:], gvec[2:3, :].partition_broadcast(128), writes=[gfin])
        h2t = [ar.alloc(f"h2t{i}", [D], F32) for i in range(2)]
        jk = ar.alloc("jk", [D], BF16)
        stf = [ar.alloc(f"stf{i}", [4], F32) for i in range(2)]
        for tt in range(4):
            hx, s = h2t[tt % 2], stf[tt % 2]
            dma("sp", hx[:], H2[t0 + tt * 128:t0 + (tt + 1) * 128, :], reads=[H2], writes=[hx])
            op("dve", lambda h, hx=hx, tt=tt: h.tensor_tensor(out=hx[:], in0=hx[:], in1=outacc[:, tt, :], op=ALU.add), [hx, outacc], [hx])
            op("act", lambda h, hx=hx, s=s: h.activation(out=jk[:], in_=hx[:], func=AF.Square, accum_out=s[:, 0:1]), [hx], [jk, s])
            op("dve", lambda h, s=s: h.tensor_scalar(out=s[:, 1:2], in0=s[:, 0:1], scalar1=1.0 / D, scalar2=EPS, op0=ALU.mult, op1=ALU.add), [s], [s])
            op("act", lambda h, s=s: h.activation(out=s[:, 2:3], in_=s[:, 1:2], func=AF.Sqrt), [s], [s])
            op("dve", lambda h, s=s: h.reciprocal(out=s[:, 3:4], in_=s[:, 2:3]), [s], [s])
            op("dve", lambda h, hx=hx, s=s: h.scalar_tensor_tensor(out=hx[:], in0=hx[:], scalar=s[:, 3:4], in1=gfin[:], op0=ALU.mult, op1=ALU.mult), [hx, s, gfin], [hx])
            dma("sp", out[t0 + tt * 128:t0 + (tt + 1) * 128, :], hx[:], reads=[hx], writes=[out])

    fw.barrier()
    fw.finish()
    return nc, fw


_CACHE = {}


def _consts():
    ident = np.eye(128, dtype=np.float32)
    causal = np.zeros((128, 2, 256), np.float32)
    kk = np.arange(128)[:, None]
    qq = np.arange(256)[None, :]
    for kc in range(2):
        causal[:, kc, :] = np.where(kc * 128 + kk <= qq, 0.0, NEG)
    sel8 = np.zeros((8, 8, 128), np.float32)
    for j in range(8):
        sel8[j, j, :] = 1.0
    return ident, causal.reshape(128, 512), sel8.reshape(8, 1024)


def _past(second_half):
    p = np.full((8, 8), -BIG, np.float32)
    for tt in range(8):
        qb = 4 + tt // 2
        for j in range(8):
            if j < qb and (j >= 4 or second_half):
                p[tt, j] = 0.0
    return np.ascontiguousarray(np.broadcast_to(p.reshape(1, 64), (128, 64)))


def make_in_maps(x, norm_mix, w_in, b_gate, conv_w, w_conv_out, w_attn_out, w_o,
                 norm_ffn, w_peer_q, sub_keys, u_emb, v_emb, norm_final, cores=range(8)):
    f = lambda a: np.ascontiguousarray(np.asarray(a, dtype=np.float32))
    x = f(x)
    ident, causal, sel8 = _consts()
    gvec = f(np.stack([np.asarray(norm_mix)[0], np.asarray(norm_ffn)[0], np.asarray(norm_final)]))
    bg = np.asarray(b_gate, np.float32)[0].reshape(64, 128).T
    cw = np.asarray(conv_w, np.float32)[0].reshape(3, 16, 128).transpose(2, 0, 1).reshape(128, 48)
    cfm = f(np.concatenate([bg, cw], axis=1))
    shared = {
        "w_in": f(w_in[0]), "w_co": f(w_conv_out[0]), "w_ao": f(w_attn_out[0]), "w_o": f(w_o[0]),
        "w_pq": f(w_peer_q[0]), "skeys": f(np.asarray(sub_keys)[0].reshape(16, 128, 128)),
        "u_emb": f(u_emb[0]), "v_emb": f(v_emb[0]), "gvec": gvec, "cfm": cfm,
        "cid": ident, "ccausal": causal, "csel8": sel8,
    }
    maps = []
    for c in cores:
        b, hf = c // 2, c % 2
        xe = np.zeros((2048, D), np.float32)
        if hf == 1:
            xe[:] = x[b]
        else:
            xe[T:] = x[b, :T]
        m = dict(shared)
        m["xe"] = xe
        m["cpast"] = _past(hf == 1)
        maps.append(m)
    return maps


def kernel(**inputs):
    if "nc" not in _CACHE:
        _CACHE["nc"] = build()[0]
    nc = _CACHE["nc"]
    in_maps = make_in_maps(**inputs)
    res = run_bass_kernel_spmd(nc, in_maps, core_ids=list(range(8)))
    outp = np.empty((4, 2048, D), np.float32)
    for c in range(8):
        b, hf = c // 2, c % 2
        outp[b, hf * T:(hf + 1) * T] = res.results[c]["out"]
    return outp
```

```python
import numpy as np
import concourse.bass as bass
import concourse.mybir as mybir
from concourse.bass_utils import run_bass_kernel_spmd

F32 = mybir.dt.float32
BF16 = mybir.dt.bfloat16
U8 = mybir.dt.uint8
AF = mybir.ActivationFunctionType
ALU = mybir.AluOpType
AX = mybir.AxisListType

SEM_LIMIT = 30000
SWDGE_MAX_DESC = 6000
D = 4096
T = 1024
NEG = -60000.0
BIG = 1.0e30
EPS = 1e-6


class SemCtr:
    def __init__(self, nc, name):
        self.nc = nc
        self.name = name
        self.n = 0
        self.sem = nc.alloc_semaphore(name=f"{name}_{self.n}")
        self.val = 0

    def bump(self, k):
        if self.val + k > SEM_LIMIT:
            self.n += 1
            self.sem = self.nc.alloc_semaphore(name=f"{self.name}_{self.n}")
            self.val = 0
        self.val += k
        return (self.sem, self.val)


class Buf:
    _id = 0

    def __init__(self, fw, name, ap):
        self.fw = fw
        self.name = name
        self.ap = ap
        self.w = []
        self.r = []
        self.dsem = None

    def __getitem__(self, idx):
        return self.ap[idx]

    def dma_sem(self):
        if self.dsem is None:
            Buf._id += 1
            self.dsem = self.fw.get_dma_sem()
        return self.dsem


class Eng:
    def __init__(self, fw, name, handle):
        self.name = name
        self.h = handle
        self.ctr = SemCtr(fw.nc, f"e_{name}")
        self.seen = {}
        self.prog = []


class FW:
    def __init__(self, nc):
        self.nc = nc
        self.engs = {
            "pe": Eng(self, "pe", nc.tensor),
            "dve": Eng(self, "dve", nc.vector),
            "act": Eng(self, "act", nc.scalar),
            "pool": Eng(self, "pool", nc.gpsimd),
            "sp": Eng(self, "sp", nc.sync),
        }
        self.dma_sems = []
        self.free_dma_sems = []
        self.n_inst = 0
        self.swq = []

    def get_dma_sem(self):
        if self.free_dma_sems:
            return self.free_dma_sems.pop()
        s = SemCtr(self.nc, f"d{len(self.dma_sems)}")
        self.dma_sems.append(s)
        return s

    def dram(self, name, shape, dtype, kind="Internal"):
        t = self.nc.dram_tensor(name, list(shape), dtype, kind=kind)
        return Buf(self, name, t.ap())

    def _deps(self, eng, reads, writes):
        deps = {}

        def add(tok):
            sem, val = tok
            k = id(sem)
            if k not in deps or deps[k][1] < val:
                deps[k] = (sem, val)

        for b in reads:
            for tok in b.w:
                add(tok)
        for b in writes:
            for tok in b.w:
                add(tok)
            for tok in b.r:
                add(tok)
        out = []
        for k, (sem, val) in deps.items():
            if eng.name == "pe" and sem is eng.ctr.sem:
                continue
            if eng.seen.get(k, 0) >= val:
                continue
            eng.seen[k] = val
            out.append((sem, val))
        return out

    def _record(self, tok, reads, writes):
        for b in writes:
            b.w = [tok]
            b.r = []
        for b in reads:
            b.r = [t for t in b.r if t[0] is not tok[0]] + [tok]

    def op(self, engname, fn, reads=(), writes=()):
        eng = self.engs[engname]
        waits = self._deps(eng, reads, writes)
        tok = eng.ctr.bump(1)
        eng.prog.append((waits, fn, tok))
        self._record(tok, reads, writes)
        self.n_inst += 1
        return tok

    def dma(self, engname, out_ap, in_ap, reads=(), writes=(), **kw):
        eng = self.engs[engname]
        waits = self._deps(eng, reads, writes)
        if engname == "pool":
            nd = kw.pop("ndesc", 4096)
            while self.swq and sum(n for _, n in self.swq) + nd > SWDGE_MAX_DESC:
                (sem, val), _ = self.swq.pop(0)
                if eng.seen.get(id(sem), 0) < val:
                    eng.seen[id(sem)] = val
                    waits.append((sem, val))
        else:
            kw.pop("ndesc", None)
        tok = writes[0].dma_sem().bump(16)
        if engname == "pool":
            self.swq.append((tok, nd))

        def fn(h, out_ap=out_ap, in_ap=in_ap, kw=kw):
            return h.dma_start(out=out_ap, in_=in_ap, **kw)

        eng.prog.append((waits, fn, ("dma", tok)))
        self._record(tok, reads, writes)
        self.n_inst += 1
        return tok

    def barrier(self, bufs=()):
        toks = []
        for e in self.engs.values():
            if e.ctr.val > 0:
                toks.append((e.ctr.sem, e.ctr.val))
        for s in self.dma_sems:
            if s.val > 0:
                toks.append((s.sem, s.val))
        for e in self.engs.values():
            waits = []
            for sem, val in toks:
                k = id(sem)
                if e.seen.get(k, 0) >= val:
                    continue
                e.seen[k] = val
                waits.append((sem, val))
            e.prog.append((waits, None, None))

    def finish(self):
        nc = self.nc
        with nc.Block() as block:
            def replay(eng):
                def body(h):
                    for waits, fn, tok in eng.prog:
                        for sem, val in waits:
                            h.wait_ge(sem, val)
                        if fn is None:
                            continue
                        inst = fn(h)
                        if tok[0] == "dma":
                            inst.then_inc(tok[1][0], 16)
                        else:
                            inst.then_inc(tok[0], 1)
                return body

            block.tensor(replay(self.engs["pe"]))
            block.vector(replay(self.engs["dve"]))
            block.scalar(replay(self.engs["act"]))
            block.gpsimd(replay(self.engs["pool"]))
            block.sync(replay(self.engs["sp"]))


class Arena:
    def __init__(self, fw, nbytes):
        self.fw = fw
        self.t = fw.nc.alloc_sbuf_tensor("arena", [128, nbytes], U8)
        self.size = nbytes
        self.off = 0

    def alloc(self, name, shape, dtype):
        esz = 2 if dtype == BF16 else 4
        n = 1
        for s in shape:
            n *= s
        nb = (n * esz + 63) // 64 * 64
        assert self.off + nb <= self.size, f"arena overflow at {name}: {self.off + nb}"
        ap = self.t[:, self.off:self.off + n * esz].bitcast(dtype)
        self.off += nb
        if len(shape) == 2:
            ap = ap.rearrange("p (a b) -> p a b", b=shape[1])
        elif len(shape) == 3:
            ap = ap.rearrange("p (a b c) -> p a b c", b=shape[1], c=shape[2])
        return Buf(self.fw, name, ap)

    def mark(self):
        return self.off

    def reset(self, mark):
        self.fw.barrier()
        self.off = mark


def build(debug=False, upto=None):
    nc = bass.Bass("TRN2", target_bir_lowering=False)
    fw = FW(nc)
    op, dma = fw.op, fw.dma
    EI = "ExternalInput"
    xe = fw.dram("xe", [2048, D], F32, EI)
    w_in = fw.dram("w_in", [D, 20480], F32, EI)
    w_co = fw.dram("w_co", [2048, D], F32, EI)
    w_ao = fw.dram("w_ao", [2048, D], F32, EI)
    w_o = fw.dram("w_o", [D, D], F32, EI)
    w_pq = fw.dram("w_pq", [D, 2048], F32, EI)
    skeys = fw.dram("skeys", [16, 128, 128], F32, EI)
    u_emb = fw.dram("u_emb", [16384, D], F32, EI)
    v_emb = fw.dram("v_emb", [16384, D], F32, EI)
    gvec = fw.dram("gvec", [3, D], F32, EI)
    cfm = fw.dram("cfm", [128, 64 + 48], F32, EI)
    cid = fw.dram("cid", [128, 128], F32, EI)
    ccausal = fw.dram("ccausal", [128, 512], F32, EI)
    csel8 = fw.dram("csel8", [8, 1024], F32, EI)
    cpast = fw.dram("cpast", [128, 64], F32, EI)
    sk = "ExternalOutput" if debug else "Internal"
    KTc = fw.dram("KTc", [16, 128, 1024], BF16, sk)
    Vc = fw.dram("Vc", [16, 8, 128, 128], BF16, sk)
    SG = fw.dram("SG", [2, D, T], F32, sk)
    ZC = fw.dram("ZC", [2048, T], BF16, sk)
    AT = fw.dram("AT", [2048, T], BF16, sk)
    H2 = fw.dram("H2", [T, D], F32, sk)
    out = fw.dram("out", [T, D], F32, "ExternalOutput")

    ar = Arena(fw, 212480)
    ps = []
    for i in range(8):
        t = nc.alloc_psum_tensor(f"ps{i}", [128, 512], F32)
        ps.append(Buf(fw, f"ps{i}", t[:]))

    def psbf(i):
        return ps[i].ap.bitcast(BF16)

    ident_f = ar.alloc("ident_f", [128], F32)
    ident = ar.alloc("ident", [128], BF16)
    ones = ar.alloc("ones", [128], BF16)
    small_mark = ar.mark()
    causal_f = ar.alloc("causal_f", [512], F32)
    causal = ar.alloc("causal", [2, 256], BF16)
    sel8_f = ar.alloc("sel8_f", [1024], F32)
    sel8 = ar.alloc("sel8", [8, 128], BF16)
    past = ar.alloc("past", [8, 8], F32)
    cf = ar.alloc("cf", [112], F32)
    kmsum = ar.alloc("kmsum", [16, 8], F32)
    halo = ar.alloc("halo", [32, 32], BF16)
    dma("sp", ident_f[:], cid[:], writes=[ident_f])
    dma("sp", causal_f[:], ccausal[:], writes=[causal_f])
    dma("sp", sel8_f[0:8], csel8[:], writes=[sel8_f])
    dma("sp", past[:].rearrange("p a b -> p (a b)"), cpast[:], writes=[past])
    dma("sp", cf[:], cfm[:], writes=[cf])
    op("dve", lambda h: h.tensor_copy(out=ident[:], in_=ident_f[:]), [ident_f], [ident])
    op("dve", lambda h: h.memset(ones[:], 1.0), [], [ones])
    op("dve", lambda h: h.tensor_copy(out=causal[:].rearrange("p a b -> p (a b)"), in_=causal_f[:]), [causal_f], [causal])
    op("dve", lambda h: h.tensor_copy(out=sel8[0:8].rearrange("p a b -> p (a b)"), in_=sel8_f[0:8]), [sel8_f], [sel8])
    base_mark = ar.mark()
    if upto == "A0":
        fw.barrier()
        fw.finish()
        return nc, fw

    def bgate(n):
        return cf[:, n:n + 1]

    def convw(k, j):
        return cf[:, 64 + k * 16 + j:64 + k * 16 + j + 1]

    def load_w(dst, src, rows, parts):
        nkc = rows // 128
        off = 0
        for (c0, wd) in parts:
            s = src[0:rows, c0:c0 + wd].rearrange("(kc p) n -> p kc n", p=128)
            dma("pool", dst[:, 0:nkc, off:off + wd], s, writes=[dst], ndesc=128 * nkc)
            off += wd

    def norm_transpose(src_fn, gidx, dstT, ntiles, tag):
        g_bc = ar.alloc(f"gbc{tag}", [D], F32)
        dma("sp", g_bc[:], gvec[gidx:gidx + 1, :].partition_broadcast(128), writes=[g_bc])
        xts = [ar.alloc(f"xt{tag}{i}", [D], F32) for i in range(2)]
        xns = [ar.alloc(f"xn{tag}{i}", [D], BF16) for i in range(2)]
        st = [ar.alloc(f"st{tag}{i}", [4], F32) for i in range(2)]
        for tt in range(ntiles):
            xt, xn, s = xts[tt % 2], xns[tt % 2], st[tt % 2]
            dma("sp", xt[:], src_fn(tt), writes=[xt])
            op("act", lambda h, xt=xt, xn=xn, s=s: h.activation(out=xn[:], in_=xt[:], func=AF.Square, accum_out=s[:, 0:1]), [xt], [xn, s])
            op("dve", lambda h, s=s: h.tensor_scalar(out=s[:, 1:2], in0=s[:, 0:1], scalar1=1.0 / D, scalar2=EPS, op0=ALU.mult, op1=ALU.add), [s], [s])
            op("act", lambda h, s=s: h.activation(out=s[:, 2:3], in_=s[:, 1:2], func=AF.Sqrt), [s], [s])
            op("dve", lambda h, s=s: h.reciprocal(out=s[:, 3:4], in_=s[:, 2:3]), [s], [s])
            op("dve", lambda h, xt=xt, xn=xn, s=s: h.scalar_tensor_tensor(out=xn[:], in0=xt[:], scalar=s[:, 3:4], in1=g_bc[:], op0=ALU.mult, op1=ALU.mult), [xt, s, g_bc], [xn])
            for b in range(4):
                pb = ps[(tt * 4 + b) % 8]
                pv = psbf((tt * 4 + b) % 8)
                for i in range(8):
                    dc = b * 8 + i
                    op("pe", lambda h, pv=pv, xn=xn, i=i, dc=dc: h.transpose(out=pv[:, i * 128:(i + 1) * 128], in_=xn[:, dc * 128:(dc + 1) * 128], identity=ident[:]), [xn, ident], [pb])
                e = "act" if b % 2 == 0 else "dve"
                src = pv.rearrange("p (a b) -> p a b", b=128)
                dst = dstT[:, b * 8:(b + 1) * 8, tt * 128:(tt + 1) * 128]
                if e == "act":
                    op("act", lambda h, src=src, dst=dst: h.activation(out=dst, in_=src, func=AF.Copy), [pb], [dstT])
                else:
                    op("dve", lambda h, src=src, dst=dst: h.tensor_copy(out=dst, in_=src), [pb], [dstT])

    def proj_fm(wb, col0, hnT, nkc, half, pbuf):
        for kc in range(nkc):
            op("pe", lambda h, kc=kc: h.matmul(pbuf[:], lhsT=wb[:, kc, col0:col0 + 128], rhs=hnT[:, kc, half * 512:(half + 1) * 512], start=(kc == 0), stop=(kc == nkc - 1)), [wb, hnT], [pbuf])

    hnT = ar.alloc("hnT", [32, T], BF16)
    m_hn = ar.mark()
    norm_transpose(lambda tt: xe[tt * 128:(tt + 1) * 128, :], 0, hnT, 8, "c")
    op("dve", lambda h: h.memset(halo[:], 0.0), [], [halo])
    op("dve", lambda h: h.tensor_copy(out=halo[:, :, 0:2], in_=hnT[:, :, T - 2:T]), [hnT], [halo])
    if upto == "A":
        fw.barrier()
        fw.finish()
        return nc, fw
    ar.reset(m_hn)

    def kv_transposes(vt, vtk, pbank):
        pv = psbf(pbank)
        for tt in range(8):
            op("pe", lambda h, tt=tt: h.transpose(out=pv[:, tt * 128:(tt + 1) * 128], in_=vt[:, tt * 128:(tt + 1) * 128], identity=ident[:]), [vt, ident], [ps[pbank]])
        op("act", lambda h: h.activation(out=vtk[:].rearrange("p a b -> p (a b)"), in_=pv, func=AF.Copy), [ps[pbank]], [vtk])

    wbs = [ar.alloc(f"wbB{i}", [32, 256], BF16) for i in range(2)]
    kts = [ar.alloc(f"ktB{i}", [T], BF16) for i in range(2)]
    vts = [ar.alloc(f"vtB{i}", [T], BF16) for i in range(2)]
    vtoks = [ar.alloc(f"vtokB{i}", [8, 128], BF16) for i in range(2)]

    def ldB(hd):
        load_w(wbs[hd % 2], w_in, D, [(4 * 2048 + hd * 128, 128), (5 * 2048 + hd * 128, 128)])

    ldB(0)
    pi = 0
    if upto == "B0":
        fw.barrier()
        fw.finish()
        return nc, fw
    for hd in range(16):
        if hd + 1 < 16:
            ldB(hd + 1)
        wb, kt, vt, vtk = wbs[hd % 2], kts[hd % 2], vts[hd % 2], vtoks[hd % 2]
        for part, dst in ((0, kt), (1, vt)):
            for half in range(2):
                pb = ps[pi % 4]
                pi += 1
                proj_fm(wb, part * 128, hnT, 32, half, pb)
                if part == 0:
                    for q_ in range(2):
                        o_ = half * 512 + q_ * 256
                        op("act", lambda h, pb=pb, dst=dst, o_=o_, q_=q_, jj=2 * half + q_, hd=hd: h.activation(out=dst[:, o_:o_ + 256], in_=pb[:, q_ * 256:(q_ + 1) * 256], func=AF.Copy, accum_out=kmsum[:, hd, jj:jj + 1]), [pb], [dst, kmsum])
                else:
                    op("act", lambda h, pb=pb, dst=dst, half=half: h.activation(out=dst[:, half * 512:(half + 1) * 512], in_=pb[:], func=AF.Copy), [pb], [dst])
        if upto == "B1" and hd == 0:
            fw.barrier()
            fw.finish()
            return nc, fw
        dma("sp", KTc[hd], kt[:], reads=[kt], writes=[KTc])
        if upto == "B2" and hd == 0:
            fw.barrier()
            fw.finish()
            return nc, fw
        kv_transposes(vt, vtk, 4 + hd % 2)
        if upto == "B3" and hd == 0:
            fw.barrier()
            fw.finish()
            return nc, fw
        dma("sp", Vc[hd].rearrange("tt p d -> p tt d"), vtk[:], reads=[vtk], writes=[Vc])

    if upto == "B":
        fw.barrier()
        fw.finish()
        return nc, fw
    ar.reset(m_hn)
    norm_transpose(lambda tt: xe[T + tt * 128:T + (tt + 1) * 128, :], 0, hnT, 8, "o")
    ar.reset(m_hn)

    wbs = [ar.alloc(f"wbG{i}", [32, 512], BF16) for i in range(2)]
    sgt = [ar.alloc(f"sgt{i}", [T], F32) for i in range(4)]

    def ldG(nb):
        load_w(wbs[nb % 2], w_in, D, [(6 * 2048 + nb * 256, 256), (6 * 2048 + 4096 + nb * 256, 256)])

    ldG(0)
    si = 0
    for nb in range(16):
        if nb + 1 < 16:
            ldG(nb + 1)
        wb = wbs[nb % 2]
        for part in range(2):
            for sc in range(2):
                n = nb * 2 + sc
                sg = sgt[si % 4]
                si += 1
                for half in range(2):
                    pb = ps[pi % 4]
                    pi += 1
                    proj_fm(wb, part * 256 + sc * 128, hnT, 32, half, pb)
                    op("act", lambda h, pb=pb, sg=sg, half=half, bn=part * 32 + n: h.activation(out=sg[:, half * 512:(half + 1) * 512], in_=pb[:], func=AF.Sigmoid, bias=bgate(bn)), [pb, cf], [sg])
                dma("sp", SG[part, n * 128:(n + 1) * 128, :], sg[:], reads=[sg], writes=[SG])

    if upto == "C2":
        fw.barrier()
        fw.finish()
        return nc, fw
    ar.reset(m_hn)
    wbs = [ar.alloc(f"wbC{i}", [32, 384], BF16) for i in range(2)]
    zb = [ar.alloc(f"z{i}", [T + 2], F32) for i in range(2)]
    zct = [ar.alloc(f"zct{i}", [T], BF16) for i in range(2)]
    usb = [ar.alloc(f"usb{i}", [512], F32) for i in range(2)]
    tmpc = [ar.alloc(f"tmpc{i}", [512], F32) for i in range(2)]
    uh = ar.alloc("uh", [2], F32)

    def ldC(j):
        load_w(wbs[j % 2], w_in, D, [(j * 128, 128), (2048 + j * 128, 128), (4096 + j * 128, 128)])

    ldC(0)
    for j in range(16):
        if j + 1 < 16:
            ldC(j + 1)
        wb, z, zc = wbs[j % 2], zb[j % 2], zct[j % 2]
        for pidx, c0 in ((0, 128), (1, 256)):
            for kc in range(32):
                op("pe", lambda h, kc=kc, c0=c0, pidx=pidx, wb=wb: h.matmul(ps[6 + pidx][:, 0:32], lhsT=wb[:, kc, c0:c0 + 128], rhs=halo[:, kc, :], start=(kc == 0), stop=(kc == 31)), [wb, halo], [ps[6 + pidx]])
        for half in range(2):
            pB, pC, pU = ps[half * 3], ps[half * 3 + 1], ps[half * 3 + 2]
            proj_fm(wb, 0, hnT, 32, half, pB)
            proj_fm(wb, 128, hnT, 32, half, pC)
            proj_fm(wb, 256, hnT, 32, half, pU)
            us, tm = usb[half], tmpc[half]
            o = half * 512
            if half == 0:
                op("act", lambda h: h.activation(out=uh[:], in_=ps[7][:, 0:2], func=AF.Copy), [ps[7], pU], [uh])
            op("act", lambda h, us=us, pU=pU: h.activation(out=us[:], in_=pU[:], func=AF.Copy), [pU], [us])
            op("dve", lambda h, z=z, o=o, pC=pC, us=us: h.tensor_tensor(out=z[:, 2 + o:2 + o + 512], in0=pC[:], in1=us[:], op=ALU.mult), [pC, us], [z])
            if half == 0:
                op("dve", lambda h, z=z: h.tensor_tensor(out=z[:, 0:2], in0=ps[6][:, 0:2], in1=uh[:], op=ALU.mult), [ps[6], uh], [z])
            op("dve", lambda h, z=z, o=o, tm=tm, j=j: h.tensor_scalar(out=tm[:], in0=z[:, 2 + o:2 + o + 512], scalar1=convw(2, j), scalar2=None, op0=ALU.mult), [z, cf], [tm])
            op("dve", lambda h, z=z, o=o, tm=tm, j=j: h.scalar_tensor_tensor(out=tm[:], in0=z[:, 1 + o:1 + o + 512], scalar=convw(1, j), in1=tm[:], op0=ALU.mult, op1=ALU.add), [z, cf, tm], [tm])
            op("dve", lambda h, z=z, o=o, tm=tm, j=j: h.scalar_tensor_tensor(out=tm[:], in0=z[:, o:o + 512], scalar=convw(0, j), in1=tm[:], op0=ALU.mult, op1=ALU.add), [z, cf, tm], [tm])
            op("dve", lambda h, zc=zc, o=o, tm=tm, pB=pB: h.tensor_tensor(out=zc[:, o:o + 512], in0=pB[:], in1=tm[:], op=ALU.mult), [pB, tm], [zc])
        dma("sp", ZC[j * 128:(j + 1) * 128, :], zc[:], reads=[zc], writes=[ZC])

    if upto == "C3":
        fw.barrier()
        fw.finish()
        return nc, fw
    ar.reset(m_hn)
    wbs = [ar.alloc(f"wbH{i}", [32, 384], BF16) for i in range(2)]
    ktcs = [ar.alloc(f"ktc{i}", [T], BF16) for i in range(2)]
    vcs = [ar.alloc(f"vc{i}", [8, 128], BF16) for i in range(2)]
    QTs = [ar.alloc(f"QT{i}", [T], BF16) for i in range(2)]
    KTs = [ar.alloc(f"KT{i}", [T], BF16) for i in range(2)]
    VTs = [ar.alloc(f"VT{i}", [T], BF16) for i in range(2)]
    vos = [ar.alloc(f"vo{i}", [8, 128], BF16) for i in range(2)]
    negTs = [ar.alloc(f"negT{i}", [T], BF16) for i in range(2)]
    kmb = ar.alloc("kmb", [8], BF16)
    gsb = ar.alloc("gsb", [8, 8], F32)
    g8 = ar.alloc("g8", [8, 8], F32)
    tg = ar.alloc("tg", [8], F32)
    selm = ar.alloc("selm", [8, 8], F32)
    negm = ar.alloc("negm", [8, 8], BF16)
    pts = [ar.alloc(f"pt{i}", [256], BF16) for i in range(3)]
    rden = ar.alloc("rden", [256], F32)
    att = [ar.alloc(f"att{i}", [T], BF16) for i in range(2)]
    scale = 128.0 ** -0.5

    def ldH(hd):
        load_w(wbs[hd % 2], w_in, D, [(3 * 2048 + hd * 128, 128), (4 * 2048 + hd * 128, 128), (5 * 2048 + hd * 128, 128)])
        dma("sp", ktcs[hd % 2][:], KTc[hd], reads=[KTc], writes=[ktcs[hd % 2]])
        dma("sp", vcs[hd % 2][:], Vc[hd].rearrange("tt p d -> p tt d"), reads=[Vc], writes=[vcs[hd % 2]])

    def stageP(hd):
        wb = wbs[hd % 2]
        QT, KT, VT, vo, negT = QTs[hd % 2], KTs[hd % 2], VTs[hd % 2], vos[hd % 2], negTs[hd % 2]
        for part, dst in ((0, QT), (1, KT), (2, VT)):
            for half in range(2):
                pb = ps[half]
                proj_fm(wb, part * 128, hnT, 32, half, pb)
                if part == 1:
                    for q_ in range(2):
                        o_ = half * 512 + q_ * 256
                        op("act", lambda h, pb=pb, dst=dst, o_=o_, q_=q_, jj=4 + 2 * half + q_, hd=hd: h.activation(out=dst[:, o_:o_ + 256], in_=pb[:, q_ * 256:(q_ + 1) * 256], func=AF.Copy, accum_out=kmsum[:, hd, jj:jj + 1]), [pb], [dst, kmsum])
                else:
                    op("act", lambda h, pb=pb, dst=dst, half=half: h.activation(out=dst[:, half * 512:(half + 1) * 512], in_=pb[:], func=AF.Copy), [pb], [dst])
        kv_transposes(VT, vo, 2)
        op("dve", lambda h, hd=hd: h.tensor_scalar(out=kmb[:], in0=kmsum[:, hd, :], scalar1=1.0 / 256.0, scalar2=None, op0=ALU.mult), [kmsum], [kmb])
        for tt in range(8):
            op("pe", lambda h, tt=tt, QT=QT: h.matmul(ps[2][:, tt * 8:(tt + 1) * 8], lhsT=QT[:, tt * 128:(tt + 1) * 128], rhs=kmb[:], start=True, stop=True), [QT, kmb], [ps[2]])
        op("dve", lambda h: h.tensor_tensor(out=gsb[:].rearrange("p a b -> p (a b)"), in0=ps[2][:, 0:64], in1=past[:].rearrange("p a b -> p (a b)"), op=ALU.add), [ps[2], past], [gsb])
        for tt in range(8):
            op("dve", lambda h, tt=tt: h.max(out=g8[:, tt, :], in_=gsb[:, tt, :]), [gsb], [g8])
        op("dve", lambda h: h.tensor_tensor(out=tg[:], in0=g8[:, :, 2], in1=g8[:, :, 3], op=ALU.add), [g8], [tg])
        op("dve", lambda h: h.tensor_scalar(out=tg[:], in0=tg[:], scalar1=0.5, scalar2=-0.9 * BIG, op0=ALU.mult, op1=ALU.max), [tg], [tg])
        op("dve", lambda h: h.tensor_tensor(out=selm[:], in0=gsb[:], in1=tg[:].unsqueeze(2).to_broadcast([128, 8, 8]), op=ALU.is_gt), [gsb, tg], [selm])
        op("dve", lambda h: h.tensor_scalar(out=negm[:], in0=selm[:], scalar1=-1.0, scalar2=-NEG, op0=ALU.add, op1=ALU.mult), [selm], [negm])

    def stageP2(hd):
        negT = negTs[hd % 2]
        pv2 = psbf(2)
        for tt in range(8):
            op("pe", lambda h, tt=tt, pv2=pv2: h.transpose(out=pv2[0:8, tt * 128:(tt + 1) * 128], in_=negm[:, tt, :], identity=ident[:]), [negm, ident], [ps[2]])
        op("act", lambda h, pv2=pv2, negT=negT: h.activation(out=negT[0:8, :], in_=pv2[0:8, :], func=AF.Copy), [ps[2]], [negT])

    sidx = [0]

    def stageA(hd):
        ktc, vc, at = ktcs[hd % 2], vcs[hd % 2], att[hd % 2]
        QT, KT, vo, negT = QTs[hd % 2], KTs[hd % 2], vos[hd % 2], negTs[hd % 2]
        for qb in range(4):
            qs = slice(qb * 256, (qb + 1) * 256)
            blocks = [(j, kc) for j in range(4 + qb + 1) for kc in range(2)]
            for bi, (j, kc) in enumerate(blocks):
                pS = ps[(3, 4, 7)[sidx[0] % 3]]
                pt = pts[sidx[0] % 3]
                sidx[0] += 1
                if j < 4:
                    kap = ktc[:, j * 256 + kc * 128:j * 256 + kc * 128 + 128]
                    vap = vc[:, 2 * j + kc, :]
                    kbuf, vbuf = ktc, vc
                else:
                    kap = KT[:, (j - 4) * 256 + kc * 128:(j - 4) * 256 + kc * 128 + 128]
                    vap = vo[:, 2 * (j - 4) + kc, :]
                    kbuf, vbuf = KT, vo
                op("pe", lambda h, pS=pS, kap=kap, qs=qs, QT=QT: h.matmul(pS[:, 0:256], lhsT=kap, rhs=QT[:, qs], start=True, stop=False), [kbuf, QT], [pS])
                if j == 4 + qb:
                    op("pe", lambda h, pS=pS, kc=kc: h.matmul(pS[:, 0:256], lhsT=ident[:], rhs=causal[:, kc, :], start=False, stop=True), [ident, causal], [pS])
                else:
                    op("pe", lambda h, pS=pS, j=j, qs=qs, negT=negT: h.matmul(pS[:, 0:256], lhsT=sel8[0:8, j, :], rhs=negT[0:8, qs], start=False, stop=True), [sel8, negT], [pS])
                op("act", lambda h, pS=pS, pt=pt: h.activation(out=pt[:], in_=pS[:, 0:256], func=AF.Exp, scale=scale), [pS], [pt])
                first, last = bi == 0, bi == len(blocks) - 1
                op("pe", lambda h, vap=vap, pt=pt, first=first, last=last: h.matmul(ps[5][:, 0:256], lhsT=vap, rhs=pt[:], start=first, stop=last), [vbuf, pt], [ps[5]])
                op("pe", lambda h, pt=pt, first=first, last=last: h.matmul(ps[6][:, 0:256], lhsT=ones[:], rhs=pt[:], start=first, stop=last), [ones, pt], [ps[6]])
            op("dve", lambda h: h.reciprocal(out=rden[:], in_=ps[6][:, 0:256]), [ps[6]], [rden])
            op("dve", lambda h, at=at, qs=qs: h.tensor_tensor(out=at[:, qs], in0=ps[5][:, 0:256], in1=rden[:], op=ALU.mult), [ps[5], rden], [at])
        dma("sp", AT[hd * 128:(hd + 1) * 128, :], at[:], reads=[at], writes=[AT])

    ldH(0)
    stageP(0)
    stageP2(0)
    for hd in range(16):
        if hd + 1 < 16:
            ldH(hd + 1)
            stageP(hd + 1)
        stageA(hd)
        if hd + 1 < 16:
            stageP2(hd + 1)

    if upto == "C4":
        fw.barrier()
        fw.finish()
        return nc, fw
    ar.reset(base_mark)
    mergedT = ar.alloc("mergedT", [32, T], BF16)
    m_mg = ar.mark()
    zcT = ar.alloc("zcT", [16, T], BF16)
    atT = ar.alloc("atT", [16, T], BF16)
    dma("sp", zcT[:], ZC[:].rearrange("(c p) t -> p c t", p=128), reads=[ZC], writes=[zcT])
    dma("sp", atT[:], AT[:].rearrange("(c p) t -> p c t", p=128), reads=[AT], writes=[atT])
    wcs = [ar.alloc(f"wco{i}", [16, 256], BF16) for i in range(2)]
    was = [ar.alloc(f"wao{i}", [16, 256], BF16) for i in range(2)]
    sgc = [ar.alloc(f"sgc{i}", [T], F32) for i in range(2)]
    sga = [ar.alloc(f"sga{i}", [T], F32) for i in range(2)]
    m1 = [ar.alloc(f"m1_{i}", [512], F32) for i in range(2)]
    m2 = [ar.alloc(f"m2_{i}", [512], F32) for i in range(2)]

    def ldD(nb):
        load_w(wcs[nb % 2], w_co, 2048, [(nb * 256, 256)])
        load_w(was[nb % 2], w_ao, 2048, [(nb * 256, 256)])

    ldD(0)
    k = 0
    for nb in range(16):
        if nb + 1 < 16:
            ldD(nb + 1)
        wc, wa = wcs[nb % 2], was[nb % 2]
        for sc in range(2):
            n = nb * 2 + sc
            gc_, ga_ = sgc[n % 2], sga[n % 2]
            dma("sp", gc_[:], SG[0, n * 128:(n + 1) * 128, :], reads=[SG], writes=[gc_])
            dma("sp", ga_[:], SG[1, n * 128:(n + 1) * 128, :], reads=[SG], writes=[ga_])
            for half in range(2):
                pc, pa = ps[(k % 2) * 2], ps[(k % 2) * 2 + 1]
                a1, a2 = m1[k % 2], m2[k % 2]
                k += 1
                hs = slice(half * 512, (half + 1) * 512)
                proj_fm(wc, sc * 128, zcT, 16, half, pc)
                proj_fm(wa, sc * 128, atT, 16, half, pa)
                op("dve", lambda h, a1=a1, pc=pc, gc_=gc_, hs=hs: h.tensor_tensor(out=a1[:], in0=pc[:], in1=gc_[:, hs], op=ALU.mult), [pc, gc_], [a1])
                op("dve", lambda h, a2=a2, pa=pa, ga_=ga_, hs=hs: h.tensor_tensor(out=a2[:], in0=pa[:], in1=ga_[:, hs], op=ALU.mult), [pa, ga_], [a2])
                op("pool", lambda h, a1=a1, a2=a2, n=n, hs=hs: h.tensor_tensor(out=mergedT[:, n, hs], in0=a1[:], in1=a2[:], op=ALU.add), [a1, a2], [mergedT])

    ar.reset(m_mg)
    wbs = [ar.alloc(f"wbO{i}", [32, 512], BF16) for i in range(2)]
    xts = [ar.alloc(f"xtE{i}", [512], F32) for i in range(3)]
    h2s = [ar.alloc(f"h2E{i}", [512], F32) for i in range(3)]

    def ldE(nb):
        load_w(wbs[nb % 2], w_o, D, [(nb * 512, 512)])

    ldE(0)
    k = 0
    for nb in range(8):
        if nb + 1 < 8:
            ldE(nb + 1)
        wb = wbs[nb % 2]
        for tt in range(8):
            pb = ps[k % 4]
            xt, h2 = xts[k % 3], h2s[k % 3]
            k += 1
            dma("sp", xt[:], xe[T + tt * 128:T + (tt + 1) * 128, nb * 512:(nb + 1) * 512], writes=[xt])
            for kc in range(32):
                op("pe", lambda h, pb=pb, kc=kc, tt=tt, wb=wb: h.matmul(pb[:], lhsT=mergedT[:, kc, tt * 128:(tt + 1) * 128], rhs=wb[:, kc, :], start=(kc == 0), stop=(kc == 31)), [mergedT, wb], [pb])
            op("dve", lambda h, pb=pb, xt=xt, h2=h2: h.tensor_tensor(out=h2[:], in0=pb[:], in1=xt[:], op=ALU.add), [pb, xt], [h2])
            dma("sp", H2[tt * 128:(tt + 1) * 128, nb * 512:(nb + 1) * 512], h2[:], reads=[h2], writes=[H2])

    if upto == "E":
        fw.barrier()
        fw.finish()
        return nc, fw
    ar.reset(small_mark)
    skT = ar.alloc("skT", [16, 128], BF16)
    base_mark = ar.mark()
    skf = ar.alloc("skf", [16, 128], F32)
    skb = ar.alloc("skb", [16, 128], BF16)
    dma("sp", skf[:], skeys[:].rearrange("c k d -> k c d"), writes=[skf])
    op("dve", lambda h: h.tensor_copy(out=skb[:], in_=skf[:]), [skf], [skb])
    for b in range(2):
        pv = psbf(b)
        for i in range(8):
            hc = b * 8 + i
            op("pe", lambda h, pv=pv, i=i, hc=hc: h.transpose(out=pv[:, i * 128:(i + 1) * 128], in_=skb[:, hc, :], identity=ident[:]), [skb, ident], [ps[b]])
        op("act", lambda h, pv=pv, b=b: h.activation(out=skT[:, b * 8:(b + 1) * 8, :].rearrange("p a b -> p (a b)"), in_=pv, func=AF.Copy), [ps[b]], [skT])

    GS = 8
    NG = 128 // GS
    for th in range(2):
        ar.reset(base_mark)
        t0 = th * 512
        hn2T = ar.alloc("hn2T", [32, 512], BF16)
        outacc = ar.alloc("outacc", [4, D], F32)
        A1 = ar.alloc("A1", [4, 8, 128], BF16)
        A2 = ar.alloc("A2", [4, 8, 128], BF16)
        rho = ar.alloc("rho", [4, 8], F32)
        m_pe = ar.mark()
        norm_transpose(lambda tt: H2[t0 + tt * 128:t0 + (tt + 1) * 128, :], 1, hn2T, 4, f"p{th}")
        ar.reset(m_pe)
        qT = ar.alloc("qT", [16, 512], BF16)
        m_q = ar.mark()
        wbs = [ar.alloc(f"wbQ{i}", [32, 256], BF16) for i in range(2)]

        def ldQ(cb):
            load_w(wbs[cb % 2], w_pq, D, [(cb * 256, 256)])

        ldQ(0)
        k = 0
        for cb in range(8):
            if cb + 1 < 8:
                ldQ(cb + 1)
            for sc in range(2):
                pb = ps[k % 4]
                k += 1
                proj_fm(wbs[cb % 2], sc * 128, hn2T, 32, 0, pb)
                op("act", lambda h, pb=pb, hc=cb * 2 + sc: h.activation(out=qT[:, hc, :], in_=pb[:], func=AF.Copy), [pb], [qT])
        ar.reset(m_q)
        S = ar.alloc("S", [16, 128], F32)
        t16 = ar.alloc("t16", [16, 16], F32)
        t16b = [Buf(fw, f"t16_{i}", t16.ap[:, i, :]) for i in range(16)]
        wkb = [ar.alloc(f"wk{i}", [128], F32) for i in range(16)]
        cand = ar.alloc("cand", [8, 256], F32)
        c24 = ar.alloc("c24", [8, 24], F32)
        c24b = [Buf(fw, f"c24_{i}", c24.ap[:, i, :]) for i in range(8)]
        wka = [ar.alloc(f"wka{i}", [256], F32) for i in range(8)]
        wkc = [ar.alloc(f"wkc{i}", [256], F32) for i in range(8)]
        sm = ar.alloc("sm", [8, 8], F32)
        ex = ar.alloc("ex", [8, 16], F32)
        for tt in range(4):
            for hc in range(16):
                pb = ps[4 + hc // 4]
                op("pe", lambda h, pb=pb, hc=hc, tt=tt: h.matmul(pb[:, (hc % 4) * 128:(hc % 4 + 1) * 128], lhsT=qT[:, hc, tt * 128:(tt + 1) * 128], rhs=skT[:, hc, :], start=True, stop=True), [qT, skT], [pb])
            for q4 in range(4):
                e = "act" if q4 % 2 == 0 else "dve"
                if e == "act":
                    op("act", lambda h, q4=q4: h.activation(out=S[:, q4 * 4:(q4 + 1) * 4, :].rearrange("p a b -> p (a b)"), in_=ps[4 + q4][:], func=AF.Copy), [ps[4 + q4]], [S])
                else:
                    op("dve", lambda h, q4=q4: h.tensor_copy(out=S[:, q4 * 4:(q4 + 1) * 4, :].rearrange("p a b -> p (a b)"), in_=ps[4 + q4][:]), [ps[4 + q4]], [S])
            for hc in range(16):
                op("dve", lambda h, hc=hc: h.max(out=t16[:, hc, 0:8], in_=S[:, hc, :]), [S], [t16b[hc]])
            for hc in range(16):
                op("dve", lambda h, hc=hc, w=wkb[hc]: h.match_replace(out=w[:], in_to_replace=t16[:, hc, 0:8], in_values=S[:, hc, :], imm_value=-BIG), [S, t16b[hc]], [wkb[hc]])
            for hc in range(16):
                op("dve", lambda h, hc=hc, w=wkb[hc]: h.max(out=t16[:, hc, 8:16], in_=w[:]), [wkb[hc]], [t16b[hc]])
            t16v = t16[:].rearrange("p (h c) k -> p h c k", c=2)
            op("dve", lambda h, t16v=t16v: h.tensor_tensor(out=cand[:].rearrange("p h (i j) -> p h i j", j=16), in0=t16v[:, :, 0, :].unsqueeze(3).to_broadcast([128, 8, 16, 16]), in1=t16v[:, :, 1, :].unsqueeze(2).to_broadcast([128, 8, 16, 16]), op=ALU.add), t16b, [cand])
            for hh in range(8):
                op("dve", lambda h, hh=hh: h.max(out=c24[:, hh, 0:8], in_=cand[:, hh, :]), [cand], [c24b[hh]])
            for hh in range(8):
                op("dve", lambda h, hh=hh, w=wka[hh]: h.match_replace(out=w[:], in_to_replace=c24[:, hh, 0:8], in_values=cand[:, hh, :], imm_value=-BIG), [cand, c24b[hh]], [wka[hh]])
            for hh in range(8):
                op("dve", lambda h, hh=hh, w=wka[hh]: h.max(out=c24[:, hh, 8:16], in_=w[:]), [wka[hh]], [c24b[hh]])
            for hh in range(8):
                op("dve", lambda h, hh=hh, w=wka[hh], w2=wkc[hh]: h.match_replace(out=w2[:], in_to_replace=c24[:, hh, 8:16], in_values=w[:], imm_value=-BIG), [wka[hh], c24b[hh]], [wkc[hh]])
            for hh in range(8):
                op("dve", lambda h, hh=hh, w2=wkc[hh]: h.max(out=c24[:, hh, 16:24], in_=w2[:]), [wkc[hh]], [c24b[hh]])
            op("dve", lambda h: h.tensor_tensor(out=sm[:, :, 0], in0=c24[:, :, 15], in1=c24[:, :, 16], op=ALU.add), c24b, [sm])
            op("dve", lambda h: h.tensor_scalar(out=sm[:, :, 0], in0=sm[:, :, 0], scalar1=0.5, scalar2=None, op0=ALU.mult), [sm], [sm])
            op("dve", lambda h: h.tensor_tensor(out=ex[:], in0=c24[:, :, 0:16], in1=c24[:, :, 0:1].to_broadcast([128, 8, 16]), op=ALU.subtract), c24b, [ex])
            for hh in range(8):
                op("act", lambda h, hh=hh: h.activation(out=ex[:, hh, :], in_=ex[:, hh, :], func=AF.Exp, accum_out=sm[:, hh, 2:3]), [ex], [ex, sm])
            op("act", lambda h: h.activation(out=sm[:, :, 3], in_=sm[:, :, 2], func=AF.Ln), [sm], [sm])
            op("dve", lambda h: h.tensor_tensor(out=sm[:, :, 4], in0=c24[:, :, 0], in1=sm[:, :, 3], op=ALU.add), c24b + [sm], [sm])
            op("dve", lambda h, t16v=t16v: h.tensor_scalar(out=sm[:, :, 5], in0=t16v[:, :, 0, 0], scalar1=-1.0, scalar2=None, op0=ALU.mult), t16b, [sm])
            op("dve", lambda h, t16v=t16v: h.tensor_tensor(out=sm[:, :, 6], in0=t16v[:, :, 0, 0], in1=sm[:, :, 4], op=ALU.subtract), t16b + [sm], [sm])
            op("dve", lambda h: h.tensor_tensor(out=sm[:, :, 7], in0=sm[:, :, 0], in1=sm[:, :, 4], op=ALU.subtract), [sm], [sm])
            op("act", lambda h, tt=tt: h.activation(out=rho[:, tt, :], in_=sm[:, :, 7], func=AF.Exp), [sm], [rho])
            for hh in range(8):
                op("act", lambda h, hh=hh, tt=tt: h.activation(out=A1[:, tt, hh, :], in_=S[:, 2 * hh, :], func=AF.Exp, bias=sm[:, hh, 5:6]), [S, sm], [A1])
                op("act", lambda h, hh=hh, tt=tt: h.activation(out=A2[:, tt, hh, :], in_=S[:, 2 * hh + 1, :], func=AF.Exp, bias=sm[:, hh, 6:7]), [S, sm], [A2])
        ar.reset(m_pe)
        Gaccs = [[ar.alloc(f"Gacc{i}_{tt}", [GS * 128], BF16) for tt in range(4)] for i in range(2)]
        Ebs = [ar.alloc(f"Eb{i}", [GS * 128], F32) for i in range(2)]
        ubs = [ar.alloc(f"ub{i}", [D], BF16) for i in range(2)]
        uTs = [ar.alloc(f"uT{i}", [32, 128], BF16) for i in range(2)]
        WTs = [ar.alloc(f"WT{i}", [GS, 512], BF16) for i in range(2)]
        vbs = [ar.alloc(f"vb{i}", [GS, 512], BF16) for i in range(2)]
        Hg = [ar.alloc(f"Hg{i}", [512], BF16) for i in range(2)]

        def ldU(ci):
            dma("pool", ubs[ci % 2][:], u_emb[ci * 128:(ci + 1) * 128, :], writes=[ubs[ci % 2]], max_dma_last_dim=8192, ndesc=256)

        def ldV(idx):
            g_, db = idx // 8, idx % 8
            vb = vbs[idx % 2]
            dma("pool", vb[:], v_emb[g_ * GS * 128:(g_ + 1) * GS * 128, db * 512:(db + 1) * 512].rearrange("(c p) n -> p c n", p=128), writes=[vb], ndesc=128 * GS)

        ldU(0)
        ldV(0)
        vidx = 0
        gcount = [0]
        pend = []

        def g_mask(item):
            g_, tt, hh, E = item
            Gb = Gaccs[g_ % 2][tt]
            rh = rho[:, tt, hh:hh + 1]
            if hh == 0:
                op("dve", lambda h, E=E, Gb=Gb, rh=rh: h.scalar_tensor_tensor(out=Gb[:], in0=E[:], scalar=rh, in1=E[:], op0=ALU.is_ge, op1=ALU.mult), [E, rho], [Gb])
            else:
                op("dve", lambda h, E=E, rh=rh: h.scalar_tensor_tensor(out=E[:], in0=E[:], scalar=rh, in1=E[:], op0=ALU.is_ge, op1=ALU.mult), [E, rho], [E])
                op("pool", lambda h, E=E, Gb=Gb: h.tensor_tensor(out=Gb[:], in0=Gb[:], in1=E[:], op=ALU.add), [Gb, E], [Gb])

        def gbuild(g_, tt, hh):
            k_ = gcount[0]
            gcount[0] += 1
            E = Ebs[k_ % 2]
            a1 = A1[:, tt, hh, g_ * GS:(g_ + 1) * GS].unsqueeze(2).to_broadcast([128, GS, 128])
            a2 = A2[:, tt, hh, :].unsqueeze(1).to_broadcast([128, GS, 128])
            Ev = E[:].rearrange("p (a b) -> p a b", b=128)
            if k_ % 2 == 0:
                op("dve", lambda h, Ev=Ev, a1=a1, a2=a2: h.scalar_tensor_tensor(out=Ev, in0=a2, scalar=1.0, in1=a1, op0=ALU.mult, op1=ALU.mult), [A1, A2], [E])
            else:
                op("pool", lambda h, Ev=Ev, a1=a1, a2=a2: h.tensor_tensor(out=Ev, in0=a1, in1=a2, op=ALU.mult), [A1, A2], [E])
            if pend:
                g_mask(pend.pop())
            pend.append((g_, tt, hh, E))

        def gflush():
            while pend:
                g_mask(pend.pop())

        gitems = [(tt, hh) for tt in range(4) for hh in range(8)]
        for (tt, hh) in gitems:
            gbuild(0, tt, hh)
        gflush()

        def stageT(ci):
            ub, uT = ubs[ci % 2], uTs[ci % 2]
            for b in range(4):
                pbk = (0, 1)[b % 2]
                pv = psbf(pbk)
                for i in range(8):
                    dc = b * 8 + i
                    op("pe", lambda h, pv=pv, i=i, dc=dc, ub=ub: h.transpose(out=pv[:, i * 128:(i + 1) * 128], in_=ub[:, dc * 128:(dc + 1) * 128], identity=ident[:]), [ub, ident], [ps[pbk]])
                op("act", lambda h, pv=pv, b=b, uT=uT: h.activation(out=uT[:, b * 8:(b + 1) * 8, :].rearrange("p a b -> p (a b)"), in_=pv, func=AF.Copy), [ps[pbk]], [uT])

        def stageH(g, ii):
            ci = g * GS + ii
            uT = uTs[ci % 2]
            pg = psbf(2)
            for tt in range(4):
                Gb = Gaccs[g % 2][tt]
                op("pe", lambda h, tt=tt, ii=ii, Gb=Gb, pg=pg: h.transpose(out=pg[:, tt * 128:(tt + 1) * 128], in_=Gb[:, ii * 128:(ii + 1) * 128], identity=ident[:]), [Gb, ident], [ps[2]])
            ph = ps[3 + ci % 2]
            for kc in range(32):
                op("pe", lambda h, ph=ph, kc=kc, uT=uT: h.matmul(ph[:], lhsT=uT[:, kc, :], rhs=hn2T[:, kc, :], start=(kc == 0), stop=(kc == 31)), [uT, hn2T], [ph])
            hg = Hg[ci % 2]
            op("act", lambda h, ph=ph, hg=hg: h.activation(out=hg[:], in_=ph[:], func=AF.Gelu), [ph], [hg])
            WTg = WTs[g % 2]
            op("dve", lambda h, hg=hg, ii=ii, pg=pg, WTg=WTg: h.tensor_tensor(out=WTg[:, ii, :], in0=pg[:, 0:512], in1=hg[:], op=ALU.mult), [ps[2], hg], [WTg])

        vstate = [0]

        def stageS(g, db):
            vidx = vstate[0]
            if vidx + 1 < NG * 8:
                ldV(vidx + 1)
            vb = vbs[vidx % 2]
            vstate[0] += 1
            WTg = WTs[g % 2]
            for tt in range(4):
                po = ps[5 + (db * 4 + tt) % 3]
                for ii in range(GS):
                    op("pe", lambda h, po=po, ii=ii, tt=tt, vb=vb, WTg=WTg: h.matmul(po[:], lhsT=WTg[:, ii, tt * 128:(tt + 1) * 128], rhs=vb[:, ii, :], start=(ii == 0), stop=(ii == GS - 1)), [WTg, vb], [po])
                dst = outacc[:, tt, db * 512:(db + 1) * 512]
                if g == 0:
                    op("dve", lambda h, po=po, dst=dst: h.tensor_copy(out=dst, in_=po[:]), [po], [outacc])
                else:
                    op("dve", lambda h, po=po, dst=dst: h.tensor_tensor(out=dst, in0=po[:], in1=dst, op=ALU.add), [po, outacc], [outacc])

        ldU(1)
        stageT(0)
        for g in range(NG):
            for ii in range(GS):
                ci = g * GS + ii
                if ci + 2 < 128:
                    ldU(ci + 2)
                if g + 1 < NG:
                    for (tt, hh) in gitems[ii * 4:(ii + 1) * 4]:
                        gbuild(g + 1, tt, hh)
                if ci + 1 < 128:
                    stageT(ci + 1)
                stageH(g, ii)
                if g >= 1:
                    stageS(g - 1, ii)
            gflush()
        for db in range(8):
            stageS(NG - 1, db)
        ar.reset(m_pe)
        gfin = ar.alloc("gfin", [D], F32)
        dma("sp", gfin[:], gvec[2:3, :].partition_broadcast(128), writes=[gfin])
        h2t = [ar.alloc(f"h2t{i}", [D], F32) for i in range(2)]
        jk = ar.alloc("jk", [D], BF16)
        stf = [ar.alloc(f"stf{i}", [4], F32) for i in range(2)]
        for tt in range(4):
            hx, s = h2t[tt % 2], stf[tt % 2]
            dma("sp", hx[:], H2[t0 + tt * 128:t0 + (tt + 1) * 128, :], reads=[H2], writes=[hx])
            op("dve", lambda h, hx=hx, tt=tt: h.tensor_tensor(out=hx[:], in0=hx[:], in1=outacc[:, tt, :], op=ALU.add), [hx, outacc], [hx])
            op("act", lambda h, hx=hx, s=s: h.activation(out=jk[:], in_=hx[:], func=AF.Square, accum_out=s[:, 0:1]), [hx], [jk, s])
            op("dve", lambda h, s=s: h.tensor_scalar(out=s[:, 1:2], in0=s[:, 0:1], scalar1=1.0 / D, scalar2=EPS, op0=ALU.mult, op1=ALU.add), [s], [s])
            op("act", lambda h, s=s: h.activation(out=s[:, 2:3], in_=s[:, 1:2], func=AF.Sqrt), [s], [s])
            op("dve", lambda h, s=s: h.reciprocal(out=s[:, 3:4], in_=s[:, 2:3]), [s], [s])
            op("dve", lambda h, hx=hx, s=s: h.scalar_tensor_tensor(out=hx[:], in0=hx[:], scalar=s[:, 3:4], in1=gfin[:], op0=ALU.mult, op1=ALU.mult), [hx, s, gfin], [hx])
            dma("sp", out[t0 + tt * 128:t0 + (tt + 1) * 128, :], hx[:], reads=[hx], writes=[out])

    fw.barrier()
    fw.finish()
    return nc, fw


_CACHE = {}


def _consts():
    ident = np.eye(128, dtype=np.float32)
    causal = np.zeros((128, 2, 256), np.float32)
    kk = np.arange(128)[:, None]
    qq = np.arange(256)[None, :]
    for kc in range(2):
        causal[:, kc, :] = np.where(kc * 128 + kk <= qq, 0.0, NEG)
    sel8 = np.zeros((8, 8, 128), np.float32)
    for j in range(8):
        sel8[j, j, :] = 1.0
    return ident, causal.reshape(128, 512), sel8.reshape(8, 1024)


def _past(second_half):
    p = np.full((8, 8), -BIG, np.float32)
    for tt in range(8):
        qb = 4 + tt // 2
        for j in range(8):
            if j < qb and (j >= 4 or second_half):
                p[tt, j] = 0.0
    return np.ascontiguousarray(np.broadcast_to(p.reshape(1, 64), (128, 64)))


def make_in_maps(x, norm_mix, w_in, b_gate, conv_w, w_conv_out, w_attn_out, w_o,
                 norm_ffn, w_peer_q, sub_keys, u_emb, v_emb, norm_final, cores=range(8)):
    f = lambda a: np.ascontiguousarray(np.asarray(a, dtype=np.float32))
    x = f(x)
    ident, causal, sel8 = _consts()
    gvec = f(np.stack([np.asarray(norm_mix)[0], np.asarray(norm_ffn)[0], np.asarray(norm_final)]))
    bg = np.asarray(b_gate, np.float32)[0].reshape(64, 128).T
    cw = np.asarray(conv_w, np.float32)[0].reshape(3, 16, 128).transpose(2, 0, 1).reshape(128, 48)
    cfm = f(np.concatenate([bg, cw], axis=1))
    shared = {
        "w_in": f(w_in[0]), "w_co": f(w_conv_out[0]), "w_ao": f(w_attn_out[0]), "w_o": f(w_o[0]),
        "w_pq": f(w_peer_q[0]), "skeys": f(np.asarray(sub_keys)[0].reshape(16, 128, 128)),
        "u_emb": f(u_emb[0]), "v_emb": f(v_emb[0]), "gvec": gvec, "cfm": cfm,
        "cid": ident, "ccausal": causal, "csel8": sel8,
    }
    maps = []
    for c in cores:
        b, hf = c // 2, c % 2
        xe = np.zeros((2048, D), np.float32)
        if hf == 1:
            xe[:] = x[b]
        else:
            xe[T:] = x[b, :T]
        m = dict(shared)
        m["xe"] = xe
        m["cpast"] = _past(hf == 1)
        maps.append(m)
    return maps


def kernel(**inputs):
    if "nc" not in _CACHE:
        _CACHE["nc"] = build()[0]
    nc = _CACHE["nc"]
    in_maps = make_in_maps(**inputs)
    res = run_bass_kernel_spmd(nc, in_maps, core_ids=list(range(8)))
    outp = np.empty((4, 2048, D), np.float32)
    for c in range(8):
        b, hf = c // 2, c % 2
        outp[b, hf * T:(hf + 1) * T] = res.results[c]["out"]
    return outp
```

```python
import numpy as np
import concourse.bass as bass
import concourse.mybir as mybir
from concourse.bass_utils import run_bass_kernel_spmd

F32 = mybir.dt.float32
BF16 = mybir.dt.bfloat16
U8 = mybir.dt.uint8
AF = mybir.ActivationFunctionType
ALU = mybir.AluOpType
AX = mybir.AxisListType

SEM_LIMIT = 30000
SWDGE_MAX_DESC = 6000
D = 4096
T = 1024
NEG = -60000.0
BIG = 1.0e30
EPS = 1e-6


class SemCtr:
    def __init__(self, nc, name):
        self.nc = nc
        self.name = name
        self.n = 0
        self.sem = nc.alloc_semaphore(name=f"{name}_{self.n}")
        self.val = 0

    def bump(self, k):
        if self.val + k > SEM_LIMIT:
            self.n += 1
            self.sem = self.nc.alloc_semaphore(name=f"{self.name}_{self.n}")
            self.val = 0
        self.val += k
        return (self.sem, self.val)


class Buf:
    _id = 0

    def __init__(self, fw, name, ap):
        self.fw = fw
        self.name = name
        self.ap = ap
        self.w = []
        self.r = []
        self.dsem = None

    def __getitem__(self, idx):
        return self.ap[idx]

    def dma_sem(self):
        if self.dsem is None:
            Buf._id += 1
            self.dsem = self.fw.get_dma_sem()
        return self.dsem


class Eng:
    def __init__(self, fw, name, handle):
        self.name = name
        self.h = handle
        self.ctr = SemCtr(fw.nc, f"e_{name}")
        self.seen = {}
        self.prog = []


class FW:
    def __init__(self, nc):
        self.nc = nc
        self.engs = {
            "pe": Eng(self, "pe", nc.tensor),
            "dve": Eng(self, "dve", nc.vector),
            "act": Eng(self, "act", nc.scalar),
            "pool": Eng(self, "pool", nc.gpsimd),
            "sp": Eng(self, "sp", nc.sync),
        }
        self.dma_sems = []
        self.free_dma_sems = []
        self.n_inst = 0
        self.swq = []

    def get_dma_sem(self):
        if self.free_dma_sems:
            return self.free_dma_sems.pop()
        s = SemCtr(self.nc, f"d{len(self.dma_sems)}")
        self.dma_sems.append(s)
        return s

    def dram(self, name, shape, dtype, kind="Internal"):
        t = self.nc.dram_tensor(name, list(shape), dtype, kind=kind)
        return Buf(self, name, t.ap())

    def _deps(self, eng, reads, writes):
        deps = {}

        def add(tok):
            sem, val = tok
            k = id(sem)
            if k not in deps or deps[k][1] < val:
                deps[k] = (sem, val)

        for b in reads:
            for tok in b.w:
                add(tok)
        for b in writes:
            for tok in b.w:
                add(tok)
            for tok in b.r:
                add(tok)
        out = []
        for k, (sem, val) in deps.items():
            if eng.name == "pe" and sem is eng.ctr.sem:
                continue
            if eng.seen.get(k, 0) >= val:
                continue
            eng.seen[k] = val
            out.append((sem, val))
        return out

    def _record(self, tok, reads, writes):
        for b in writes:
            b.w = [tok]
            b.r = []
        for b in reads:
            b.r = [t for t in b.r if t[0] is not tok[0]] + [tok]

    def op(self, engname, fn, reads=(), writes=()):
        eng = self.engs[engname]
        waits = self._deps(eng, reads, writes)
        tok = eng.ctr.bump(1)
        eng.prog.append((waits, fn, tok))
        self._record(tok, reads, writes)
        self.n_inst += 1
        return tok

    def dma(self, engname, out_ap, in_ap, reads=(), writes=(), **kw):
        eng = self.engs[engname]
        waits = self._deps(eng, reads, writes)
        if engname == "pool":
            nd = kw.pop("ndesc", 4096)
            while self.swq and sum(n for _, n in self.swq) + nd > SWDGE_MAX_DESC:
                (sem, val), _ = self.swq.pop(0)
                if eng.seen.get(id(sem), 0) < val:
                    eng.seen[id(sem)] = val
                    waits.append((sem, val))
        else:
            kw.pop("ndesc", None)
        tok = writes[0].dma_sem().bump(16)
        if engname == "pool":
            self.swq.append((tok, nd))

        def fn(h, out_ap=out_ap, in_ap=in_ap, kw=kw):
            return h.dma_start(out=out_ap, in_=in_ap, **kw)

        eng.prog.append((waits, fn, ("dma", tok)))
        self._record(tok, reads, writes)
        self.n_inst += 1
        return tok

    def barrier(self, bufs=()):
        toks = []
        for e in self.engs.values():
            if e.ctr.val > 0:
                toks.append((e.ctr.sem, e.ctr.val))
        for s in self.dma_sems:
            if s.val > 0:
                toks.append((s.sem, s.val))
        for e in self.engs.values():
            waits = []
            for sem, val in toks:
                k = id(sem)
                if e.seen.get(k, 0) >= val:
                    continue
                e.seen[k] = val
                waits.append((sem, val))
            e.prog.append((waits, None, None))

    def finish(self):
        nc = self.nc
        with nc.Block() as block:
            def replay(eng):
                def body(h):
                    for waits, fn, tok in eng.prog:
                        for sem, val in waits:
                            h.wait_ge(sem, val)
                        if fn is None:
                            continue
                        inst = fn(h)
                        if tok[0] == "dma":
                            inst.then_inc(tok[1][0], 16)
                        else:
                            inst.then_inc(tok[0], 1)
                return body

            block.tensor(replay(self.engs["pe"]))
            block.vector(replay(self.engs["dve"]))
            block.scalar(replay(self.engs["act"]))
            block.gpsimd(replay(self.engs["pool"]))
            block.sync(replay(self.engs["sp"]))


class Arena:
    def __init__(self, fw, nbytes):
        self.fw = fw
        self.t = fw.nc.alloc_sbuf_tensor("arena", [128, nbytes], U8)
        self.size = nbytes
        self.off = 0

    def alloc(self, name, shape, dtype):
        esz = 2 if dtype == BF16 else 4
        n = 1
        for s in shape:
            n *= s
        nb = (n * esz + 63) // 64 * 64
        assert self.off + nb <= self.size, f"arena overflow at {name}: {self.off + nb}"
        ap = self.t[:, self.off:self.off + n * esz].bitcast(dtype)
        self.off += nb
        if len(shape) == 2:
            ap = ap.rearrange("p (a b) -> p a b", b=shape[1])
        elif len(shape) == 3:
            ap = ap.rearrange("p (a b c) -> p a b c", b=shape[1], c=shape[2])
        return Buf(self.fw, name, ap)

    def mark(self):
        return self.off

    def reset(self, mark):
        self.fw.barrier()
        self.off = mark


def build(debug=False, upto=None):
    nc = bass.Bass("TRN2", target_bir_lowering=False)
    fw = FW(nc)
    op, dma = fw.op, fw.dma
    EI = "ExternalInput"
    xe = fw.dram("xe", [2048, D], F32, EI)
    w_in = fw.dram("w_in", [D, 20480], F32, EI)
    w_co = fw.dram("w_co", [2048, D], F32, EI)
    w_ao = fw.dram("w_ao", [2048, D], F32, EI)
    w_o = fw.dram("w_o", [D, D], F32, EI)
    w_pq = fw.dram("w_pq", [D, 2048], F32, EI)
    skeys = fw.dram("skeys", [16, 128, 128], F32, EI)
    u_emb = fw.dram("u_emb", [16384, D], F32, EI)
    v_emb = fw.dram("v_emb", [16384, D], F32, EI)
    gvec = fw.dram("gvec", [3, D], F32, EI)
    cfm = fw.dram("cfm", [128, 64 + 48], F32, EI)
    cid = fw.dram("cid", [128, 128], F32, EI)
    ccausal = fw.dram("ccausal", [128, 512], F32, EI)
    csel8 = fw.dram("csel8", [8, 1024], F32, EI)
    cpast = fw.dram("cpast", [128, 64], F32, EI)
    sk = "ExternalOutput" if debug else "Internal"
    KTc = fw.dram("KTc", [16, 128, 1024], BF16, sk)
    Vc = fw.dram("Vc", [16, 8, 128, 128], BF16, sk)
    SG = fw.dram("SG", [2, D, T], F32, sk)
    ZC = fw.dram("ZC", [2048, T], BF16, sk)
    AT = fw.dram("AT", [2048, T], BF16, sk)
    H2 = fw.dram("H2", [T, D], F32, sk)
    out = fw.dram("out", [T, D], F32, "ExternalOutput")

    ar = Arena(fw, 212480)
    ps = []
    for i in range(8):
        t = nc.alloc_psum_tensor(f"ps{i}", [128, 512], F32)
        ps.append(Buf(fw, f"ps{i}", t[:]))

    def psbf(i):
        return ps[i].ap.bitcast(BF16)

    ident_f = ar.alloc("ident_f", [128], F32)
    ident = ar.alloc("ident", [128], BF16)
    ones = ar.alloc("ones", [128], BF16)
    small_mark = ar.mark()
    causal_f = ar.alloc("causal_f", [512], F32)
    causal = ar.alloc("causal", [2, 256], BF16)
    sel8_f = ar.alloc("sel8_f", [1024], F32)
    sel8 = ar.alloc("sel8", [8, 128], BF16)
    past = ar.alloc("past", [8, 8], F32)
    cf = ar.alloc("cf", [112], F32)
    kmsum = ar.alloc("kmsum", [16, 8], F32)
    halo = ar.alloc("halo", [32, 32], BF16)
    dma("sp", ident_f[:], cid[:], writes=[ident_f])
    dma("sp", causal_f[:], ccausal[:], writes=[causal_f])
    dma("sp", sel8_f[0:8], csel8[:], writes=[sel8_f])
    dma("sp", past[:].rearrange("p a b -> p (a b)"), cpast[:], writes=[past])
    dma("sp", cf[:], cfm[:], writes=[cf])
    op("dve", lambda h: h.tensor_copy(out=ident[:], in_=ident_f[:]), [ident_f], [ident])
    op("dve", lambda h: h.memset(ones[:], 1.0), [], [ones])
    op("dve", lambda h: h.tensor_copy(out=causal[:].rearrange("p a b -> p (a b)"), in_=causal_f[:]), [causal_f], [causal])
    op("dve", lambda h: h.tensor_copy(out=sel8[0:8].rearrange("p a b -> p (a b)"), in_=sel8_f[0:8]), [sel8_f], [sel8])
    base_mark = ar.mark()
    if upto == "A0":
        fw.barrier()
        fw.finish()
        return nc, fw

    def bgate(n):
        return cf[:, n:n + 1]

    def convw(k, j):
        return cf[:, 64 + k * 16 + j:64 + k * 16 + j + 1]

    def load_w(dst, src, rows, parts):
        nkc = rows // 128
        off = 0
        for (c0, wd) in parts:
            s = src[0:rows, c0:c0 + wd].rearrange("(kc p) n -> p kc n", p=128)
            dma("pool", dst[:, 0:nkc, off:off + wd], s, writes=[dst], ndesc=128 * nkc)
            off += wd

    def norm_transpose(src_fn, gidx, dstT, ntiles, tag):
        g_bc = ar.alloc(f"gbc{tag}", [D], F32)
        dma("sp", g_bc[:], gvec[gidx:gidx + 1, :].partition_broadcast(128), writes=[g_bc])
        xts = [ar.alloc(f"xt{tag}{i}", [D], F32) for i in range(2)]
        xns = [ar.alloc(f"xn{tag}{i}", [D], BF16) for i in range(2)]
        st = [ar.alloc(f"st{tag}{i}", [4], F32) for i in range(2)]
        for tt in range(ntiles):
            xt, xn, s = xts[tt % 2], xns[tt % 2], st[tt % 2]
            dma("sp", xt[:], src_fn(tt), writes=[xt])
            op("act", lambda h, xt=xt, xn=xn, s=s: h.activation(out=xn[:], in_=xt[:], func=AF.Square, accum_out=s[:, 0:1]), [xt], [xn, s])
            op("dve", lambda h, s=s: h.tensor_scalar(out=s[:, 1:2], in0=s[:, 0:1], scalar1=1.0 / D, scalar2=EPS, op0=ALU.mult, op1=ALU.add), [s], [s])
            op("act", lambda h, s=s: h.activation(out=s[:, 2:3], in_=s[:, 1:2], func=AF.Sqrt), [s], [s])
            op("dve", lambda h, s=s: h.reciprocal(out=s[:, 3:4], in_=s[:, 2:3]), [s], [s])
            op("dve", lambda h, xt=xt, xn=xn, s=s: h.scalar_tensor_tensor(out=xn[:], in0=xt[:], scalar=s[:, 3:4], in1=g_bc[:], op0=ALU.mult, op1=ALU.mult), [xt, s, g_bc], [xn])
            for b in range(4):
                pb = ps[(tt * 4 + b) % 8]
                pv = psbf((tt * 4 + b) % 8)
                for i in range(8):
                    dc = b * 8 + i
                    op("pe", lambda h, pv=pv, xn=xn, i=i, dc=dc: h.transpose(out=pv[:, i * 128:(i + 1) * 128], in_=xn[:, dc * 128:(dc + 1) * 128], identity=ident[:]), [xn, ident], [pb])
                e = "act" if b % 2 == 0 else "dve"
                src = pv.rearrange("p (a b) -> p a b", b=128)
                dst = dstT[:, b * 8:(b + 1) * 8, tt * 128:(tt + 1) * 128]
                if e == "act":
                    op("act", lambda h, src=src, dst=dst: h.activation(out=dst, in_=src, func=AF.Copy), [pb], [dstT])
                else:
                    op("dve", lambda h, src=src, dst=dst: h.tensor_copy(out=dst, in_=src), [pb], [dstT])

    def proj_fm(wb, col0, hnT, nkc, half, pbuf):
        for kc in range(nkc):
            op("pe", lambda h, kc=kc: h.matmul(pbuf[:], lhsT=wb[:, kc, col0:col0 + 128], rhs=hnT[:, kc, half * 512:(half + 1) * 512], start=(kc == 0), stop=(kc == nkc - 1)), [wb, hnT], [pbuf])

    hnT = ar.alloc("hnT", [32, T], BF16)
    m_hn = ar.mark()
    norm_transpose(lambda tt: xe[tt * 128:(tt + 1) * 128, :], 0, hnT, 8, "c")
    op("dve", lambda h: h.memset(halo[:], 0.0), [], [halo])
    op("dve", lambda h: h.tensor_copy(out=halo[:, :, 0:2], in_=hnT[:, :, T - 2:T]), [hnT], [halo])
    if upto == "A":
        fw.barrier()
        fw.finish()
        return nc, fw
    ar.reset(m_hn)

    def kv_transposes(vt, vtk, pbank):
        pv = psbf(pbank)
        for tt in range(8):
            op("pe", lambda h, tt=tt: h.transpose(out=pv[:, tt * 128:(tt + 1) * 128], in_=vt[:, tt * 128:(tt + 1) * 128], identity=ident[:]), [vt, ident], [ps[pbank]])
        op("act", lambda h: h.activation(out=vtk[:].rearrange("p a b -> p (a b)"), in_=pv, func=AF.Copy), [ps[pbank]], [vtk])

    wbs = [ar.alloc(f"wbB{i}", [32, 256], BF16) for i in range(2)]
    kts = [ar.alloc(f"ktB{i}", [T], BF16) for i in range(2)]
    vts = [ar.alloc(f"vtB{i}", [T], BF16) for i in range(2)]
    vtoks = [ar.alloc(f"vtokB{i}", [8, 128], BF16) for i in range(2)]

    def ldB(hd):
        load_w(wbs[hd % 2], w_in, D, [(4 * 2048 + hd * 128, 128), (5 * 2048 + hd * 128, 128)])

    ldB(0)
    pi = 0
    if upto == "B0":
        fw.barrier()
        fw.finish()
        return nc, fw
    for hd in range(16):
        if hd + 1 < 16:
            ldB(hd + 1)
        wb, kt, vt, vtk = wbs[hd % 2], kts[hd % 2], vts[hd % 2], vtoks[hd % 2]
        for part, dst in ((0, kt), (1, vt)):
            for half in range(2):
                pb = ps[pi % 4]
                pi += 1
                proj_fm(wb, part * 128, hnT, 32, half, pb)
                if part == 0:
                    for q_ in range(2):
                        o_ = half * 512 + q_ * 256
                        op("act", lambda h, pb=pb, dst=dst, o_=o_, q_=q_, jj=2 * half + q_, hd=hd: h.activation(out=dst[:, o_:o_ + 256], in_=pb[:, q_ * 256:(q_ + 1) * 256], func=AF.Copy, accum_out=kmsum[:, hd, jj:jj + 1]), [pb], [dst, kmsum])
                else:
                    op("act", lambda h, pb=pb, dst=dst, half=half: h.activation(out=dst[:, half * 512:(half + 1) * 512], in_=pb[:], func=AF.Copy), [pb], [dst])
        if upto == "B1" and hd == 0:
            fw.barrier()
            fw.finish()
            return nc, fw
        dma("sp", KTc[hd], kt[:], reads=[kt], writes=[KTc])
        if upto == "B2" and hd == 0:
            fw.barrier()
            fw.finish()
            return nc, fw
        kv_transposes(vt, vtk, 4 + hd % 2)
        if upto == "B3" and hd == 0:
            fw.barrier()
            fw.finish()
            return nc, fw
        dma("sp", Vc[hd].rearrange("tt p d -> p tt d"), vtk[:], reads=[vtk], writes=[Vc])

    if upto == "B":
        fw.barrier()
        fw.finish()
        return nc, fw
    ar.reset(m_hn)
    norm_transpose(lambda tt: xe[T + tt * 128:T + (tt + 1) * 128, :], 0, hnT, 8, "o")
    ar.reset(m_hn)

    wbs = [ar.alloc(f"wbG{i}", [32, 512], BF16) for i in range(2)]
    sgt = [ar.alloc(f"sgt{i}", [T], F32) for i in range(4)]

    def ldG(nb):
        load_w(wbs[nb % 2], w_in, D, [(6 * 2048 + nb * 256, 256), (6 * 2048 + 4096 + nb * 256, 256)])

    ldG(0)
    si = 0
    for nb in range(16):
        if nb + 1 < 16:
            ldG(nb + 1)
        wb = wbs[nb % 2]
        for part in range(2):
            for sc in range(2):
                n = nb * 2 + sc
                sg = sgt[si % 4]
                si += 1
                for half in range(2):
                    pb = ps[pi % 4]
                    pi += 1
                    proj_fm(wb, part * 256 + sc * 128, hnT, 32, half, pb)
                    op("act", lambda h, pb=pb, sg=sg, half=half, bn=part * 32 + n: h.activation(out=sg[:, half * 512:(half + 1) * 512], in_=pb[:], func=AF.Sigmoid, bias=bgate(bn)), [pb, cf], [sg])
                dma("sp", SG[part, n * 128:(n + 1) * 128, :], sg[:], reads=[sg], writes=[SG])

    if upto == "C2":
        fw.barrier()
        fw.finish()
        return nc, fw
    ar.reset(m_hn)
    wbs = [ar.alloc(f"wbC{i}", [32, 384], BF16) for i in range(2)]
    zb = [ar.alloc(f"z{i}", [T + 2], F32) for i in range(2)]
    zct = [ar.alloc(f"zct{i}", [T], BF16) for i in range(2)]
    usb = [ar.alloc(f"usb{i}", [512], F32) for i in range(2)]
    tmpc = [ar.alloc(f"tmpc{i}", [512], F32) for i in range(2)]
    uh = ar.alloc("uh", [2], F32)

    def ldC(j):
        load_w(wbs[j % 2], w_in, D, [(j * 128, 128), (2048 + j * 128, 128), (4096 + j * 128, 128)])

    ldC(0)
    for j in range(16):
        if j + 1 < 16:
            ldC(j + 1)
        wb, z, zc = wbs[j % 2], zb[j % 2], zct[j % 2]
        for pidx, c0 in ((0, 128), (1, 256)):
            for kc in range(32):
                op("pe", lambda h, kc=kc, c0=c0, pidx=pidx, wb=wb: h.matmul(ps[6 + pidx][:, 0:32], lhsT=wb[:, kc, c0:c0 + 128], rhs=halo[:, kc, :], start=(kc == 0), stop=(kc == 31)), [wb, halo], [ps[6 + pidx]])
        for half in range(2):
            pB, pC, pU = ps[half * 3], ps[half * 3 + 1], ps[half * 3 + 2]
            proj_fm(wb, 0, hnT, 32, half, pB)
            proj_fm(wb, 128, hnT, 32, half, pC)
            proj_fm(wb, 256, hnT, 32, half, pU)
            us, tm = usb[half], tmpc[half]
            o = half * 512
            if half == 0:
                op("act", lambda h: h.activation(out=uh[:], in_=ps[7][:, 0:2], func=AF.Copy), [ps[7], pU], [uh])
            op("act", lambda h, us=us, pU=pU: h.activation(out=us[:], in_=pU[:], func=AF.Copy), [pU], [us])
            op("dve", lambda h, z=z, o=o, pC=pC, us=us: h.tensor_tensor(out=z[:, 2 + o:2 + o + 512], in0=pC[:], in1=us[:], op=ALU.mult), [pC, us], [z])
            if half == 0:
                op("dve", lambda h, z=z: h.tensor_tensor(out=z[:, 0:2], in0=ps[6][:, 0:2], in1=uh[:], op=ALU.mult), [ps[6], uh], [z])
            op("dve", lambda h, z=z, o=o, tm=tm, j=j: h.tensor_scalar(out=tm[:], in0=z[:, 2 + o:2 + o + 512], scalar1=convw(2, j), scalar2=None, op0=ALU.mult), [z, cf], [tm])
            op("dve", lambda h, z=z, o=o, tm=tm, j=j: h.scalar_tensor_tensor(out=tm[:], in0=z[:, 1 + o:1 + o + 512], scalar=convw(1, j), in1=tm[:], op0=ALU.mult, op1=ALU.add), [z, cf, tm], [tm])
            op("dve", lambda h, z=z, o=o, tm=tm, j=j: h.scalar_tensor_tensor(out=tm[:], in0=z[:, o:o + 512], scalar=convw(0, j), in1=tm[:], op0=ALU.mult, op1=ALU.add), [z, cf, tm], [tm])
            op("dve", lambda h, zc=zc, o=o, tm=tm, pB=pB: h.tensor_tensor(out=zc[:, o:o + 512], in0=pB[:], in1=tm[:], op=ALU.mult), [pB, tm], [zc])
        dma("sp", ZC[j * 128:(j + 1) * 128, :], zc[:], reads=[zc], writes=[ZC])

    if upto == "C3":
        fw.barrier()
        fw.finish()
        return nc, fw
    ar.reset(m_hn)
    wbs = [ar.alloc(f"wbH{i}", [32, 384], BF16) for i in range(2)]
    ktcs = [ar.alloc(f"ktc{i}", [T], BF16) for i in range(2)]
    vcs = [ar.alloc(f"vc{i}", [8, 128], BF16) for i in range(2)]
    QTs = [ar.alloc(f"QT{i}", [T], BF16) for i in range(2)]
    KTs = [ar.alloc(f"KT{i}", [T], BF16) for i in range(2)]
    VTs = [ar.alloc(f"VT{i}", [T], BF16) for i in range(2)]
    vos = [ar.alloc(f"vo{i}", [8, 128], BF16) for i in range(2)]
    negTs = [ar.alloc(f"negT{i}", [T], BF16) for i in range(2)]
    kmb = ar.alloc("kmb", [8], BF16)
    gsb = ar.alloc("gsb", [8, 8], F32)
    g8 = ar.alloc("g8", [8, 8], F32)
    tg = ar.alloc("tg", [8], F32)
    selm = ar.alloc("selm", [8, 8], F32)
    negm = ar.alloc("negm", [8, 8], BF16)
    pts = [ar.alloc(f"pt{i}", [256], BF16) for i in range(3)]
    rden = ar.alloc("rden", [256], F32)
    att = [ar.alloc(f"att{i}", [T], BF16) for i in range(2)]
    scale = 128.0 ** -0.5

    def ldH(hd):
        load_w(wbs[hd % 2], w_in, D, [(3 * 2048 + hd * 128, 128), (4 * 2048 + hd * 128, 128), (5 * 2048 + hd * 128, 128)])
        dma("sp", ktcs[hd % 2][:], KTc[hd], reads=[KTc], writes=[ktcs[hd % 2]])
        dma("sp", vcs[hd % 2][:], Vc[hd].rearrange("tt p d -> p tt d"), reads=[Vc], writes=[vcs[hd % 2]])

    def stageP(hd):
        wb = wbs[hd % 2]
        QT, KT, VT, vo, negT = QTs[hd % 2], KTs[hd % 2], VTs[hd % 2], vos[hd % 2], negTs[hd % 2]
        for part, dst in ((0, QT), (1, KT), (2, VT)):
            for half in range(2):
                pb = ps[half]
                proj_fm(wb, part * 128, hnT, 32, half, pb)
                if part == 1:
                    for q_ in range(2):
                        o_ = half * 512 + q_ * 256
                        op("act", lambda h, pb=pb, dst=dst, o_=o_, q_=q_, jj=4 + 2 * half + q_, hd=hd: h.activation(out=dst[:, o_:o_ + 256], in_=pb[:, q_ * 256:(q_ + 1) * 256], func=AF.Copy, accum_out=kmsum[:, hd, jj:jj + 1]), [pb], [dst, kmsum])
                else:
                    op("act", lambda h, pb=pb, dst=dst, half=half: h.activation(out=dst[:, half * 512:(half + 1) * 512], in_=pb[:], func=AF.Copy), [pb], [dst])
        kv_transposes(VT, vo, 2)
        op("dve", lambda h, hd=hd: h.tensor_scalar(out=kmb[:], in0=kmsum[:, hd, :], scalar1=1.0 / 256.0, scalar2=None, op0=ALU.mult), [kmsum], [kmb])
        for tt in range(8):
            op("pe", lambda h, tt=tt, QT=QT: h.matmul(ps[2][:, tt * 8:(tt + 1) * 8], lhsT=QT[:, tt * 128:(tt + 1) * 128], rhs=kmb[:], start=True, stop=True), [QT, kmb], [ps[2]])
        op("dve", lambda h: h.tensor_tensor(out=gsb[:].rearrange("p a b -> p (a b)"), in0=ps[2][:, 0:64], in1=past[:].rearrange("p a b -> p (a b)"), op=ALU.add), [ps[2], past], [gsb])
        for tt in range(8):
            op("dve", lambda h, tt=tt: h.max(out=g8[:, tt, :], in_=gsb[:, tt, :]), [gsb], [g8])
        op("dve", lambda h: h.tensor_tensor(out=tg[:], in0=g8[:, :, 2], in1=g8[:, :, 3], op=ALU.add), [g8], [tg])
        op("dve", lambda h: h.tensor_scalar(out=tg[:], in0=tg[:], scalar1=0.5, scalar2=-0.9 * BIG, op0=ALU.mult, op1=ALU.max), [tg], [tg])
        op("dve", lambda h: h.tensor_tensor(out=selm[:], in0=gsb[:], in1=tg[:].unsqueeze(2).to_broadcast([128, 8, 8]), op=ALU.is_gt), [gsb, tg], [selm])
        op("dve", lambda h: h.tensor_scalar(out=negm[:], in0=selm[:], scalar1=-1.0, scalar2=-NEG, op0=ALU.add, op1=ALU.mult), [selm], [negm])

    def stageP2(hd):
        negT = negTs[hd % 2]
        pv2 = psbf(2)
        for tt in range(8):
            op("pe", lambda h, tt=tt, pv2=pv2: h.transpose(out=pv2[0:8, tt * 128:(tt + 1) * 128], in_=negm[:, tt, :], identity=ident[:]), [negm, ident], [ps[2]])
        op("act", lambda h, pv2=pv2, negT=negT: h.activation(out=negT[0:8, :], in_=pv2[0:8, :], func=AF.Copy), [ps[2]], [negT])

    sidx = [0]

    def stageA(hd):
        ktc, vc, at = ktcs[hd % 2], vcs[hd % 2], att[hd % 2]
        QT, KT, vo, negT = QTs[hd % 2], KTs[hd % 2], vos[hd % 2], negTs[hd % 2]
        tiles = []
        for qb in range(4):
            blocks = [(j, kc) for j in range(4 + qb + 1) for kc in range(2)]
            for bi, (j, kc) in enumerate(blocks):
                tiles.append((qb, j, kc, bi == 0, bi == len(blocks) - 1, sidx[0]))
                sidx[0] += 1

        def emitS(t):
            qb, j, kc, first, last, k_ = t
            qs = slice(qb * 256, (qb + 1) * 256)
            pS = ps[(3, 4, 7)[k_ % 3]]
            pt = pts[k_ % 3]
            if j < 4:
                kap, kbuf = ktc[:, j * 256 + kc * 128:j * 256 + kc * 128 + 128], ktc
            else:
                kap, kbuf = KT[:, (j - 4) * 256 + kc * 128:(j - 4) * 256 + kc * 128 + 128], KT
            op("pe", lambda h, pS=pS, kap=kap, qs=qs, QT=QT: h.matmul(pS[:, 0:256], lhsT=kap, rhs=QT[:, qs], start=True, stop=False), [kbuf, QT], [pS])
            if j == 4 + qb:
                op("pe", lambda h, pS=pS, kc=kc: h.matmul(pS[:, 0:256], lhsT=ident[:], rhs=causal[:, kc, :], start=False, stop=True), [ident, causal], [pS])
            else:
                op("pe", lambda h, pS=pS, j=j, qs=qs, negT=negT: h.matmul(pS[:, 0:256], lhsT=sel8[0:8, j, :], rhs=negT[0:8, qs], start=False, stop=True), [sel8, negT], [pS])
            op("act", lambda h, pS=pS, pt=pt: h.activation(out=pt[:], in_=pS[:, 0:256], func=AF.Exp, scale=scale), [pS], [pt])

        def emitPV(t):
            qb, j, kc, first, last, k_ = t
            qs = slice(qb * 256, (qb + 1) * 256)
            pt = pts[k_ % 3]
            if j < 4:
                vap, vbuf = vc[:, 2 * j + kc, :], vc
            else:
                vap, vbuf = vo[:, 2 * (j - 4) + kc, :], vo
            op("pe", lambda h, vap=vap, pt=pt, first=first, last=last: h.matmul(ps[5][:, 0:256], lhsT=vap, rhs=pt[:], start=first, stop=last), [vbuf, pt], [ps[5]])
            op("pe", lambda h, pt=pt, first=first, last=last: h.matmul(ps[6][:, 0:256], lhsT=ones[:], rhs=pt[:], start=first, stop=last), [ones, pt], [ps[6]])
            if last:
                op("dve", lambda h: h.reciprocal(out=rden[:], in_=ps[6][:, 0:256]), [ps[6]], [rden])
                op("dve", lambda h, at=at, qs=qs: h.tensor_tensor(out=at[:, qs], in0=ps[5][:, 0:256], in1=rden[:], op=ALU.mult), [ps[5], rden], [at])

        emitS(tiles[0])
        for i, t in enumerate(tiles):
            if i + 1 < len(tiles):
                emitS(tiles[i + 1])
            emitPV(t)
        dma("sp", AT[hd * 128:(hd + 1) * 128, :], at[:], reads=[at], writes=[AT])

    ldH(0)
    stageP(0)
    stageP2(0)
    for hd in range(16):
        if hd + 1 < 16:
            ldH(hd + 1)
            stageP(hd + 1)
        stageA(hd)
        if hd + 1 < 16:
            stageP2(hd + 1)

    if upto == "C4":
        fw.barrier()
        fw.finish()
        return nc, fw
    ar.reset(base_mark)
    mergedT = ar.alloc("mergedT", [32, T], BF16)
    m_mg = ar.mark()
    zcT = ar.alloc("zcT", [16, T], BF16)
    atT = ar.alloc("atT", [16, T], BF16)
    dma("sp", zcT[:], ZC[:].rearrange("(c p) t -> p c t", p=128), reads=[ZC], writes=[zcT])
    dma("sp", atT[:], AT[:].rearrange("(c p) t -> p c t", p=128), reads=[AT], writes=[atT])
    wcs = [ar.alloc(f"wco{i}", [16, 256], BF16) for i in range(2)]
    was = [ar.alloc(f"wao{i}", [16, 256], BF16) for i in range(2)]
    sgc = [ar.alloc(f"sgc{i}", [T], F32) for i in range(2)]
    sga = [ar.alloc(f"sga{i}", [T], F32) for i in range(2)]
    m1 = [ar.alloc(f"m1_{i}", [512], F32) for i in range(2)]
    m2 = [ar.alloc(f"m2_{i}", [512], F32) for i in range(2)]

    def ldD(nb):
        load_w(wcs[nb % 2], w_co, 2048, [(nb * 256, 256)])
        load_w(was[nb % 2], w_ao, 2048, [(nb * 256, 256)])

    ldD(0)
    k = 0
    for nb in range(16):
        if nb + 1 < 16:
            ldD(nb + 1)
        wc, wa = wcs[nb % 2], was[nb % 2]
        for sc in range(2):
            n = nb * 2 + sc
            gc_, ga_ = sgc[n % 2], sga[n % 2]
            dma("sp", gc_[:], SG[0, n * 128:(n + 1) * 128, :], reads=[SG], writes=[gc_])
            dma("sp", ga_[:], SG[1, n * 128:(n + 1) * 128, :], reads=[SG], writes=[ga_])
            for half in range(2):
                pc, pa = ps[(k % 2) * 2], ps[(k % 2) * 2 + 1]
                a1, a2 = m1[k % 2], m2[k % 2]
                k += 1
                hs = slice(half * 512, (half + 1) * 512)
                proj_fm(wc, sc * 128, zcT, 16, half, pc)
                proj_fm(wa, sc * 128, atT, 16, half, pa)
                op("dve", lambda h, a1=a1, pc=pc, gc_=gc_, hs=hs: h.tensor_tensor(out=a1[:], in0=pc[:], in1=gc_[:, hs], op=ALU.mult), [pc, gc_], [a1])
                op("dve", lambda h, a2=a2, pa=pa, ga_=ga_, hs=hs: h.tensor_tensor(out=a2[:], in0=pa[:], in1=ga_[:, hs], op=ALU.mult), [pa, ga_], [a2])
                op("pool", lambda h, a1=a1, a2=a2, n=n, hs=hs: h.tensor_tensor(out=mergedT[:, n, hs], in0=a1[:], in1=a2[:], op=ALU.add), [a1, a2], [mergedT])

    ar.reset(m_mg)
    wbs = [ar.alloc(f"wbO{i}", [32, 512], BF16) for i in range(2)]
    xts = [ar.alloc(f"xtE{i}", [512], F32) for i in range(3)]
    h2s = [ar.alloc(f"h2E{i}", [512], F32) for i in range(3)]

    def ldE(nb):
        load_w(wbs[nb % 2], w_o, D, [(nb * 512, 512)])

    ldE(0)
    k = 0
    for nb in range(8):
        if nb + 1 < 8:
            ldE(nb + 1)
        wb = wbs[nb % 2]
        for tt in range(8):
            pb = ps[k % 4]
            xt, h2 = xts[k % 3], h2s[k % 3]
            k += 1
            dma("sp", xt[:], xe[T + tt * 128:T + (tt + 1) * 128, nb * 512:(nb + 1) * 512], writes=[xt])
            for kc in range(32):
                op("pe", lambda h, pb=pb, kc=kc, tt=tt, wb=wb: h.matmul(pb[:], lhsT=mergedT[:, kc, tt * 128:(tt + 1) * 128], rhs=wb[:, kc, :], start=(kc == 0), stop=(kc == 31)), [mergedT, wb], [pb])
            op("dve", lambda h, pb=pb, xt=xt, h2=h2: h.tensor_tensor(out=h2[:], in0=pb[:], in1=xt[:], op=ALU.add), [pb, xt], [h2])
            dma("sp", H2[tt * 128:(tt + 1) * 128, nb * 512:(nb + 1) * 512], h2[:], reads=[h2], writes=[H2])

    if upto == "E":
        fw.barrier()
        fw.finish()
        return nc, fw
    ar.reset(small_mark)
    skT = ar.alloc("skT", [16, 128], BF16)
    base_mark = ar.mark()
    skf = ar.alloc("skf", [16, 128], F32)
    skb = ar.alloc("skb", [16, 128], BF16)
    dma("sp", skf[:], skeys[:].rearrange("c k d -> k c d"), writes=[skf])
    op("dve", lambda h: h.tensor_copy(out=skb[:], in_=skf[:]), [skf], [skb])
    for b in range(2):
        pv = psbf(b)
        for i in range(8):
            hc = b * 8 + i
            op("pe", lambda h, pv=pv, i=i, hc=hc: h.transpose(out=pv[:, i * 128:(i + 1) * 128], in_=skb[:, hc, :], identity=ident[:]), [skb, ident], [ps[b]])
        op("act", lambda h, pv=pv, b=b: h.activation(out=skT[:, b * 8:(b + 1) * 8, :].rearrange("p a b -> p (a b)"), in_=pv, func=AF.Copy), [ps[b]], [skT])

    GS = 8
    NG = 128 // GS
    for th in range(2):
        ar.reset(base_mark)
        t0 = th * 512
        hn2T = ar.alloc("hn2T", [32, 512], BF16)
        outacc = ar.alloc("outacc", [4, D], F32)
        A1 = ar.alloc("A1", [4, 8, 128], BF16)
        A2 = ar.alloc("A2", [4, 8, 128], BF16)
        rho = ar.alloc("rho", [4, 8], F32)
        m_pe = ar.mark()
        norm_transpose(lambda tt: H2[t0 + tt * 128:t0 + (tt + 1) * 128, :], 1, hn2T, 4, f"p{th}")
        ar.reset(m_pe)
        qT = ar.alloc("qT", [16, 512], BF16)
        m_q = ar.mark()
        wbs = [ar.alloc(f"wbQ{i}", [32, 256], BF16) for i in range(2)]

        def ldQ(cb):
            load_w(wbs[cb % 2], w_pq, D, [(cb * 256, 256)])

        ldQ(0)
        k = 0
        for cb in range(8):
            if cb + 1 < 8:
                ldQ(cb + 1)
            for sc in range(2):
                pb = ps[k % 4]
                k += 1
                proj_fm(wbs[cb % 2], sc * 128, hn2T, 32, 0, pb)
                op("act", lambda h, pb=pb, hc=cb * 2 + sc: h.activation(out=qT[:, hc, :], in_=pb[:], func=AF.Copy), [pb], [qT])
        ar.reset(m_q)
        S = ar.alloc("S", [16, 128], F32)
        t16 = ar.alloc("t16", [16, 16], F32)
        t16b = [Buf(fw, f"t16_{i}", t16.ap[:, i, :]) for i in range(16)]
        wkb = [ar.alloc(f"wk{i}", [128], F32) for i in range(16)]
        cand = ar.alloc("cand", [8, 256], F32)
        c24 = ar.alloc("c24", [8, 24], F32)
        c24b = [Buf(fw, f"c24_{i}", c24.ap[:, i, :]) for i in range(8)]
        wka = [ar.alloc(f"wka{i}", [256], F32) for i in range(8)]
        wkc = [ar.alloc(f"wkc{i}", [256], F32) for i in range(8)]
        sm = ar.alloc("sm", [8, 8], F32)
        ex = ar.alloc("ex", [8, 16], F32)
        for tt in range(4):
            for hc in range(16):
                pb = ps[4 + hc // 4]
                op("pe", lambda h, pb=pb, hc=hc, tt=tt: h.matmul(pb[:, (hc % 4) * 128:(hc % 4 + 1) * 128], lhsT=qT[:, hc, tt * 128:(tt + 1) * 128], rhs=skT[:, hc, :], start=True, stop=True), [qT, skT], [pb])
            for q4 in range(4):
                e = "act" if q4 % 2 == 0 else "dve"
                if e == "act":
                    op("act", lambda h, q4=q4: h.activation(out=S[:, q4 * 4:(q4 + 1) * 4, :].rearrange("p a b -> p (a b)"), in_=ps[4 + q4][:], func=AF.Copy), [ps[4 + q4]], [S])
                else:
                    op("dve", lambda h, q4=q4: h.tensor_copy(out=S[:, q4 * 4:(q4 + 1) * 4, :].rearrange("p a b -> p (a b)"), in_=ps[4 + q4][:]), [ps[4 + q4]], [S])
            for hc in range(16):
                op("dve", lambda h, hc=hc: h.max(out=t16[:, hc, 0:8], in_=S[:, hc, :]), [S], [t16b[hc]])
            for hc in range(16):
                op("dve", lambda h, hc=hc, w=wkb[hc]: h.match_replace(out=w[:], in_to_replace=t16[:, hc, 0:8], in_values=S[:, hc, :], imm_value=-BIG), [S, t16b[hc]], [wkb[hc]])
            for hc in range(16):
                op("dve", lambda h, hc=hc, w=wkb[hc]: h.max(out=t16[:, hc, 8:16], in_=w[:]), [wkb[hc]], [t16b[hc]])
            t16v = t16[:].rearrange("p (h c) k -> p h c k", c=2)
            op("dve", lambda h, t16v=t16v: h.tensor_tensor(out=cand[:].rearrange("p h (i j) -> p h i j", j=16), in0=t16v[:, :, 0, :].unsqueeze(3).to_broadcast([128, 8, 16, 16]), in1=t16v[:, :, 1, :].unsqueeze(2).to_broadcast([128, 8, 16, 16]), op=ALU.add), t16b, [cand])
            for hh in range(8):
                op("dve", lambda h, hh=hh: h.max(out=c24[:, hh, 0:8], in_=cand[:, hh, :]), [cand], [c24b[hh]])
            for hh in range(8):
                op("dve", lambda h, hh=hh, w=wka[hh]: h.match_replace(out=w[:], in_to_replace=c24[:, hh, 0:8], in_values=cand[:, hh, :], imm_value=-BIG), [cand, c24b[hh]], [wka[hh]])
            for hh in range(8):
                op("dve", lambda h, hh=hh, w=wka[hh]: h.max(out=c24[:, hh, 8:16], in_=w[:]), [wka[hh]], [c24b[hh]])
            for hh in range(8):
                op("dve", lambda h, hh=hh, w=wka[hh], w2=wkc[hh]: h.match_replace(out=w2[:], in_to_replace=c24[:, hh, 8:16], in_values=w[:], imm_value=-BIG), [wka[hh], c24b[hh]], [wkc[hh]])
            for hh in range(8):
                op("dve", lambda h, hh=hh, w2=wkc[hh]: h.max(out=c24[:, hh, 16:24], in_=w2[:]), [wkc[hh]], [c24b[hh]])
            op("dve", lambda h: h.tensor_tensor(out=sm[:, :, 0], in0=c24[:, :, 15], in1=c24[:, :, 16], op=ALU.add), c24b, [sm])
            op("dve", lambda h: h.tensor_scalar(out=sm[:, :, 0], in0=sm[:, :, 0], scalar1=0.5, scalar2=None, op0=ALU.mult), [sm], [sm])
            op("dve", lambda h: h.tensor_tensor(out=ex[:], in0=c24[:, :, 0:16], in1=c24[:, :, 0:1].to_broadcast([128, 8, 16]), op=ALU.subtract), c24b, [ex])
            for hh in range(8):
                op("act", lambda h, hh=hh: h.activation(out=ex[:, hh, :], in_=ex[:, hh, :], func=AF.Exp, accum_out=sm[:, hh, 2:3]), [ex], [ex, sm])
            op("act", lambda h: h.activation(out=sm[:, :, 3], in_=sm[:, :, 2], func=AF.Ln), [sm], [sm])
            op("dve", lambda h: h.tensor_tensor(out=sm[:, :, 4], in0=c24[:, :, 0], in1=sm[:, :, 3], op=ALU.add), c24b + [sm], [sm])
            op("dve", lambda h, t16v=t16v: h.tensor_scalar(out=sm[:, :, 5], in0=t16v[:, :, 0, 0], scalar1=-1.0, scalar2=None, op0=ALU.mult), t16b, [sm])
            op("dve", lambda h, t16v=t16v: h.tensor_tensor(out=sm[:, :, 6], in0=t16v[:, :, 0, 0], in1=sm[:, :, 4], op=ALU.subtract), t16b + [sm], [sm])
            op("dve", lambda h: h.tensor_tensor(out=sm[:, :, 7], in0=sm[:, :, 0], in1=sm[:, :, 4], op=ALU.subtract), [sm], [sm])
            op("act", lambda h, tt=tt: h.activation(out=rho[:, tt, :], in_=sm[:, :, 7], func=AF.Exp), [sm], [rho])
            for hh in range(8):
                op("act", lambda h, hh=hh, tt=tt: h.activation(out=A1[:, tt, hh, :], in_=S[:, 2 * hh, :], func=AF.Exp, bias=sm[:, hh, 5:6]), [S, sm], [A1])
                op("act", lambda h, hh=hh, tt=tt: h.activation(out=A2[:, tt, hh, :], in_=S[:, 2 * hh + 1, :], func=AF.Exp, bias=sm[:, hh, 6:7]), [S, sm], [A2])
        ar.reset(m_pe)
        Gaccs = [[ar.alloc(f"Gacc{i}_{tt}", [GS * 128], BF16) for tt in range(4)] for i in range(2)]
        Ebs = [ar.alloc(f"Eb{i}", [GS * 128], F32) for i in range(2)]
        ubs = [ar.alloc(f"ub{i}", [D], BF16) for i in range(2)]
        uTs = [ar.alloc(f"uT{i}", [32, 128], BF16) for i in range(2)]
        WTs = [ar.alloc(f"WT{i}", [GS, 512], BF16) for i in range(2)]
        vbs = [ar.alloc(f"vb{i}", [GS, 512], BF16) for i in range(2)]
        Hg = [ar.alloc(f"Hg{i}", [512], BF16) for i in range(2)]

        def ldU(ci):
            dma("pool", ubs[ci % 2][:], u_emb[ci * 128:(ci + 1) * 128, :], writes=[ubs[ci % 2]], max_dma_last_dim=8192, ndesc=256)

        def ldV(idx):
            g_, db = idx // 8, idx % 8
            vb = vbs[idx % 2]
            dma("pool", vb[:], v_emb[g_ * GS * 128:(g_ + 1) * GS * 128, db * 512:(db + 1) * 512].rearrange("(c p) n -> p c n", p=128), writes=[vb], ndesc=128 * GS)

        ldU(0)
        ldV(0)
        vidx = 0
        gcount = [0]
        pend = []

        def g_mask(item):
            g_, tt, hh, E = item
            Gb = Gaccs[g_ % 2][tt]
            rh = rho[:, tt, hh:hh + 1]
            if hh == 0:
                op("dve", lambda h, E=E, Gb=Gb, rh=rh: h.scalar_tensor_tensor(out=Gb[:], in0=E[:], scalar=rh, in1=E[:], op0=ALU.is_ge, op1=ALU.mult), [E, rho], [Gb])
            else:
                op("dve", lambda h, E=E, rh=rh: h.scalar_tensor_tensor(out=E[:], in0=E[:], scalar=rh, in1=E[:], op0=ALU.is_ge, op1=ALU.mult), [E, rho], [E])
                op("pool", lambda h, E=E, Gb=Gb: h.tensor_tensor(out=Gb[:], in0=Gb[:], in1=E[:], op=ALU.add), [Gb, E], [Gb])

        def gbuild(g_, tt, hh):
            k_ = gcount[0]
            gcount[0] += 1
            E = Ebs[k_ % 2]
            a1 = A1[:, tt, hh, g_ * GS:(g_ + 1) * GS].unsqueeze(2).to_broadcast([128, GS, 128])
            a2 = A2[:, tt, hh, :].unsqueeze(1).to_broadcast([128, GS, 128])
            Ev = E[:].rearrange("p (a b) -> p a b", b=128)
            if k_ % 2 == 0:
                op("dve", lambda h, Ev=Ev, a1=a1, a2=a2: h.scalar_tensor_tensor(out=Ev, in0=a2, scalar=1.0, in1=a1, op0=ALU.mult, op1=ALU.mult), [A1, A2], [E])
            else:
                op("pool", lambda h, Ev=Ev, a1=a1, a2=a2: h.tensor_tensor(out=Ev, in0=a1, in1=a2, op=ALU.mult), [A1, A2], [E])
            if pend:
                g_mask(pend.pop())
            pend.append((g_, tt, hh, E))

        def gflush():
            while pend:
                g_mask(pend.pop())

        gitems = [(tt, hh) for tt in range(4) for hh in range(8)]
        for (tt, hh) in gitems:
            gbuild(0, tt, hh)
        gflush()

        def stageT(ci):
            ub, uT = ubs[ci % 2], uTs[ci % 2]
            for b in range(4):
                pbk = (0, 1)[b % 2]
                pv = psbf(pbk)
                for i in range(8):
                    dc = b * 8 + i
                    op("pe", lambda h, pv=pv, i=i, dc=dc, ub=ub: h.transpose(out=pv[:, i * 128:(i + 1) * 128], in_=ub[:, dc * 128:(dc + 1) * 128], identity=ident[:]), [ub, ident], [ps[pbk]])
                op("act", lambda h, pv=pv, b=b, uT=uT: h.activation(out=uT[:, b * 8:(b + 1) * 8, :].rearrange("p a b -> p (a b)"), in_=pv, func=AF.Copy), [ps[pbk]], [uT])

        def stageH(g, ii):
            ci = g * GS + ii
            uT = uTs[ci % 2]
            pg = psbf(2)
            for tt in range(4):
                Gb = Gaccs[g % 2][tt]
                op("pe", lambda h, tt=tt, ii=ii, Gb=Gb, pg=pg: h.transpose(out=pg[:, tt * 128:(tt + 1) * 128], in_=Gb[:, ii * 128:(ii + 1) * 128], identity=ident[:]), [Gb, ident], [ps[2]])
            ph = ps[3 + ci % 2]
            for kc in range(32):
                op("pe", lambda h, ph=ph, kc=kc, uT=uT: h.matmul(ph[:], lhsT=uT[:, kc, :], rhs=hn2T[:, kc, :], start=(kc == 0), stop=(kc == 31)), [uT, hn2T], [ph])
            hg = Hg[ci % 2]
            op("act", lambda h, ph=ph, hg=hg: h.activation(out=hg[:], in_=ph[:], func=AF.Gelu), [ph], [hg])
            WTg = WTs[g % 2]
            op("dve", lambda h, hg=hg, ii=ii, pg=pg, WTg=WTg: h.tensor_tensor(out=WTg[:, ii, :], in0=pg[:, 0:512], in1=hg[:], op=ALU.mult), [ps[2], hg], [WTg])

        vstate = [0]

        def stageS(g, db):
            vidx = vstate[0]
            if vidx + 1 < NG * 8:
                ldV(vidx + 1)
            vb = vbs[vidx % 2]
            vstate[0] += 1
            WTg = WTs[g % 2]
            for tt in range(4):
                po = ps[5 + (db * 4 + tt) % 3]
                for ii in range(GS):
                    op("pe", lambda h, po=po, ii=ii, tt=tt, vb=vb, WTg=WTg: h.matmul(po[:], lhsT=WTg[:, ii, tt * 128:(tt + 1) * 128], rhs=vb[:, ii, :], start=(ii == 0), stop=(ii == GS - 1)), [WTg, vb], [po])
                dst = outacc[:, tt, db * 512:(db + 1) * 512]
                if g == 0:
                    op("dve", lambda h, po=po, dst=dst: h.tensor_copy(out=dst, in_=po[:]), [po], [outacc])
                else:
                    op("dve", lambda h, po=po, dst=dst: h.tensor_tensor(out=dst, in0=po[:], in1=dst, op=ALU.add), [po, outacc], [outacc])

        ldU(1)
        stageT(0)
        for g in range(NG):
            for ii in range(GS):
                ci = g * GS + ii
                if ci + 2 < 128:
                    ldU(ci + 2)
                if g + 1 < NG:
                    for (tt, hh) in gitems[ii * 4:(ii + 1) * 4]:
                        gbuild(g + 1, tt, hh)
                if ci + 1 < 128:
                    stageT(ci + 1)
                stageH(g, ii)
                if g >= 1:
                    stageS(g - 1, ii)
            gflush()
        for db in range(8):
            stageS(NG - 1, db)
        ar.reset(m_pe)
        gfin = ar.alloc("gfin", [D], F32)
        dma("sp", gfin[:], gvec[2:3, :].partition_broadcast(128), writes=[gfin])
        h2t = [ar.alloc(f"h2t{i}", [D], F32) for i in range(2)]
        jk = ar.alloc("jk", [D], BF16)
        stf = [ar.alloc(f"stf{i}", [4], F32) for i in range(2)]
        for tt in range(4):
            hx, s = h2t[tt % 2], stf[tt % 2]
            dma("sp", hx[:], H2[t0 + tt * 128:t0 + (tt + 1) * 128, :], reads=[H2], writes=[hx])
            op("dve", lambda h, hx=hx, tt=tt: h.tensor_tensor(out=hx[:], in0=hx[:], in1=outacc[:, tt, :], op=ALU.add), [hx, outacc], [hx])
            op("act", lambda h, hx=hx, s=s: h.activation(out=jk[:], in_=hx[:], func=AF.Square, accum_out=s[:, 0:1]), [hx], [jk, s])
            op("dve", lambda h, s=s: h.tensor_scalar(out=s[:, 1:2], in0=s[:, 0:1], scalar1=1.0 / D, scalar2=EPS, op0=ALU.mult, op1=ALU.add), [s], [s])
            op("act", lambda h, s=s: h.activation(out=s[:, 2:3], in_=s[:, 1:2], func=AF.Sqrt), [s], [s])
            op("dve", lambda h, s=s: h.reciprocal(out=s[:, 3:4], in_=s[:, 2:3]), [s], [s])
            op("dve", lambda h, hx=hx, s=s: h.scalar_tensor_tensor(out=hx[:], in0=hx[:], scalar=s[:, 3:4], in1=gfin[:], op0=ALU.mult, op1=ALU.mult), [hx, s, gfin], [hx])
            dma("sp", out[t0 + tt * 128:t0 + (tt + 1) * 128, :], hx[:], reads=[hx], writes=[out])

    fw.barrier()
    fw.finish()
    return nc, fw


_CACHE = {}


def _consts():
    ident = np.eye(128, dtype=np.float32)
    causal = np.zeros((128, 2, 256), np.float32)
    kk = np.arange(128)[:, None]
    qq = np.arange(256)[None, :]
    for kc in range(2):
        causal[:, kc, :] = np.where(kc * 128 + kk <= qq, 0.0, NEG)
    sel8 = np.zeros((8, 8, 128), np.float32)
    for j in range(8):
        sel8[j, j, :] = 1.0
    return ident, causal.reshape(128, 512), sel8.reshape(8, 1024)


def _past(second_half):
    p = np.full((8, 8), -BIG, np.float32)
    for tt in range(8):
        qb = 4 + tt // 2
        for j in range(8):
            if j < qb and (j >= 4 or second_half):
                p[tt, j] = 0.0
    return np.ascontiguousarray(np.broadcast_to(p.reshape(1, 64), (128, 64)))


def make_in_maps(x, norm_mix, w_in, b_gate, conv_w, w_conv_out, w_attn_out, w_o,
                 norm_ffn, w_peer_q, sub_keys, u_emb, v_emb, norm_final, cores=range(8)):
    f = lambda a: np.ascontiguousarray(np.asarray(a, dtype=np.float32))
    x = f(x)
    ident, causal, sel8 = _consts()
    gvec = f(np.stack([np.asarray(norm_mix)[0], np.asarray(norm_ffn)[0], np.asarray(norm_final)]))
    bg = np.asarray(b_gate, np.float32)[0].reshape(64, 128).T
    cw = np.asarray(conv_w, np.float32)[0].reshape(3, 16, 128).transpose(2, 0, 1).reshape(128, 48)
    cfm = f(np.concatenate([bg, cw], axis=1))
    shared = {
        "w_in": f(w_in[0]), "w_co": f(w_conv_out[0]), "w_ao": f(w_attn_out[0]), "w_o": f(w_o[0]),
        "w_pq": f(w_peer_q[0]), "skeys": f(np.asarray(sub_keys)[0].reshape(16, 128, 128)),
        "u_emb": f(u_emb[0]), "v_emb": f(v_emb[0]), "gvec": gvec, "cfm": cfm,
        "cid": ident, "ccausal": causal, "csel8": sel8,
    }
    maps = []
    for c in cores:
        b, hf = c // 2, c % 2
        xe = np.zeros((2048, D), np.float32)
        if hf == 1:
            xe[:] = x[b]
        else:
            xe[T:] = x[b, :T]
        m = dict(shared)
        m["xe"] = xe
        m["cpast"] = _past(hf == 1)
        maps.append(m)
    return maps


def kernel(**inputs):
    if "nc" not in _CACHE:
        _CACHE["nc"] = build()[0]
    nc = _CACHE["nc"]
    in_maps = make_in_maps(**inputs)
    res = run_bass_kernel_spmd(nc, in_maps, core_ids=list(range(8)))
    outp = np.empty((4, 2048, D), np.float32)
    for c in range(8):
        b, hf = c // 2, c % 2
        outp[b, hf * T:(hf + 1) * T] = res.results[c]["out"]
    return outp
```
